# Optimizing a Trainium2 kernel written in Bass

```python
import jax, jax.numpy as jnp
from jax import lax
import numpy as np

D_MODEL = 2048
BATCH = 16
SEQ = 2048
DEPTH = 2

N_ATTN_HEADS = 8
ATTN_HEAD_DIM = 128
ATTN_ROT_DIM = ATTN_HEAD_DIM // 4
ROPE_THETA = 500000.0
Q_BLOCK = 128
TOPK_MAX = 256
N_IDX_HEADS = 16
IDX_HEAD_DIM = 64
IDX_ROT_DIM = IDX_HEAD_DIM // 4
N_RET_HEADS = 8
RET_KEY_DIM = 64
RET_VAL_DIM = 128
RET_CHUNK = 128
RET_THETA = 10000.0
ATTN_WIDTH = N_ATTN_HEADS * ATTN_HEAD_DIM
RET_WIDTH = N_RET_HEADS * RET_VAL_DIM
MIX_WIDTH = ATTN_WIDTH + RET_WIDTH
D_FF = 4 * D_MODEL
PLE_DIM = 256
RMS_EPS = 1e-6
GN_EPS = 1e-5
SPLIT_SIZES = (
    ATTN_WIDTH,
    ATTN_HEAD_DIM,
    ATTN_HEAD_DIM,
    N_IDX_HEADS * IDX_HEAD_DIM,
    IDX_HEAD_DIM,
    N_IDX_HEADS,
    N_RET_HEADS * RET_KEY_DIM,
    N_RET_HEADS * RET_KEY_DIM,
    RET_WIDTH,
    RET_WIDTH,
)
IN_WIDTH = sum(SPLIT_SIZES)

kernel_name = "hymba_dsa_retention_hybrid"


def rmsnorm(x, w):
    xf = x.astype(jnp.float32)
    y = xf * lax.rsqrt(jnp.mean(xf * xf, axis=-1, keepdims=True) + RMS_EPS)
    return (y * w.astype(jnp.float32)).astype(x.dtype)


def rope_tables(positions, rot_dim, theta):
    half = rot_dim // 2
    inv = theta ** (-jnp.arange(half, dtype=jnp.float32) / half)
    ang = positions.astype(jnp.float32)[..., None] * inv
    return jnp.cos(ang), jnp.sin(ang)


def apply_rope(x, cos, sin):
    half = cos.shape[-1]
    if x.ndim == 4:
        cos, sin = cos[:, :, None, :], sin[:, :, None, :]
    cos, sin = cos.astype(x.dtype), sin.astype(x.dtype)
    x1, x2, rest = x[..., :half], x[..., half:2 * half], x[..., 2 * half:]
    return jnp.concatenate([x1 * cos - x2 * sin, x2 * cos + x1 * sin, rest], axis=-1)


def split_columns(z):
    offsets, acc = [], 0
    for s in SPLIT_SIZES[:-1]:
        acc += s
        offsets.append(acc)
    return jnp.split(z, offsets, axis=-1)


def dsa_attention(q, k, v, iq, ik, iw, topk):
    B, S = q.shape[0], q.shape[1]
    nb = S // Q_BLOCK
    key_pos = jnp.arange(S)
    ikf = ik.astype(jnp.float32)
    idx_scale = IDX_HEAD_DIM ** -0.5
    w_scale = N_IDX_HEADS ** -0.5
    attn_scale = ATTN_HEAD_DIM ** -0.5
    gather = jax.vmap(lambda arr, ids: arr[ids])

    def to_blocks(a):
        return a.reshape((B, nb, Q_BLOCK) + a.shape[2:]).swapaxes(0, 1)

    def one_block(args):
        qb, iqb, iwb, qpos = args
        logits = jnp.einsum('bqhd,bsd->bqhs', iqb.astype(jnp.float32), ikf) * idx_scale
        score = jnp.einsum('bqh,bqhs->bqs', iwb.astype(jnp.float32) * w_scale,
                           jax.nn.relu(logits))
        causal = key_pos[None, :] <= qpos[:, None]
        score = jnp.where(causal[None], score, -jnp.inf)
        _, sel = lax.top_k(score, topk)
        k_sel = gather(k, sel)
        v_sel = gather(v, sel)
        valid = sel <= qpos[None, :, None]
        s = jnp.einsum('bqhd,bqkd->bqhk', qb, k_sel).astype(jnp.float32) * attn_scale
        s = jnp.where(valid[:, :, None, :], s, -jnp.inf)
        pr = jax.nn.softmax(s, axis=-1).astype(v.dtype)
        return jnp.einsum('bqhk,bqkd->bqhd', pr, v_sel)

    qpos = jnp.arange(S).reshape(nb, Q_BLOCK)
    out = lax.map(one_block, (to_blocks(q), to_blocks(iq), to_blocks(iw), qpos))
    return out.swapaxes(0, 1).reshape(B, S, N_ATTN_HEADS, ATTN_HEAD_DIM)


def retention(q, k, v):
    B, S, H, dk = q.shape
    dv = v.shape[-1]
    C = RET_CHUNK
    nc = S // C
    gamma = 1.0 - 2.0 ** (-5.0 - jnp.arange(H, dtype=jnp.float32))
    log_g = jnp.log(gamma)
    i = jnp.arange(C, dtype=jnp.float32)
    diff = i[:, None] - i[None, :]
    decay = jnp.where(diff[None] >= 0, jnp.exp(jnp.maximum(diff, 0.0)[None] * log_g[:, None, None]), 0.0)
    zeta = jnp.exp((C - 1.0 - i)[None, :] * log_g[:, None])
    xi = jnp.exp((i + 1.0)[None, :] * log_g[:, None])
    g_chunk = jnp.exp(C * log_g)

    def to_chunks(a):
        return a.astype(jnp.float32).reshape(B, nc, C, H, a.shape[-1]).transpose(1, 0, 3, 2, 4)

    qc = to_chunks(q)
    kc = to_chunks(k) * (dk ** -0.5)
    vc = to_chunks(v)

    def step(R, inp):
        qb, kb, vb = inp
        inner = jnp.einsum('bhid,bhjd->bhij', qb, kb) * decay[None]
        o = (jnp.einsum('bhij,bhjv->bhiv', inner, vb)
             + jnp.einsum('bhid,bhdv->bhiv', qb, R) * xi[None, :, :, None])
        R = g_chunk[None, :, None, None] * R + jnp.einsum(
            'bhjd,bhjv->bhdv', kb * zeta[None, :, :, None], vb)
        return R, o

    R0 = jnp.zeros((B, H, dk, dv), jnp.float32)
    _, o = lax.scan(step, R0, (qc, kc, vc))
    return o.transpose(1, 0, 3, 2, 4).reshape(B, S, H, dv)


def head_groupnorm(o, w):
    mu = jnp.mean(o, axis=-1, keepdims=True)
    var = jnp.mean(jnp.square(o - mu), axis=-1, keepdims=True)
    y = (o - mu) * lax.rsqrt(var + GN_EPS)
    return y.reshape(o.shape[0], o.shape[1], -1) * w.astype(jnp.float32)


def hybrid_layer(h, p_i, rope_attn, rope_idx, rope_ret, w_in, w_out, w_ff1, w_ff2,
                 w_ple, w_ple_gate, pre_mix_norm, post_mix_norm, pre_ff_norm,
                 post_ff_norm, ple_norm, ret_gn, topk):
    B, S, _ = h.shape
    a = rmsnorm(h, pre_mix_norm)
    z = a @ w_in
    aq, ak, av, iq, ik, iw, rq, rk, rv, rg = split_columns(z)
    aq = apply_rope(aq.reshape(B, S, N_ATTN_HEADS, ATTN_HEAD_DIM), *rope_attn)
    ak = apply_rope(ak, *rope_attn)
    iq = apply_rope(iq.reshape(B, S, N_IDX_HEADS, IDX_HEAD_DIM), *rope_idx)
    ik = apply_rope(ik, *rope_idx)
    attn = dsa_attention(aq, ak, av, iq, ik, iw, topk).reshape(B, S, ATTN_WIDTH)

    rq = apply_rope(rq.reshape(B, S, N_RET_HEADS, RET_KEY_DIM), *rope_ret)
    rk = apply_rope(rk.reshape(B, S, N_RET_HEADS, RET_KEY_DIM), *rope_ret)
    rv = rv.reshape(B, S, N_RET_HEADS, RET_VAL_DIM)
    ret = retention(rq, rk, rv)
    ret = (head_groupnorm(ret, ret_gn) * jax.nn.silu(rg.astype(jnp.float32))).astype(h.dtype)

    mix = jnp.concatenate([attn, ret], axis=-1) @ w_out
    h = h + rmsnorm(mix, post_mix_norm)
    m = rmsnorm(h, pre_ff_norm)
    y = jnp.square(jax.nn.relu(m @ w_ff1)) @ w_ff2
    h = h + rmsnorm(y, post_ff_norm)
    e = p_i @ w_ple
    g = jax.nn.sigmoid(h @ w_ple_gate)
    h = h + rmsnorm(g * e, ple_norm)
    return h


def setup_inputs(seed: int = 0) -> dict:
    key = jax.random.key(seed)
    ks = jax.random.split(key, 16)
    f32 = jnp.float32

    def nrm(k, shape, scale):
        return jax.random.normal(k, shape, f32) * scale

    def gain(k, n):
        return 1.0 + 0.02 * jax.random.normal(k, (DEPTH, n), f32)

    return {
        "x": nrm(ks[0], (BATCH, SEQ, D_MODEL), 1.0),
        "p": nrm(ks[1], (DEPTH, BATCH, SEQ, PLE_DIM), 1.0),
        "positions": jnp.broadcast_to(jnp.arange(SEQ, dtype=jnp.int32), (BATCH, SEQ)),
        "w_in": nrm(ks[2], (DEPTH, D_MODEL, IN_WIDTH), D_MODEL ** -0.5),
        "w_out": nrm(ks[3], (DEPTH, MIX_WIDTH, D_MODEL), MIX_WIDTH ** -0.5),
        "w_ff1": nrm(ks[4], (DEPTH, D_MODEL, D_FF), D_MODEL ** -0.5),
        "w_ff2": nrm(ks[5], (DEPTH, D_FF, D_MODEL), D_FF ** -0.5),
        "w_ple": nrm(ks[6], (DEPTH, PLE_DIM, D_MODEL), PLE_DIM ** -0.5),
        "w_ple_gate": nrm(ks[7], (DEPTH, D_MODEL, D_MODEL), D_MODEL ** -0.5),
        "pre_mix_norm": gain(ks[8], D_MODEL),
        "post_mix_norm": gain(ks[9], D_MODEL),
        "pre_ff_norm": gain(ks[10], D_MODEL),
        "post_ff_norm": gain(ks[11], D_MODEL),
        "ple_norm": gain(ks[12], D_MODEL),
        "ret_gn": gain(ks[13], RET_WIDTH),
    }


def reference(x, p, positions, w_in, w_out, w_ff1, w_ff2, w_ple, w_ple_gate,
              pre_mix_norm, post_mix_norm, pre_ff_norm, post_ff_norm, ple_norm, ret_gn):
    seq = x.shape[1]
    topk = min(TOPK_MAX, seq // 4)
    rope_attn = rope_tables(positions, ATTN_ROT_DIM, ROPE_THETA)
    rope_idx = rope_tables(positions, IDX_ROT_DIM, ROPE_THETA)
    rope_ret = rope_tables(positions, RET_KEY_DIM, RET_THETA)
    h = x
    for i in range(DEPTH):
        h = hybrid_layer(h, p[i], rope_attn, rope_idx, rope_ret,
                         w_in[i], w_out[i], w_ff1[i], w_ff2[i], w_ple[i], w_ple_gate[i],
                         pre_mix_norm[i], post_mix_norm[i], pre_ff_norm[i],
                         post_ff_norm[i], ple_norm[i], ret_gn[i], topk)
    return h
```

```python
import numpy as np
import ml_dtypes
from contextlib import ExitStack
import concourse.bass as bass
import concourse.mybir as mybir
from concourse.bass_utils import run_bass_kernel_spmd

F32 = mybir.dt.float32
BF16 = mybir.dt.bfloat16
I32 = mybir.dt.int32
AF = mybir.ActivationFunctionType
ALU = mybir.AluOpType
AX = mybir.AxisListType

D = 2048
S = 2048
DEPTH = 2
KC = 16
TT = 512
NB = S // 128
AQ, AK, AV, IQ, IK, IW, RQ, RK, RV, RG, INW = 0, 1024, 1152, 1280, 2304, 2368, 2384, 2896, 3408, 4432, 5456
IN_GROUPS = [
    [(AQ, 512, 0)], [(AQ + 512, 512, 0)],
    [(AK, 128, 0), (IK, 64, 128), (IK, 64, 192), (AV, 128, 256), (IW, 16, 384), (AV, 112, 400)],
    [(IQ, 512, 0)], [(IQ + 512, 512, 0)],
    [(RQ, 512, 0)], [(RK, 512, 0)], [(RV, 512, 0)], [(RV + 512, 512, 0)],
    [(RG, 512, 0)], [(RG + 512, 512, 0)],
]
NEG = -1.0e30
SEM_LIMIT = 60000


class Buf:
    __slots__ = ("w", "r")

    def __init__(self):
        self.w = None
        self.r = {}


def bufs(n):
    return [Buf() for _ in range(n)]


class FW:
    def __init__(self, nc, es):
        self.nc = nc
        self.es = es
        self.nsem = 0
        self.E = {}
        for name in ["tensor", "vector", "scalar", "gpsimd", "sync"]:
            self.E[name] = dict(h=getattr(nc, name), sem=self._sem(), count=0, seen={},
                                selfsync=(name != "tensor"), epoch=0, last=None)
        self.pool = {q: [dict(sem=self._sem(), val=0) for _ in range(n)]
                     for q, n in [("sync", 24), ("gpsimd", 40), ("scalar", 2)]}
        self.rr = {q: 0 for q in self.pool}
        self.log = {n: [] for n in self.E}

    def _sem(self):
        self.nsem += 1
        return self.es.enter_context(self.nc.semaphore(f"sm{self.nsem}"))

    def _deps(self, reads, writes):
        d = []
        for b in reads:
            if b.w is not None:
                d.append(b.w)
        for b in writes:
            if b.w is not None:
                d.append(b.w)
            d.extend(b.r.values())
        return d

    def _wait(self, ename, deps):
        E = self.E[ename]
        need = {}
        for (key, sem, val, owner) in deps:
            if owner == ename and not E["selfsync"]:
                continue
            if E["seen"].get(key, 0) >= val:
                continue
            if key not in need or need[key][1] < val:
                need[key] = (sem, val)
        for key, (sem, val) in need.items():
            E["h"].wait_ge(sem, val)
            self.log[ename].append(("wait", id(sem), val))
            E["seen"][key] = val

    def _record(self, tok, reads, writes):
        for b in reads:
            o = b.r.get(tok[0])
            if o is None or o[2] < tok[2]:
                b.r[tok[0]] = tok
        for b in writes:
            b.w = tok
            b.r = {}

    def op(self, ename, fn, reads=(), writes=(), signal=True):
        E = self.E[ename]
        self._wait(ename, self._deps(reads, writes))
        if E["count"] >= SEM_LIMIT:
            E["sem"] = self._sem()
            E["count"] = 0
            E["epoch"] += 1
        ins = fn(E["h"])
        key = (ename, E["epoch"])
        if signal:
            E["count"] += 1
            ins.then_inc(E["sem"], 1)
            self.log[ename].append(("inc", id(E["sem"]), 1))
            tok = (key, E["sem"], E["count"], ename)
            E["last"] = tok
        else:
            tok = (key, E["sem"], E["count"] + 1, ename)
        self._record(tok, reads, writes)
        return ins

    def dma(self, q, out, in_, reads=(), writes=(), **kw):
        E = self.E[q]
        pool = self.pool[q]
        i = self.rr[q]
        self.rr[q] = (i + 1) % len(pool)
        slot = pool[i]
        if slot["val"] + 16 > SEM_LIMIT:
            slot["sem"] = self._sem()
            slot["val"] = 0
        deps = self._deps(reads, writes)
        key = ("dma", id(slot["sem"]))
        if slot["val"] > 0:
            deps.append((key, slot["sem"], slot["val"], "dma"))
        self._wait(q, deps)
        ins = E["h"].dma_start(out=out, in_=in_, **kw)
        slot["val"] += 16
        ins.then_inc(slot["sem"], 16)
        self.log[q].append(("inc", id(slot["sem"]), 16))
        tok = (key, slot["sem"], slot["val"], "dma")
        self._record(tok, reads, writes)
        return ins

    def all_tokens(self):
        toks = []
        for n, E in self.E.items():
            if E["last"] is not None:
                toks.append(E["last"])
        for q, pool in self.pool.items():
            for s in pool:
                if s["val"] > 0:
                    toks.append((("dma", id(s["sem"])), s["sem"], s["val"], "dma"))
        return toks

    def barrier(self, engines=None):
        toks = self.all_tokens()
        for n in (engines or list(self.E.keys())):
            E = self.E[n]
            ss = E["selfsync"]
            E["selfsync"] = True
            self._wait(n, toks)
            E["selfsync"] = ss


def _consts():
    c = {}
    c["c_identf"] = np.eye(128, dtype=np.float32)
    c["c_identb"] = np.eye(128, dtype=np.float32).astype(ml_dtypes.bfloat16)
    c["c_irep"] = np.tile(np.eye(128, dtype=np.float32), (1, 4)).astype(ml_dtypes.bfloat16)
    pm = np.zeros((3, 128, 128), np.float32)
    invf = np.zeros((128, 3), np.float64)
    for i in range(16):
        pm[0, i + 16, i] = -1.0
        pm[0, i, i + 16] = 1.0
        invf[i, 0] = invf[i + 16, 0] = 500000.0 ** (-i / 16.0)
    for o in (0, 64):
        for i in range(8):
            pm[1, o + i + 8, o + i] = -1.0
            pm[1, o + i, o + i + 8] = 1.0
            invf[o + i, 1] = invf[o + i + 8, 1] = 500000.0 ** (-i / 8.0)
    for o in (0, 64):
        for i in range(32):
            pm[2, o + i + 32, o + i] = -1.0
            pm[2, o + i, o + i + 32] = 1.0
            invf[o + i, 2] = invf[o + i + 32, 2] = 10000.0 ** (-i / 32.0)
    c["c_pm"] = pm.astype(ml_dtypes.bfloat16)
    c["c_invf"] = (invf.astype(np.float32).astype(np.float64) / (2 * np.pi)).astype(np.float32)
    q = np.arange(128)[:, None]
    k = np.arange(128)[None, :]
    c["c_causb"] = np.where(k <= q, 0.0, -30000.0).astype(np.float32).astype(ml_dtypes.bfloat16)
    c["c_causf"] = np.where(k <= q, 0.0, NEG).astype(np.float32)
    H = 8
    C = 128
    gamma = (1.0 - 2.0 ** (-5.0 - np.arange(H, dtype=np.float32))).astype(np.float32)
    log_g = np.log(gamma).astype(np.float32)
    i = np.arange(C, dtype=np.float32)
    dt = np.zeros((128, H, 128), np.float32)
    for h in range(H):
        diff = i[None, :] - i[:, None]
        dt[:, h, :] = np.where(diff >= 0, np.exp(np.maximum(diff, 0.0) * log_g[h]), 0.0)
    c["c_dt"] = np.ascontiguousarray(dt.reshape(128, 4, 2, 128).transpose(0, 2, 1, 3)).reshape(128, 1024).astype(np.float32)
    zeta = np.exp((C - 1.0 - i)[None, :] * log_g[:, None]).astype(np.float32)
    xi = np.exp((i + 1.0)[None, :] * log_g[:, None]).astype(np.float32)
    gch = np.exp(C * log_g).astype(np.float32)
    xit = np.zeros((128, 4, 128), np.float32)
    gt = np.zeros((128, 4), np.float32)
    for cc in range(4):
        for hh in range(2):
            xit[hh * 64:(hh + 1) * 64, cc, :] = xi[2 * cc + hh][None, :]
            gt[hh * 64:(hh + 1) * 64, cc] = gch[2 * cc + hh]
    c["c_xi"] = xit.reshape(128, 512)
    c["c_gt"] = gt
    zt = np.zeros((128, H, 64), np.float32)
    for h in range(H):
        zt[:, h, :] = zeta[h][:, None]
    c["c_zt"] = zt.reshape(128, 512)
    return c


CONST_SPECS = {
    "c_identf": ([128, 128], F32), "c_identb": ([128, 128], BF16), "c_irep": ([128, 512], BF16),
    "c_pm": ([3, 128, 128], BF16), "c_invf": ([128, 3], F32), "c_causb": ([128, 128], BF16),
    "c_causf": ([128, 128], F32), "c_dt": ([128, 1024], F32), "c_xi": ([128, 512], F32),
    "c_gt": ([128, 4], F32), "c_zt": ([128, 512], F32),
}
GAIN_NAMES = ["g_premix", "g_postmix", "g_preff", "g_postff", "g_ple"]


def build(nseq=2, dbg=False, stop_after=None, nlayers=DEPTH, only_seq=False, nblk=NB, parts=("idx", "attn", "ret"), retlvl=9):
    NT = nseq * S
    NTILE = NT // TT
    nc = bass.Bass("TRN2", target_bir_lowering=False)
    kin = "ExternalInput"
    ksc = "ExternalOutput" if dbg else "Internal"

    def dram(name, shape, dt, kind):
        return nc.dram_tensor(name, shape, dt, kind=kind).ap()

    kin_ = kin
    if only_seq:
        kin = "Internal"
    x_d = dram("x", [NT, D], F32, kin)
    p_d = dram("p", [DEPTH, NT, 256], F32, kin)
    pos_d = dram("pos", [1, NT], I32, kin)
    w_in_d = dram("w_in", [DEPTH, D, INW], F32, kin)
    w_out_d = dram("w_out", [DEPTH, D, D], F32, kin)
    w_ff1_d = dram("w_ff1", [DEPTH, D, 4 * D], F32, kin)
    w_ff2_d = dram("w_ff2", [DEPTH, 4 * D, D], F32, kin)
    w_ple_d = dram("w_ple", [DEPTH, 256, D], F32, kin)
    w_gate_d = dram("w_gate", [DEPTH, D, D], F32, kin)
    kin = kin_
    gains_d = {n: dram(n, [DEPTH, 128, 16], F32, kin) for n in GAIN_NAMES}
    gn_d = dram("g_gn", [DEPTH, 128, 8], F32, kin)
    cd = {n: dram(n, sh, dt, kin) for n, (sh, dt) in CONST_SPECS.items()}
    out_d = dram("out", [NT, D], F32, "ExternalOutput")

    wb_in = dram("wb_in", [DEPTH, 11, 128, 8192], BF16, "Internal")
    wb_out = dram("wb_out", [DEPTH, 4, 128, 8192], BF16, "Internal")
    wb_ff1 = dram("wb_ff1", [DEPTH, 16, 128, 8192], BF16, "Internal")
    wb_ff2 = dram("wb_ff2", [DEPTH, 16, 128, 8192], BF16, "Internal")
    wb_gate = dram("wb_gate", [DEPTH, 4, 128, 8192], BF16, "Internal")
    wb_ple = dram("wb_ple", [DEPTH, 4, 128, 1024], BF16, "Internal")
    ksi = kin if only_seq else ksc
    hT_d = dram("hT", [D, NT], F32, ksc)
    tabs_d = dram("tabs", [6, 128, NT], F32, ksc)
    qT_d = dram("qT", [1024, NT], BF16, ksi)
    kT_d = dram("kT", [128, NT], BF16, ksi)
    ikT_d = dram("ikT", [128, NT], BF16, ksi)
    iqT_d = dram("iqT", [1024, NT], BF16, ksi)
    rqT_d = dram("rqT", [512, NT], BF16, ksi)
    rkT_d = dram("rkT", [512, NT], BF16, ksi)
    rgT_d = dram("rgT", [1024, NT], BF16, ksi)
    v_d = dram("v", [NT, 128], BF16, ksi)
    iw_d = dram("iw", [NT, 16], F32, ksi)
    rk_d = dram("rk", [NT, 512], BF16, ksi)
    rv_d = dram("rv", [NT, 1024], BF16, ksi)
    mixT_d = dram("mixT", [D, NT], BF16, ksc)

    with ExitStack() as es:
        fw = FW(nc, es)
        nc._fw = fw

        uid = [0]

        def sb(ctx, name, shape, dt):
            uid[0] += 1
            return ctx.enter_context(nc.sbuf_tensor(f"s{uid[0]}_{name}", shape, dt))

        def pst(ctx, name, shape, dt):
            uid[0] += 1
            return ctx.enter_context(nc.psum_tensor(f"p{uid[0]}_{name}", shape, dt))

        identf = sb(es, "identf", [128, 128], F32)
        identb = sb(es, "identb", [128, 128], BF16)
        onesb = sb(es, "onesb", [128, 128], BF16)
        gains = {n: sb(es, "t_" + n, [128, DEPTH * 16], F32) for n in GAIN_NAMES}
        gn_t = sb(es, "t_gn", [128, DEPTH * 8], F32)
        cbuf = Buf()
        fw.dma("sync", identf[:], cd["c_identf"], writes=[cbuf])
        fw.dma("sync", identb[:], cd["c_identb"], writes=[cbuf])
        for n in GAIN_NAMES:
            for l in range(DEPTH):
                fw.dma("sync", gains[n][:, l * 16:(l + 1) * 16], gains_d[n][l], writes=[cbuf])
        for l in range(DEPTH):
            fw.dma("sync", gn_t[:, l * 8:(l + 1) * 8], gn_d[l], writes=[cbuf])
        fw.op("vector", lambda e: e.memset(onesb[:], 1.0), writes=[cbuf])

        wbuf = {}

        def conv(key, dst, src):
            b = wbuf.setdefault(key, Buf())
            fw.dma("gpsimd", dst, src, writes=[b])

        def conv_layer(l):
            for g, pieces in enumerate(IN_GROUPS):
                for (s0, wd, d0) in pieces:
                    dst = wb_in[l, g].rearrange("p (k c) -> p k c", c=512)[:, :, d0:d0 + wd]
                    src = w_in_d[l][:, s0:s0 + wd].rearrange("(k p) c -> p k c", p=128)
                    conv(("in", l, g), dst, src)
            for g in range(4):
                dst = wb_out[l, g].rearrange("p (k c) -> p k c", c=512)
                src = w_out_d[l][:, g * 512:(g + 1) * 512].rearrange("(k p) c -> p k c", p=128)
                conv(("out", l, g), dst, src)
            for g in range(16):
                dst = wb_ff1[l, g].rearrange("p (k c) -> p k c", c=512)
                src = w_ff1_d[l][:, g * 512:(g + 1) * 512].rearrange("(k p) c -> p k c", p=128)
                conv(("ff1", l, g), dst, src)
            for q in range(4):
                for og in range(4):
                    dst = wb_ff2[l, q * 4 + og].rearrange("p (k c) -> p k c", c=512)
                    src = w_ff2_d[l][q * 2048:(q + 1) * 2048, og * 512:(og + 1) * 512].rearrange(
                        "(k p) c -> p k c", p=128)
                    conv(("ff2", l, q * 4 + og), dst, src)
            for g in range(4):
                dst = wb_ple[l, g].rearrange("p (k c) -> p k c", c=512)
                src = w_ple_d[l][:, g * 512:(g + 1) * 512].rearrange("(k p) c -> p k c", p=128)
                conv(("ple", l, g), dst, src)
                dst = wb_gate[l, g].rearrange("p (k c) -> p k c", c=512)
                src = w_gate_d[l][:, g * 512:(g + 1) * 512].rearrange("(k p) c -> p k c", p=128)
                conv(("gate", l, g), dst, src)

        if not only_seq:
            conv_layer(0)

        def tables_phase():
            with ExitStack() as ph:
                invf = sb(ph, "invf", [128, 3], F32)
                posi = sb(ph, "posi", [128, S], I32)
                posf = sb(ph, "posf", [128, S], F32)
                ys = sb(ph, "ys", [128, S], F32)
                ki = sb(ph, "ki", [128, S], I32)
                kf = sb(ph, "kf", [128, S], F32)
                fr = sb(ph, "fr", [128, S], F32)
                tb = [sb(ph, f"tb{i}", [128, S], F32) for i in range(2)]
                b_invf, b_posi, b_posf, b_ys, b_ki, b_kf, b_fr = bufs(7)
                b_tb = bufs(2)
                fw.dma("sync", invf[:], cd["c_invf"], writes=[b_invf])
                n = 0
                for s in range(nseq):
                    fw.dma("sync", posi[:], pos_d[0:1, s * S:(s + 1) * S].partition_broadcast(128),
                           writes=[b_posi])
                    fw.op("vector", lambda e: e.tensor_copy(out=posf[:], in_=posi[:]),
                          reads=[b_posi], writes=[b_posf])
                    for t in range(3):
                        for cs in range(2):
                            if cs == 0:
                                fw.op("vector", lambda e, t=t: e.tensor_scalar(
                                    out=ys[:], in0=posf[:], scalar1=invf[:, t:t + 1], scalar2=0.25,
                                    op0=ALU.mult, op1=ALU.add), reads=[b_posf, b_invf], writes=[b_ys])
                            else:
                                fw.op("vector", lambda e, t=t: e.tensor_scalar(
                                    out=ys[:], in0=posf[:], scalar1=invf[:, t:t + 1], scalar2=None,
                                    op0=ALU.mult), reads=[b_posf, b_invf], writes=[b_ys])
                            fw.op("vector", lambda e: e.tensor_copy(out=ki[:], in_=ys[:]),
                                  reads=[b_ys], writes=[b_ki])
                            fw.op("vector", lambda e: e.tensor_copy(out=kf[:], in_=ki[:]),
                                  reads=[b_ki], writes=[b_kf])
                            fw.op("vector", lambda e: e.tensor_tensor(out=fr[:], in0=ys[:], in1=kf[:],
                                                                      op=ALU.subtract),
                                  reads=[b_ys, b_kf], writes=[b_fr])
                            fw.op("vector", lambda e: e.tensor_scalar(out=kf[:], in0=fr[:], scalar1=0.0,
                                                                      scalar2=None, op0=ALU.is_lt),
                                  reads=[b_fr], writes=[b_kf])
                            fw.op("vector", lambda e: e.tensor_tensor(out=fr[:], in0=fr[:], in1=kf[:],
                                                                      op=ALU.add),
                                  reads=[b_fr, b_kf], writes=[b_fr])
                            tt = tb[n % 2]
                            bt = b_tb[n % 2]
                            n += 1
                            fw.op("scalar", lambda e, tt=tt: e.activation(
                                out=tt[:], in_=fr[:], func=AF.Sin, scale=-2.0 * np.pi, bias=pib[:, 0:1]),
                                reads=[b_fr, cbuf], writes=[bt])
                            fw.dma("sync", tabs_d[2 * t + cs][:, s * S:(s + 1) * S], tt[:], reads=[bt])
                fw.barrier()

        pib = sb(es, "pib", [128, 2], F32)
        fw.op("vector", lambda e: e.memset(pib[:, 0:1], float(np.pi)), writes=[cbuf])
        fw.op("vector", lambda e: e.memset(pib[:, 1:2], 1e-6), writes=[cbuf])
        epsb = sb(es, "epsb", [128, 1], F32)
        fw.op("vector", lambda e: e.memset(epsb[:], 1e-5), writes=[cbuf])
        if only_seq:
            seq_phase_holder = []
        else:
            tables_phase()
        if stop_after == "tables":
            fw.barrier()
            return nc

        hbuf = bufs(NTILE)

        def dense_phase(l_prev, l_next):
            with ExitStack() as ph:
                hT = sb(ph, "hT", [128, 16 * TT], F32)
                yT = sb(ph, "yT", [128, 16 * TT], F32)
                act = sb(ph, "act", [128, 16 * TT], BF16)
                uT = sb(ph, "uT", [128, 16 * TT], BF16)
                NW = 3
                Wt = [sb(ph, f"Wt{i}", [128, 8192], BF16) for i in range(NW)]
                Wp = [sb(ph, f"Wp{i}", [128, 1024], BF16) for i in range(2)]
                tabs = sb(ph, "tabs_t", [128, 6 * TT], F32)
                rstd = sb(ph, "rstd", [128, TT], F32)
                sqr = [sb(ph, f"sqr{i}", [128, TT], BF16) for i in range(2)]
                tmp = [sb(ph, f"tmp{i}", [128, TT], F32) for i in range(4)]
                qb = [sb(ph, f"qb{i}", [128, TT], BF16) for i in range(2)]
                stg = [sb(ph, f"stg{i}", [128, TT], BF16) for i in range(3)]
                tok = [sb(ph, f"tok{i}", [128, 512], BF16) for i in range(3)]
                iwst = sb(ph, "iwst", [128, 64], F32)
                pin = sb(ph, "pin", [128, 1024], F32)
                pbt = sb(ph, "pbt", [128, 1024], BF16)
                pT = sb(ph, "pT", [128, 1024], BF16)
                pm = sb(ph, "pm", [128, 3 * 128], BF16)
                acc = [pst(ph, f"acc{i}", [128, 512], F32) for i in range(4)]
                ssb = pst(ph, "ssb", [128, 512], F32)
                ppb = [pst(ph, f"ppb{i}", [128, 512], F32) for i in range(2)]
                trb = pst(ph, "trb", [128, 512], F32)
                trb16 = trb[:].bitcast(BF16)

                b_hT, b_yT, b_act = bufs(16), bufs(16), bufs(16)
                b_uT = bufs(16)
                b_W, b_Wp = bufs(NW), bufs(2)
                b_tabs, b_rstd, b_ss, b_trb, b_pin, b_pbt, b_pT, b_pm, b_iwst = bufs(9)
                b_sqr, b_tmp, b_qb, b_stg, b_tok = bufs(2), bufs(4), bufs(2), bufs(3), bufs(3)
                b_acc, b_pp = bufs(4), bufs(2)
                ctr = dict(w=0, acc=0, sqr=0, tmp=0, qb=0, stg=0, tok=0, pp=0, wp=0)

                def nxt(k, n):
                    i = ctr[k] % n
                    ctr[k] += 1
                    return i

                for t in range(3):
                    fw.dma("sync", pm[:, t * 128:(t + 1) * 128], cd["c_pm"][t], writes=[b_pm])

                def hc(c):
                    return hT[:, c * TT:(c + 1) * TT]

                def yc(c):
                    return yT[:, c * TT:(c + 1) * TT]

                def ac(c):
                    return act[:, c * TT:(c + 1) * TT]

                def uc(c):
                    return uT[:, c * TT:(c + 1) * TT]

                plan = []
                for ti_p in range(NTILE):
                    if l_prev is not None:
                        lp = l_prev
                        plan += [(wb_out[lp, og], ("out", lp, og)) for og in range(4)]
                        for q in range(4):
                            plan += [(wb_ff1[lp, q * 4 + g1], ("ff1", lp, q * 4 + g1)) for g1 in range(4)]
                            plan += [(wb_ff2[lp, q * 4 + og], ("ff2", lp, q * 4 + og)) for og in range(4)]
                        plan += [(wb_gate[lp, og], ("gate", lp, og)) for og in range(4)]
                    if l_next is not None:
                        plan += [(wb_in[l_next, g], ("in", l_next, g)) for g in range(11)]
                wstate = dict(issue=0, use=0)

                def wload(src, key):
                    k = wstate["use"]
                    assert plan[k][1] == key, (plan[k][1], key)
                    while wstate["issue"] < min(len(plan), k + NW):
                        ki = wstate["issue"]
                        fw.dma("sync", Wt[ki % NW][:], plan[ki][0], reads=[wbuf[plan[ki][1]]],
                               writes=[b_W[ki % NW]])
                        wstate["issue"] += 1
                    wstate["use"] += 1
                    return Wt[k % NW], b_W[k % NW]

                def gemm_f(W, bW, j, rhs_fn, rhs_bufs, nk):
                    i = nxt("acc", 4)
                    for k in range(nk):
                        fw.op("tensor", lambda e, k=k: e.matmul(
                            acc[i][:], lhsT=W[:, k * 512 + j * 128:k * 512 + (j + 1) * 128], rhs=rhs_fn(k),
                            start=(k == 0), stop=(k == nk - 1)),
                            reads=[bW] + rhs_bufs, writes=[b_acc[i]], signal=(k == nk - 1))
                    return acc[i], b_acc[i]

                def rstd_from(src_fn, src_bufs):
                    for c in range(16):
                        i = nxt("sqr", 2)
                        fw.op("scalar", lambda e, c=c, i=i: e.activation(out=sqr[i][:], in_=src_fn(c),
                                                                         func=AF.Square),
                              reads=[src_bufs[c]], writes=[b_sqr[i]])
                        fw.op("tensor", lambda e, c=c, i=i: e.matmul(ssb[:], lhsT=onesb[:], rhs=sqr[i][:],
                                                                     start=(c == 0), stop=(c == 15)),
                              reads=[b_sqr[i], cbuf], writes=[b_ss], signal=True)
                    fw.op("scalar", lambda e: e.activation(out=rstd[:], in_=ssb[:], func=AF.Sqrt,
                                                           scale=1.0 / D, bias=pib[:, 1:2]),
                          reads=[b_ss, cbuf], writes=[b_rstd])
                    fw.op("vector", lambda e: e.reciprocal(out=rstd[:], in_=rstd[:]),
                          reads=[b_rstd], writes=[b_rstd])

                def norm_to_act(gname, l):
                    rstd_from(hc, b_hT)
                    g = gains[gname]
                    for c in range(16):
                        fw.op("vector", lambda e, c=c: e.scalar_tensor_tensor(
                            out=ac(c), in0=hc(c), scalar=g[:, l * 16 + c:l * 16 + c + 1], in1=rstd[:],
                            op0=ALU.mult, op1=ALU.mult),
                            reads=[b_hT[c], b_rstd, cbuf], writes=[b_act[c]])

                def norm_add(gname, l):
                    rstd_from(yc, b_yT)
                    g = gains[gname]
                    for c in range(16):
                        i = nxt("tmp", 4)
                        fw.op("vector", lambda e, c=c, i=i: e.scalar_tensor_tensor(
                            out=tmp[i][:], in0=yc(c), scalar=g[:, l * 16 + c:l * 16 + c + 1], in1=rstd[:],
                            op0=ALU.mult, op1=ALU.mult),
                            reads=[b_yT[c], b_rstd, cbuf], writes=[b_tmp[i]])
                        fw.op("gpsimd", lambda e, c=c, i=i: e.tensor_tensor(out=hc(c), in0=hc(c), in1=tmp[i][:],
                                                                            op=ALU.add),
                              reads=[b_tmp[i], b_hT[c]], writes=[b_hT[c]])

                def p1(l, ti):
                    tsl = slice(ti * TT, (ti + 1) * TT)
                    fw.dma("sync", tabs[:].rearrange("p (a t) -> p a t", t=TT),
                           tabs_d[:, :, tsl].rearrange("a p t -> p a t"), writes=[b_tabs])
                    norm_to_act("g_premix", l)

                    def rope_epi(a, ba, typ, scale, dst):
                        i = nxt("qb", 2)
                        fw.op("scalar", lambda e: e.activation(out=qb[i][:], in_=a[:], func=AF.Copy,
                                                               scale=float(scale)),
                              reads=[ba], writes=[b_qb[i]])
                        ip = nxt("pp", 2)
                        fw.op("tensor", lambda e: e.matmul(ppb[ip][:], lhsT=pm[:, typ * 128:(typ + 1) * 128],
                                                           rhs=qb[i][:], start=True, stop=True),
                              reads=[b_qb[i], b_pm], writes=[b_pp[ip]])
                        i1 = nxt("tmp", 4)
                        i2 = nxt("tmp", 4)
                        Ct = tabs[:, (2 * typ) * TT:(2 * typ + 1) * TT]
                        St = tabs[:, (2 * typ + 1) * TT:(2 * typ + 2) * TT]
                        fw.op("gpsimd", lambda e: e.tensor_tensor(out=tmp[i1][:], in0=qb[i][:], in1=Ct,
                                                                  op=ALU.mult),
                              reads=[b_qb[i], b_tabs], writes=[b_tmp[i1]])
                        fw.op("vector", lambda e: e.tensor_tensor(out=tmp[i2][:], in0=ppb[ip][:], in1=St,
                                                                  op=ALU.mult),
                              reads=[b_pp[ip], b_tabs], writes=[b_tmp[i2]])
                        si = nxt("stg", 3)
                        fw.op("gpsimd", lambda e: e.tensor_tensor(out=stg[si][:], in0=tmp[i1][:],
                                                                  in1=tmp[i2][:], op=ALU.add),
                              reads=[b_tmp[i1], b_tmp[i2]], writes=[b_stg[si]])
                        fw.dma("sync", dst, stg[si][:], reads=[b_stg[si]])
                        return si

                    rhs_act = lambda k: ac(k)
                    for g in range(11):
                        W, bW = wload(wb_in[l, g], ("in", l, g))
                        if g in (0, 1):
                            for j in range(4):
                                h = g * 4 + j
                                a, ba = gemm_f(W, bW, j, rhs_act, b_act, 16)
                                rope_epi(a, ba, 0, 128.0 ** -0.5, qT_d[h * 128:(h + 1) * 128, tsl])
                        elif g == 2:
                            a, ba = gemm_f(W, bW, 0, rhs_act, b_act, 16)
                            rope_epi(a, ba, 0, 1.0, kT_d[:, tsl])
                            a, ba = gemm_f(W, bW, 1, rhs_act, b_act, 16)
                            rope_epi(a, ba, 1, 1.0, ikT_d[:, tsl])
                            ti_ = nxt("tok", 3)
                            for tb in range(4):
                                i = nxt("acc", 4)
                                for k in range(16):
                                    fw.op("tensor", lambda e, k=k, tb=tb, i=i: e.matmul(
                                        acc[i][:, 0:144], lhsT=act[:, k * TT + tb * 128:k * TT + (tb + 1) * 128],
                                        rhs=W[:, k * 512 + 256:k * 512 + 400], start=(k == 0), stop=(k == 15)),
                                        reads=[bW] + b_act, writes=[b_acc[i]], signal=(k == 15))
                                fw.op("scalar", lambda e, tb=tb, i=i: e.activation(
                                    out=tok[ti_][:, tb * 128:(tb + 1) * 128], in_=acc[i][:, 0:128], func=AF.Copy),
                                    reads=[b_acc[i]], writes=[b_tok[ti_]])
                                fw.op("scalar", lambda e, tb=tb, i=i: e.activation(
                                    out=iwst[:, tb * 16:(tb + 1) * 16], in_=acc[i][:, 128:144], func=AF.Copy,
                                    scale=1.0 / 32.0),
                                    reads=[b_acc[i]], writes=[b_iwst])
                            fw.dma("sync", v_d[tsl, :].rearrange("(b p) f -> p b f", p=128),
                                   tok[ti_][:].rearrange("p (b f) -> p b f", f=128), reads=[b_tok[ti_]])
                            fw.dma("sync", iw_d[tsl, :].rearrange("(b p) f -> p b f", p=128),
                                   iwst[:].rearrange("p (b f) -> p b f", f=16), reads=[b_iwst])
                        elif g in (3, 4):
                            for j in range(4):
                                pr = (g - 3) * 4 + j
                                a, ba = gemm_f(W, bW, j, rhs_act, b_act, 16)
                                rope_epi(a, ba, 1, 1.0, iqT_d[pr * 128:(pr + 1) * 128, tsl])
                        elif g == 5:
                            for j in range(4):
                                a, ba = gemm_f(W, bW, j, rhs_act, b_act, 16)
                                rope_epi(a, ba, 2, 1.0, rqT_d[j * 128:(j + 1) * 128, tsl])
                        elif g == 6:
                            for j in range(4):
                                a, ba = gemm_f(W, bW, j, rhs_act, b_act, 16)
                                si = rope_epi(a, ba, 2, 0.125, rkT_d[j * 128:(j + 1) * 128, tsl])
                                for tb in range(4):
                                    fw.op("tensor", lambda e, tb=tb: e.transpose(
                                        out=trb16[:, tb * 128:(tb + 1) * 128],
                                        in_=stg[si][:, tb * 128:(tb + 1) * 128], identity=identb[:]),
                                        reads=[b_stg[si], cbuf], writes=[b_trb], signal=(tb == 3))
                                ti_ = nxt("tok", 3)
                                fw.op("scalar", lambda e: e.activation(out=tok[ti_][:], in_=trb16[:, 0:512],
                                                                       func=AF.Copy),
                                      reads=[b_trb], writes=[b_tok[ti_]])
                                fw.dma("sync",
                                       rk_d[tsl, j * 128:(j + 1) * 128].rearrange("(b p) f -> p b f", p=128),
                                       tok[ti_][:].rearrange("p (b f) -> p b f", f=128), reads=[b_tok[ti_]])
                        elif g in (7, 8):
                            for tb in range(4):
                                i = nxt("acc", 4)
                                for k in range(16):
                                    fw.op("tensor", lambda e, k=k, tb=tb, i=i: e.matmul(
                                        acc[i][:], lhsT=act[:, k * TT + tb * 128:k * TT + (tb + 1) * 128],
                                        rhs=W[:, k * 512:(k + 1) * 512], start=(k == 0), stop=(k == 15)),
                                        reads=[bW] + b_act, writes=[b_acc[i]], signal=(k == 15))
                                ti_ = nxt("tok", 3)
                                fw.op("scalar", lambda e, i=i: e.activation(out=tok[ti_][:], in_=acc[i][:],
                                                                            func=AF.Copy),
                                      reads=[b_acc[i]], writes=[b_tok[ti_]])
                                r0 = ti * TT + tb * 128
                                fw.dma("sync", rv_d[r0:r0 + 128, (g - 7) * 512:(g - 6) * 512], tok[ti_][:],
                                       reads=[b_tok[ti_]])
                        else:
                            for j in range(4):
                                hh = (g - 9) * 4 + j
                                a, ba = gemm_f(W, bW, j, rhs_act, b_act, 16)
                                si = nxt("stg", 3)
                                fw.op("scalar", lambda e: e.activation(out=stg[si][:], in_=a[:], func=AF.Silu),
                                      reads=[ba], writes=[b_stg[si]])
                                fw.dma("sync", rgT_d[hh * 128:(hh + 1) * 128, tsl], stg[si][:], reads=[b_stg[si]])

                def p3(l, ti):
                    tsl = slice(ti * TT, (ti + 1) * TT)
                    fw.dma("sync", act[:].rearrange("p (c t) -> p c t", t=TT),
                           mixT_d[:, tsl].rearrange("(c p) t -> p c t", p=128), writes=b_act)
                    rhs_act = lambda k: ac(k)
                    for og in range(4):
                        W, bW = wload(wb_out[l, og], ("out", l, og))
                        for j in range(4):
                            oc = og * 4 + j
                            a, ba = gemm_f(W, bW, j, rhs_act, b_act, 16)
                            fw.op("scalar", lambda e, oc=oc: e.activation(out=yc(oc), in_=a[:], func=AF.Copy),
                                  reads=[ba], writes=[b_yT[oc]])
                    norm_add("g_postmix", l)
                    norm_to_act("g_preff", l)
                    for q in range(4):
                        for g1 in range(4):
                            W, bW = wload(wb_ff1[l, q * 4 + g1], ("ff1", l, q * 4 + g1))
                            for j in range(4):
                                ucx = g1 * 4 + j
                                a, ba = gemm_f(W, bW, j, rhs_act, b_act, 16)
                                i = nxt("tmp", 4)
                                fw.op("scalar", lambda e, i=i: e.activation(out=tmp[i][:], in_=a[:], func=AF.Relu),
                                      reads=[ba], writes=[b_tmp[i]])
                                fw.op("gpsimd", lambda e, i=i, ucx=ucx: e.tensor_tensor(
                                    out=uc(ucx), in0=tmp[i][:], in1=tmp[i][:], op=ALU.mult),
                                    reads=[b_tmp[i]], writes=[b_uT[ucx]])
                        for og in range(4):
                            W, bW = wload(wb_ff2[l, q * 4 + og], ("ff2", l, q * 4 + og))
                            for j in range(4):
                                oc = og * 4 + j
                                a, ba = gemm_f(W, bW, j, lambda k: uc(k), b_uT, 16)
                                if q == 0:
                                    fw.op("scalar", lambda e, oc=oc: e.activation(out=yc(oc), in_=a[:],
                                                                                  func=AF.Copy),
                                          reads=[ba], writes=[b_yT[oc]])
                                else:
                                    fw.op("vector", lambda e, oc=oc: e.tensor_tensor(out=yc(oc), in0=yc(oc),
                                                                                     in1=a[:], op=ALU.add),
                                          reads=[ba, b_yT[oc]], writes=[b_yT[oc]])
                    norm_add("g_postff", l)
                    fw.dma("sync", pin[:].rearrange("p (b f) -> p b f", f=256),
                           p_d[l][tsl, :].rearrange("(b p) f -> p b f", p=128), writes=[b_pin])
                    fw.op("vector", lambda e: e.tensor_copy(out=pbt[:], in_=pin[:]), reads=[b_pin], writes=[b_pbt])
                    for k2 in range(2):
                        for tb in range(4):
                            fw.op("tensor", lambda e, k2=k2, tb=tb: e.transpose(
                                out=trb16[:, tb * 128:(tb + 1) * 128],
                                in_=pbt[:, tb * 256 + k2 * 128:tb * 256 + (k2 + 1) * 128], identity=identb[:]),
                                reads=[b_pbt, cbuf], writes=[b_trb], signal=(tb == 3))
                        fw.op("scalar", lambda e, k2=k2: e.activation(out=pT[:, k2 * 512:(k2 + 1) * 512],
                                                                      in_=trb16[:, 0:512], func=AF.Copy),
                              reads=[b_trb], writes=[b_pT])
                    for c in range(16):
                        fw.op("gpsimd", lambda e, c=c: e.tensor_copy(out=ac(c), in_=hc(c)),
                              reads=[b_hT[c]], writes=[b_act[c]])
                    for og in range(4):
                        W, bW = wload(wb_gate[l, og], ("gate", l, og))
                        ip = nxt("wp", 2)
                        fw.dma("sync", Wp[ip][:], wb_ple[l, og], reads=[wbuf[("ple", l, og)]], writes=[b_Wp[ip]])
                        for j in range(4):
                            oc = og * 4 + j
                            a, ba = gemm_f(W, bW, j, rhs_act, b_act, 16)
                            a2, ba2 = gemm_f(Wp[ip], b_Wp[ip], j, lambda k: pT[:, k * 512:(k + 1) * 512], [b_pT], 2)
                            i = nxt("tmp", 4)
                            fw.op("scalar", lambda e, i=i: e.activation(out=tmp[i][:], in_=a[:], func=AF.Sigmoid),
                                  reads=[ba], writes=[b_tmp[i]])
                            fw.op("vector", lambda e, i=i, oc=oc: e.tensor_tensor(out=yc(oc), in0=tmp[i][:],
                                                                                  in1=a2[:], op=ALU.mult),
                                  reads=[b_tmp[i], ba2], writes=[b_yT[oc]])
                    norm_add("g_ple", l)

                for ti in range(NTILE):
                    tsl = slice(ti * TT, (ti + 1) * TT)
                    if l_prev is None:
                        for tb in range(4):
                            r0 = ti * TT + tb * 128
                            fw.dma("sync", yT[:, tb * 2048:(tb + 1) * 2048], x_d[r0:r0 + 128, :],
                                   writes=b_yT[tb * 4:(tb + 1) * 4])
                        for c in range(16):
                            for tb in range(4):
                                fw.op("tensor", lambda e, c=c, tb=tb: e.transpose(
                                    out=trb[:, tb * 128:(tb + 1) * 128],
                                    in_=yT[:, tb * 2048 + c * 128:tb * 2048 + (c + 1) * 128], identity=identf[:]),
                                    reads=b_yT[tb * 4:(tb + 1) * 4] + [cbuf], writes=[b_trb], signal=(tb == 3))
                            fw.op("scalar", lambda e, c=c: e.activation(out=hc(c), in_=trb[:], func=AF.Copy),
                                  reads=[b_trb], writes=[b_hT[c]])
                    else:
                        fw.dma("sync", hT[:].rearrange("p (c t) -> p c t", t=TT),
                               hT_d[:, tsl].rearrange("(c p) t -> p c t", p=128),
                               reads=[hbuf[ti]], writes=b_hT)
                        p3(l_prev, ti)
                    if l_next is not None:
                        fw.dma("sync", hT_d[:, tsl].rearrange("(c p) t -> p c t", p=128),
                               hT[:].rearrange("p (c t) -> p c t", t=TT), reads=b_hT, writes=[hbuf[ti]])
                        p1(l_next, ti)
                    else:
                        for tb in range(4):
                            for c4 in range(4):
                                for cc in range(4):
                                    c = c4 * 4 + cc
                                    fw.op("tensor", lambda e, c=c, cc=cc, tb=tb: e.transpose(
                                        out=trb[:, cc * 128:(cc + 1) * 128],
                                        in_=hT[:, c * TT + tb * 128:c * TT + (tb + 1) * 128], identity=identf[:]),
                                        reads=[b_hT[c], cbuf], writes=[b_trb], signal=(cc == 3))
                                fw.op("scalar", lambda e, c4=c4, tb=tb: e.activation(
                                    out=yT[:, tb * 2048 + c4 * 512:tb * 2048 + (c4 + 1) * 512], in_=trb[:],
                                    func=AF.Copy), reads=[b_trb], writes=[b_yT[tb * 4 + c4]])
                            r0 = ti * TT + tb * 128
                            fw.dma("sync", out_d[r0:r0 + 128, :], yT[:, tb * 2048:(tb + 1) * 2048],
                                   reads=b_yT[tb * 4:(tb + 1) * 4])
                fw.barrier()

        def seq_phase(l):
            with ExitStack() as ph:
                kTs = sb(ph, "kTs", [128, S], BF16)
                ikTs = sb(ph, "ikTs", [128, S], BF16)
                Vs = sb(ph, "Vs", [128, S], BF16)
                NBUF = 2
                qTb = [sb(ph, f"qTb{i}", [128, 1024], BF16) for i in range(NBUF)]
                iqTb = [sb(ph, f"iqTb{i}", [128, 1024], BF16) for i in range(NBUF)]
                iwb = [sb(ph, f"iwb{i}", [128, 16], F32) for i in range(NBUF)]
                rqTb = [sb(ph, f"rqTb{i}", [128, 512], BF16) for i in range(NBUF)]
                rkTb = [sb(ph, f"rkTb{i}", [128, 512], BF16) for i in range(NBUF)]
                rkb = [sb(ph, f"rkb{i}", [128, 512], BF16) for i in range(NBUF)]
                rvb = [sb(ph, f"rvb{i}", [128, 1024], BF16) for i in range(NBUF)]
                rgTb = [sb(ph, f"rgTb{i}", [128, 1024], BF16) for i in range(NBUF)]
                sacc = sb(ph, "sacc", [128, S], F32)
                work = sb(ph, "work", [128, S], F32)
                mb = sb(ph, "mb", [128, S], BF16)
                mx = sb(ph, "mx", [128, 8], F32)
                rr = [sb(ph, f"rr{i}", [128, 512], F32) for i in range(3)]
                ET = [sb(ph, f"ET{i}", [128, 1024], BF16) for i in range(2)]
                rec = sb(ph, "rec", [128, 1024], F32)
                ast = sb(ph, "ast", [128, 1024], BF16)
                Aall = sb(ph, "Aall", [128, 1024], BF16)
                rqxi = sb(ph, "rqxi", [128, 512], BF16)
                rkz = sb(ph, "rkz", [128, 512], BF16)
                Rst = sb(ph, "Rst", [128, 512], F32)
                Rb = sb(ph, "Rb", [128, 512], BF16)
                ocp = sb(ph, "ocp", [128, 1024], F32)
                osq = sb(ph, "osq", [128, 1024], F32)
                st = sb(ph, "st", [128, 64], F32)
                yb = sb(ph, "yb", [128, 1024], BF16)
                rst = sb(ph, "rst", [128, 1024], BF16)
                irep = sb(ph, "irep", [128, 512], BF16)
                causb = sb(ph, "causb", [128, 128], BF16)
                causf = sb(ph, "causf", [128, 128], F32)
                dtt = sb(ph, "dtt", [128, 1024], F32)
                xit = sb(ph, "xit", [128, 512], F32)
                ztt = sb(ph, "ztt", [128, 512], F32)
                gtt = sb(ph, "gtt", [128, 4], F32)
                pA = pst(ph, "pA", [128, 1024], F32)
                pS = pst(ph, "pS", [128, 1024], F32)
                pO = pst(ph, "pO", [128, 1024], F32)
                pD = pst(ph, "pD", [128, 1024], F32)
                pD16 = pD[:].bitcast(BF16)

                b_c = Buf()
                for tdst, nm in [(irep, "c_irep"), (causb, "c_causb"), (causf, "c_causf"), (dtt, "c_dt"),
                                 (xit, "c_xi"), (ztt, "c_zt"), (gtt, "c_gt")]:
                    fw.dma("sync", tdst[:], cd[nm], writes=[b_c])
                b_kTs, b_ikTs, b_Vs = bufs(3)
                b_blk = bufs(NBUF)
                b_sacc, b_work, b_mb, b_mx, b_rec, b_ast, b_Aall, b_rqxi, b_rkz, b_R, b_Rb = bufs(11)
                b_ocp, b_osq, b_st, b_yb, b_rst = bufs(5)
                b_rr, b_ET = bufs(3), bufs(2)
                b_pA, b_pS, b_pO, b_pD = bufs(2), Buf(), Buf(), Buf()
                ctr = dict(rr=0, et=0, pa=0)

                def nxt(k, n):
                    i = ctr[k] % n
                    ctr[k] += 1
                    return i

                gb = 0
                for s in range(nseq):
                    ssl = slice(s * S, (s + 1) * S)
                    fw.dma("sync", kTs[:], kT_d[:, ssl], writes=[b_kTs])
                    fw.dma("sync", ikTs[:], ikT_d[:, ssl], writes=[b_ikTs])
                    fw.dma("sync", Vs[:].rearrange("p (b f) -> p b f", f=128),
                           v_d[ssl, :].rearrange("(b p) f -> p b f", p=128), writes=[b_Vs])
                    fw.op("vector", lambda e: e.memset(Rst[:], 0.0), writes=[b_R])
                    fw.op("vector", lambda e: e.memset(Rb[:], 0.0), writes=[b_Rb])
                    for j in range(nblk):
                        bi = gb % NBUF
                        gb += 1
                        bb = b_blk[bi]
                        t0 = s * S + j * 128
                        bsl = slice(t0, t0 + 128)
                        nk = (j + 1) * 128
                        fw.dma("sync", qTb[bi][:].rearrange("p (h t) -> p h t", t=128),
                               qT_d[:, bsl].rearrange("(h p) t -> p h t", p=128), writes=[bb])
                        fw.dma("sync", iqTb[bi][:].rearrange("p (h t) -> p h t", t=128),
                               iqT_d[:, bsl].rearrange("(h p) t -> p h t", p=128), writes=[bb])
                        fw.dma("sync", iwb[bi][:], iw_d[bsl, :], writes=[bb])
                        fw.dma("sync", rqTb[bi][:].rearrange("p (h t) -> p h t", t=128),
                               rqT_d[:, bsl].rearrange("(h p) t -> p h t", p=128), writes=[bb])
                        fw.dma("sync", rkTb[bi][:].rearrange("p (h t) -> p h t", t=128),
                               rkT_d[:, bsl].rearrange("(h p) t -> p h t", p=128), writes=[bb])
                        fw.dma("sync", rkb[bi][:], rk_d[bsl, :], writes=[bb])
                        fw.dma("sync", rvb[bi][:], rv_d[bsl, :], writes=[bb])
                        fw.dma("sync", rgTb[bi][:].rearrange("p (h t) -> p h t", t=128),
                               rgT_d[:, bsl].rearrange("(h p) t -> p h t", p=128), writes=[bb])

                        if j >= 2 and "idx" in parts:
                            npc = (nk + 511) // 512
                            for pc in range(npc):
                                w = min(512, nk - pc * 512)
                                csl = slice(pc * 512, pc * 512 + w)
                                for h in range(16):
                                    base = 64 * (h % 2)
                                    pr = h // 2
                                    ia = nxt("pa", 2)
                                    fw.op("tensor", lambda e, base=base, pr=pr, ia=ia, csl=csl, w=w: e.matmul(
                                        pA[:, ia * 512:ia * 512 + w],
                                        lhsT=iqTb[bi][base:base + 64, pr * 128:(pr + 1) * 128],
                                        rhs=ikTs[base:base + 64, csl], start=True, stop=True),
                                        reads=[bb, b_ikTs], writes=[b_pA[ia]])
                                    ir = nxt("rr", 3)
                                    fw.op("scalar", lambda e, ia=ia, ir=ir, w=w: e.activation(
                                        out=rr[ir][:, 0:w], in_=pA[:, ia * 512:ia * 512 + w], func=AF.Relu),
                                        reads=[b_pA[ia]], writes=[b_rr[ir]])
                                    if h == 0:
                                        fw.op("vector", lambda e, ir=ir, w=w, csl=csl: e.tensor_scalar(
                                            out=sacc[:, csl], in0=rr[ir][:, 0:w], scalar1=iwb[bi][:, 0:1],
                                            scalar2=None, op0=ALU.mult),
                                            reads=[b_rr[ir], bb], writes=[b_sacc])
                                    else:
                                        fw.op("vector", lambda e, ir=ir, w=w, csl=csl, h=h: e.scalar_tensor_tensor(
                                            out=sacc[:, csl], in0=rr[ir][:, 0:w], scalar=iwb[bi][:, h:h + 1],
                                            in1=sacc[:, csl], op0=ALU.mult, op1=ALU.add),
                                            reads=[b_rr[ir], bb, b_sacc], writes=[b_sacc])
                            dsl = slice(j * 128, (j + 1) * 128)
                            fw.op("vector", lambda e, dsl=dsl: e.tensor_tensor(out=sacc[:, dsl], in0=sacc[:, dsl],
                                                                               in1=causf[:], op=ALU.add),
                                  reads=[b_sacc, b_c], writes=[b_sacc])
                            src = sacc
                            bsrc = b_sacc
                            for r in range(32):
                                fw.op("vector", lambda e, src=src: e.max(out=mx[:], in_=src[:, 0:nk]),
                                      reads=[bsrc], writes=[b_mx])
                                if r < 31:
                                    fw.op("vector", lambda e, src=src: e.match_replace(
                                        out=work[:, 0:nk], in_to_replace=mx[:], in_values=src[:, 0:nk],
                                        imm_value=NEG), reads=[bsrc, b_mx], writes=[b_work])
                                    src = work
                                    bsrc = b_work
                            fw.op("vector", lambda e: e.tensor_scalar(
                                out=mb[:, 0:nk], in0=sacc[:, 0:nk], scalar1=mx[:, 7:8], scalar2=-30000.0,
                                op0=ALU.is_lt, op1=ALU.mult), reads=[b_sacc, b_mx], writes=[b_mb])
                        else:
                            if j == 1:
                                fw.op("vector", lambda e: e.memset(mb[:, 0:128], 0.0), writes=[b_mb])
                            dsl = slice(j * 128, (j + 1) * 128)
                            fw.op("vector", lambda e, dsl=dsl: e.tensor_copy(out=mb[:, dsl], in_=causb[:]),
                                  reads=[b_c], writes=[b_mb])

                        if "attn" in parts:
                            for kc in range(j + 1):
                                ksl = slice(kc * 128, (kc + 1) * 128)
                                for half in range(2):
                                    hs = slice(half * 512, (half + 1) * 512)
                                    fw.op("tensor", lambda e, ksl=ksl, hs=hs: e.matmul(
                                        pS[:, hs], lhsT=kTs[:, ksl], rhs=qTb[bi][:, hs], start=True, stop=False),
                                        reads=[b_kTs, bb], writes=[b_pS], signal=False)
                                    fw.op("tensor", lambda e, ksl=ksl, hs=hs: e.matmul(
                                        pS[:, hs], lhsT=mb[:, ksl], rhs=irep[:], start=False, stop=True),
                                        reads=[b_mb, b_c], writes=[b_pS], signal=(half == 1))
                                ie = nxt("et", 2)
                                fw.op("scalar", lambda e, ie=ie: e.activation(out=ET[ie][:], in_=pS[:], func=AF.Exp),
                                      reads=[b_pS], writes=[b_ET[ie]])
                                for half in range(2):
                                    hs = slice(half * 512, (half + 1) * 512)
                                    fw.op("tensor", lambda e, ksl=ksl, hs=hs, ie=ie, kc=kc: e.matmul(
                                        pO[:, hs], lhsT=Vs[:, ksl], rhs=ET[ie][:, hs], start=(kc == 0), stop=(kc == j)),
                                        reads=[b_Vs, b_ET[ie]], writes=[b_pO], signal=False)
                                    fw.op("tensor", lambda e, hs=hs, ie=ie, kc=kc: e.matmul(
                                        pD[:, hs], lhsT=onesb[:], rhs=ET[ie][:, hs], start=(kc == 0), stop=(kc == j)),
                                        reads=[cbuf, b_ET[ie]], writes=[b_pD], signal=(half == 1))
                            fw.op("vector", lambda e: e.reciprocal(out=rec[:], in_=pD[:]), reads=[b_pD], writes=[b_rec])
                            fw.op("vector", lambda e: e.tensor_tensor(out=ast[:], in0=pO[:], in1=rec[:], op=ALU.mult),
                                  reads=[b_pO, b_rec], writes=[b_ast])
                            fw.dma("sync", mixT_d[0:1024, bsl].rearrange("(h p) t -> p h t", p=128),
                                   ast[:].rearrange("p (h t) -> p h t", t=128), reads=[b_ast])

                        if "ret" in parts:
                            for h in range(8):
                                base = 64 * (h % 2)
                                c = h // 2
                                fw.op("tensor", lambda e, h=h, base=base, c=c: e.matmul(
                                    pA[:, (h % 2) * 512 + c * 128:(h % 2) * 512 + (c + 1) * 128],
                                    lhsT=rkTb[bi][base:base + 64, c * 128:(c + 1) * 128],
                                    rhs=rqTb[bi][base:base + 64, c * 128:(c + 1) * 128], start=True, stop=True),
                                    reads=[bb], writes=b_pA, signal=(h == 7))
                            if retlvl < 0.5:
                                continue
                            fw.op("vector", lambda e: e.tensor_tensor(out=Aall[:], in0=pA[:], in1=dtt[:], op=ALU.mult),
                                  reads=b_pA + [b_c], writes=[b_Aall])
                            if retlvl < 0.8:
                                continue
                            fw.op("gpsimd", lambda e: e.tensor_tensor(out=rqxi[:], in0=rqTb[bi][:], in1=xit[:],
                                                                      op=ALU.mult),
                                  reads=[bb, b_c], writes=[b_rqxi])
                            fw.op("gpsimd", lambda e: e.tensor_tensor(out=rkz[:], in0=rkb[bi][:], in1=ztt[:],
                                                                      op=ALU.mult),
                                  reads=[bb, b_c], writes=[b_rkz])
                            if retlvl < 2:
                                continue
                            for h in range(8):
                                base = 64 * (h % 2)
                                c = h // 2
                                hsl = slice(h * 128, (h + 1) * 128)
                                asl = slice((h % 2) * 512 + c * 128, (h % 2) * 512 + (c + 1) * 128)
                                fw.op("tensor", lambda e, hsl=hsl, asl=asl: e.matmul(
                                    pS[:, hsl], lhsT=Aall[:, asl], rhs=rvb[bi][:, hsl], start=True, stop=False),
                                    reads=[b_Aall, bb], writes=[b_pS], signal=False)
                                fw.op("tensor", lambda e, hsl=hsl, base=base, c=c: e.matmul(
                                    pS[:, hsl], lhsT=rqxi[base:base + 64, c * 128:(c + 1) * 128],
                                    rhs=Rb[base:base + 64, c * 128:(c + 1) * 128], start=False, stop=True),
                                    reads=[b_rqxi, b_Rb], writes=[b_pS], signal=(h == 7))
                            if retlvl < 3:
                                continue
                            for c in range(4):
                                fw.op("tensor", lambda e, c=c: e.matmul(
                                    pO[:, c * 256:(c + 1) * 256], lhsT=rkz[:, c * 128:(c + 1) * 128],
                                    rhs=rvb[bi][:, c * 256:(c + 1) * 256], start=True, stop=True),
                                    reads=[b_rkz, bb], writes=[b_pO], signal=(c == 3))
                            for c in range(4):
                                for hh in range(2):
                                    ps_ = slice(hh * 64, (hh + 1) * 64)
                                    fw.op("vector", lambda e, c=c, hh=hh, ps_=ps_: e.scalar_tensor_tensor(
                                        out=Rst[ps_, c * 128:(c + 1) * 128], in0=Rst[ps_, c * 128:(c + 1) * 128],
                                        scalar=gtt[ps_, c:c + 1],
                                        in1=pO[ps_, c * 256 + hh * 128:c * 256 + (hh + 1) * 128],
                                        op0=ALU.mult, op1=ALU.add),
                                        reads=[b_R, b_pO, b_c], writes=[b_R])
                            fw.op("vector", lambda e: e.tensor_copy(out=Rb[:], in_=Rst[:]), reads=[b_R], writes=[b_Rb])
                            if retlvl < 4:
                                continue
                            fw.op("scalar", lambda e: e.activation(out=ocp[:], in_=pS[:], func=AF.Copy),
                                  reads=[b_pS], writes=[b_ocp])
                            fw.op("scalar", lambda e: e.activation(out=osq[:], in_=pS[:], func=AF.Square),
                                  reads=[b_pS], writes=[b_osq])
                            fw.op("vector", lambda e: e.tensor_reduce(
                                out=st[:, 0:8], in_=ocp[:].rearrange("p (h f) -> p h f", f=128), axis=AX.X, op=ALU.add),
                                reads=[b_ocp], writes=[b_st])
                            fw.op("vector", lambda e: e.tensor_reduce(
                                out=st[:, 8:16], in_=osq[:].rearrange("p (h f) -> p h f", f=128), axis=AX.X, op=ALU.add),
                                reads=[b_osq], writes=[b_st])
                            fw.op("vector", lambda e: e.tensor_scalar(out=st[:, 16:24], in0=st[:, 0:8],
                                                                      scalar1=1.0 / 128, scalar2=None, op0=ALU.mult),
                                  reads=[b_st], writes=[b_st])
                            fw.op("vector", lambda e: e.tensor_tensor(out=st[:, 24:32], in0=st[:, 16:24],
                                                                      in1=st[:, 16:24], op=ALU.mult),
                                  reads=[b_st], writes=[b_st])
                            fw.op("vector", lambda e: e.scalar_tensor_tensor(
                                out=st[:, 32:40], in0=st[:, 8:16], scalar=1.0 / 128, in1=st[:, 24:32],
                                op0=ALU.mult, op1=ALU.subtract), reads=[b_st], writes=[b_st])
                            fw.op("scalar", lambda e: e.activation(out=st[:, 40:48], in_=st[:, 32:40], func=AF.Sqrt,
                                                                   bias=epsb[:, 0:1]),
                                  reads=[b_st, cbuf], writes=[b_st])
                            fw.op("vector", lambda e: e.reciprocal(out=st[:, 40:48], in_=st[:, 40:48]),
                                  reads=[b_st], writes=[b_st])
                            fw.op("vector", lambda e: e.scalar_tensor_tensor(
                                out=st[:, 48:56], in0=st[:, 16:24], scalar=-1.0, in1=st[:, 40:48],
                                op0=ALU.mult, op1=ALU.mult), reads=[b_st], writes=[b_st])
                            if retlvl < 5:
                                continue
                            for h in range(8):
                                hsl = slice(h * 128, (h + 1) * 128)
                                fw.op("gpsimd", lambda e, h=h, hsl=hsl: e.tensor_scalar(
                                    out=yb[:, hsl], in0=ocp[:, hsl], scalar1=st[:, 40 + h:41 + h],
                                    scalar2=st[:, 48 + h:49 + h], op0=ALU.mult, op1=ALU.add),
                                    reads=[b_ocp, b_st], writes=[b_yb])
                            for h in range(8):
                                hsl = slice(h * 128, (h + 1) * 128)
                                fw.op("tensor", lambda e, hsl=hsl: e.transpose(out=pD16[:, hsl], in_=yb[:, hsl],
                                                                               identity=identb[:]),
                                      reads=[b_yb, cbuf], writes=[b_pD], signal=(h == 7))
                            for h in range(8):
                                hsl = slice(h * 128, (h + 1) * 128)
                                fw.op("vector", lambda e, h=h, hsl=hsl: e.scalar_tensor_tensor(
                                    out=rst[:, hsl], in0=pD16[:, hsl], scalar=gn_t[:, l * 8 + h:l * 8 + h + 1],
                                    in1=rgTb[bi][:, hsl], op0=ALU.mult, op1=ALU.mult),
                                    reads=[b_pD, bb, cbuf], writes=[b_rst])
                            fw.dma("sync", mixT_d[1024:2048, bsl].rearrange("(h p) t -> p h t", p=128),
                                   rst[:].rearrange("p (h t) -> p h t", t=128), reads=[b_rst])
                fw.barrier()

        if only_seq:
            seq_phase(0)
            fw.barrier()
            return nc
        dense_phase(None, 0)
        if stop_after == "p1":
            return nc
        for l in range(nlayers):
            if l + 1 < nlayers:
                conv_layer(l + 1)
            seq_phase(l)
            if stop_after == f"p2_{l}":
                return nc
            dense_phase(l, l + 1 if l + 1 < nlayers else None)
        fw.barrier()
    return nc


_NC_CACHE = {}


def _prep_core_inputs(inputs, c, nseq, consts):
    b0 = c * nseq
    m = {}
    m["x"] = np.ascontiguousarray(inputs["x"][b0:b0 + nseq]).reshape(nseq * S, D)
    m["p"] = np.ascontiguousarray(inputs["p"][:, b0:b0 + nseq]).reshape(DEPTH, nseq * S, 256)
    m["pos"] = np.ascontiguousarray(inputs["positions"][b0:b0 + nseq]).reshape(1, nseq * S).astype(np.int32)
    return m


def _shared_inputs(inputs):
    m = {}
    for k_, n_ in [("w_in", "w_in"), ("w_out", "w_out"), ("w_ff1", "w_ff1"), ("w_ff2", "w_ff2"),
                   ("w_ple", "w_ple"), ("w_ple_gate", "w_gate")]:
        m[n_] = np.ascontiguousarray(np.asarray(inputs[k_], dtype=np.float32))
    for k_, n_ in [("pre_mix_norm", "g_premix"), ("post_mix_norm", "g_postmix"), ("pre_ff_norm", "g_preff"),
                   ("post_ff_norm", "g_postff"), ("ple_norm", "g_ple")]:
        g = np.asarray(inputs[k_], dtype=np.float32)
        m[n_] = np.ascontiguousarray(g.reshape(DEPTH, 16, 128).transpose(0, 2, 1))
    g = np.asarray(inputs["ret_gn"], dtype=np.float32)
    m["g_gn"] = np.ascontiguousarray(g.reshape(DEPTH, 8, 128).transpose(0, 2, 1))
    m.update(_consts())
    return m


def kernel(**inputs):
    inputs = {k: np.asarray(v) for k, v in inputs.items()}
    B = inputs["x"].shape[0]
    ncores = 8
    nseq = B // ncores
    if "nc" not in _NC_CACHE:
        _NC_CACHE["nc"] = build(nseq=nseq)
    nc = _NC_CACHE["nc"]
    shared = _shared_inputs(inputs)
    in_maps = []
    for c in range(ncores):
        m = dict(shared)
        m.update(_prep_core_inputs(inputs, c, nseq, None))
        in_maps.append(m)
    res = run_bass_kernel_spmd(nc, in_maps, core_ids=list(range(ncores)))
    outs = [np.asarray(r["out"]).reshape(nseq, S, D) for r in res.results]
    return np.concatenate(outs, axis=0).astype(np.float32)
```

```python
import numpy as np
import ml_dtypes
from contextlib import ExitStack
import concourse.bass as bass
import concourse.mybir as mybir
from concourse.bass_utils import run_bass_kernel_spmd

F32 = mybir.dt.float32
BF16 = mybir.dt.bfloat16
I32 = mybir.dt.int32
AF = mybir.ActivationFunctionType
ALU = mybir.AluOpType
AX = mybir.AxisListType

D = 2048
S = 2048
DEPTH = 2
KC = 16
TT = 512
NB = S // 128
AQ, AK, AV, IQ, IK, IW, RQ, RK, RV, RG, INW = 0, 1024, 1152, 1280, 2304, 2368, 2384, 2896, 3408, 4432, 5456
IN_GROUPS = [
    [(AQ, 512, 0)], [(AQ + 512, 512, 0)],
    [(AK, 128, 0), (IK, 64, 128), (IK, 64, 192), (AV, 128, 256), (IW, 16, 384), (AV, 112, 400)],
    [(IQ, 512, 0)], [(IQ + 512, 512, 0)],
    [(RQ, 512, 0)], [(RK, 512, 0)], [(RV, 512, 0)], [(RV + 512, 512, 0)],
    [(RG, 512, 0)], [(RG + 512, 512, 0)],
]
NEG = -1.0e30
SEM_LIMIT = 60000


class Buf:
    __slots__ = ("w", "r")

    def __init__(self):
        self.w = None
        self.r = {}


def bufs(n):
    return [Buf() for _ in range(n)]


class FW:
    def __init__(self, nc, es):
        self.nc = nc
        self.es = es
        self.nsem = 0
        self.E = {}
        for name in ["tensor", "vector", "scalar", "gpsimd", "sync"]:
            self.E[name] = dict(h=getattr(nc, name), sem=self._sem(), count=0, seen={},
                                selfsync=(name != "tensor"), epoch=0, last=None)
        self.pool = {q: [dict(sem=self._sem(), val=0) for _ in range(n)]
                     for q, n in [("sync", 24), ("gpsimd", 40), ("scalar", 2)]}
        self.rr = {q: 0 for q in self.pool}
        self.log = {n: [] for n in self.E}

    def _sem(self):
        self.nsem += 1
        return self.es.enter_context(self.nc.semaphore(f"sm{self.nsem}"))

    def _deps(self, reads, writes):
        d = []
        for b in reads:
            if b.w is not None:
                d.append(b.w)
        for b in writes:
            if b.w is not None:
                d.append(b.w)
            d.extend(b.r.values())
        return d

    def _wait(self, ename, deps):
        E = self.E[ename]
        need = {}
        for (key, sem, val, owner) in deps:
            if owner == ename and not E["selfsync"]:
                continue
            if E["seen"].get(key, 0) >= val:
                continue
            if key not in need or need[key][1] < val:
                need[key] = (sem, val)
        for key, (sem, val) in need.items():
            E["h"].wait_ge(sem, val)
            self.log[ename].append(("wait", id(sem), val))
            E["seen"][key] = val

    def _record(self, tok, reads, writes):
        for b in reads:
            o = b.r.get(tok[0])
            if o is None or o[2] < tok[2]:
                b.r[tok[0]] = tok
        for b in writes:
            b.w = tok
            b.r = {}

    def op(self, ename, fn, reads=(), writes=(), signal=True):
        E = self.E[ename]
        self._wait(ename, self._deps(reads, writes))
        if E["count"] >= SEM_LIMIT:
            E["sem"] = self._sem()
            E["count"] = 0
            E["epoch"] += 1
        ins = fn(E["h"])
        key = (ename, E["epoch"])
        if signal:
            E["count"] += 1
            ins.then_inc(E["sem"], 1)
            self.log[ename].append(("inc", id(E["sem"]), 1))
            tok = (key, E["sem"], E["count"], ename)
            E["last"] = tok
        else:
            tok = (key, E["sem"], E["count"] + 1, ename)
        self._record(tok, reads, writes)
        return ins

    def dma(self, q, out, in_, reads=(), writes=(), **kw):
        E = self.E[q]
        pool = self.pool[q]
        i = self.rr[q]
        self.rr[q] = (i + 1) % len(pool)
        slot = pool[i]
        if slot["val"] + 16 > SEM_LIMIT:
            slot["sem"] = self._sem()
            slot["val"] = 0
        deps = self._deps(reads, writes)
        key = ("dma", id(slot["sem"]))
        if slot["val"] > 0:
            deps.append((key, slot["sem"], slot["val"], "dma"))
        self._wait(q, deps)
        ins = E["h"].dma_start(out=out, in_=in_, **kw)
        slot["val"] += 16
        ins.then_inc(slot["sem"], 16)
        self.log[q].append(("inc", id(slot["sem"]), 16))
        tok = (key, slot["sem"], slot["val"], "dma")
        self._record(tok, reads, writes)
        return ins

    def all_tokens(self):
        toks = []
        for n, E in self.E.items():
            if E["last"] is not None:
                toks.append(E["last"])
        for q, pool in self.pool.items():
            for s in pool:
                if s["val"] > 0:
                    toks.append((("dma", id(s["sem"])), s["sem"], s["val"], "dma"))
        return toks

    def barrier(self, engines=None):
        toks = self.all_tokens()
        for n in (engines or list(self.E.keys())):
            E = self.E[n]
            ss = E["selfsync"]
            E["selfsync"] = True
            self._wait(n, toks)
            E["selfsync"] = ss


def _consts():
    c = {}
    c["c_identf"] = np.eye(128, dtype=np.float32)
    c["c_identb"] = np.eye(128, dtype=np.float32).astype(ml_dtypes.bfloat16)
    c["c_irep"] = np.tile(np.eye(128, dtype=np.float32), (1, 4)).astype(ml_dtypes.bfloat16)
    pm = np.zeros((3, 128, 128), np.float32)
    invf = np.zeros((128, 3), np.float64)
    for i in range(16):
        pm[0, i + 16, i] = -1.0
        pm[0, i, i + 16] = 1.0
        invf[i, 0] = invf[i + 16, 0] = 500000.0 ** (-i / 16.0)
    for o in (0, 64):
        for i in range(8):
            pm[1, o + i + 8, o + i] = -1.0
            pm[1, o + i, o + i + 8] = 1.0
            invf[o + i, 1] = invf[o + i + 8, 1] = 500000.0 ** (-i / 8.0)
    for o in (0, 64):
        for i in range(32):
            pm[2, o + i + 32, o + i] = -1.0
            pm[2, o + i, o + i + 32] = 1.0
            invf[o + i, 2] = invf[o + i + 32, 2] = 10000.0 ** (-i / 32.0)
    c["c_pm"] = pm.astype(ml_dtypes.bfloat16)
    c["c_invf"] = (invf.astype(np.float32).astype(np.float64) / (2 * np.pi)).astype(np.float32)
    q = np.arange(128)[:, None]
    k = np.arange(128)[None, :]
    c["c_causb"] = np.where(k <= q, 0.0, -30000.0).astype(np.float32).astype(ml_dtypes.bfloat16)
    c["c_causf"] = np.where(k <= q, 0.0, NEG).astype(np.float32)
    H = 8
    C = 128
    gamma = (1.0 - 2.0 ** (-5.0 - np.arange(H, dtype=np.float32))).astype(np.float32)
    log_g = np.log(gamma).astype(np.float32)
    i = np.arange(C, dtype=np.float32)
    dt = np.zeros((128, H, 128), np.float32)
    for h in range(H):
        diff = i[None, :] - i[:, None]
        dt[:, h, :] = np.where(diff >= 0, np.exp(np.maximum(diff, 0.0) * log_g[h]), 0.0)
    c["c_dt"] = np.ascontiguousarray(dt.reshape(128, 4, 2, 128).transpose(0, 2, 1, 3)).reshape(128, 1024).astype(np.float32)
    zeta = np.exp((C - 1.0 - i)[None, :] * log_g[:, None]).astype(np.float32)
    xi = np.exp((i + 1.0)[None, :] * log_g[:, None]).astype(np.float32)
    gch = np.exp(C * log_g).astype(np.float32)
    xit = np.zeros((128, 4, 128), np.float32)
    gt = np.zeros((128, 4), np.float32)
    for cc in range(4):
        for hh in range(2):
            xit[hh * 64:(hh + 1) * 64, cc, :] = xi[2 * cc + hh][None, :]
            gt[hh * 64:(hh + 1) * 64, cc] = gch[2 * cc + hh]
    c["c_xi"] = xit.reshape(128, 512)
    c["c_gt"] = gt
    zt = np.zeros((128, H, 64), np.float32)
    for h in range(H):
        zt[:, h, :] = zeta[h][:, None]
    c["c_zt"] = zt.reshape(128, 512)
    return c


CONST_SPECS = {
    "c_identf": ([128, 128], F32), "c_identb": ([128, 128], BF16), "c_irep": ([128, 512], BF16),
    "c_pm": ([3, 128, 128], BF16), "c_invf": ([128, 3], F32), "c_causb": ([128, 128], BF16),
    "c_causf": ([128, 128], F32), "c_dt": ([128, 1024], F32), "c_xi": ([128, 512], F32),
    "c_gt": ([128, 4], F32), "c_zt": ([128, 512], F32),
}
GAIN_NAMES = ["g_premix", "g_postmix", "g_preff", "g_postff", "g_ple"]


def build(nseq=2, dbg=False, stop_after=None, nlayers=DEPTH, only_seq=False, nblk=NB, parts=("idx", "attn", "ret"), retlvl=9):
    NT = nseq * S
    NTILE = NT // TT
    nc = bass.Bass("TRN2", target_bir_lowering=False)
    kin = "ExternalInput"
    ksc = "ExternalOutput" if dbg else "Internal"

    def dram(name, shape, dt, kind):
        return nc.dram_tensor(name, shape, dt, kind=kind).ap()

    kin_ = kin
    if only_seq:
        kin = "Internal"
    x_d = dram("x", [NT, D], F32, kin)
    p_d = dram("p", [DEPTH, NT, 256], F32, kin)
    pos_d = dram("pos", [1, NT], I32, kin)
    w_in_d = dram("w_in", [DEPTH, D, INW], F32, kin)
    w_out_d = dram("w_out", [DEPTH, D, D], F32, kin)
    w_ff1_d = dram("w_ff1", [DEPTH, D, 4 * D], F32, kin)
    w_ff2_d = dram("w_ff2", [DEPTH, 4 * D, D], F32, kin)
    w_ple_d = dram("w_ple", [DEPTH, 256, D], F32, kin)
    w_gate_d = dram("w_gate", [DEPTH, D, D], F32, kin)
    kin = kin_
    gains_d = {n: dram(n, [DEPTH, 128, 16], F32, kin) for n in GAIN_NAMES}
    gn_d = dram("g_gn", [DEPTH, 128, 8], F32, kin)
    cd = {n: dram(n, sh, dt, kin) for n, (sh, dt) in CONST_SPECS.items()}
    out_d = dram("out", [NT, D], F32, "ExternalOutput")

    wb_in = dram("wb_in", [DEPTH, 11, 128, 8192], BF16, "Internal")
    wb_out = dram("wb_out", [DEPTH, 4, 128, 8192], BF16, "Internal")
    wb_ff1 = dram("wb_ff1", [DEPTH, 16, 128, 8192], BF16, "Internal")
    wb_ff2 = dram("wb_ff2", [DEPTH, 16, 128, 8192], BF16, "Internal")
    wb_gate = dram("wb_gate", [DEPTH, 4, 128, 8192], BF16, "Internal")
    wb_ple = dram("wb_ple", [DEPTH, 4, 128, 1024], BF16, "Internal")
    ksi = kin if only_seq else ksc
    hT_d = dram("hT", [D, NT], F32, ksc)
    tabs_d = dram("tabs", [6, 128, NT], F32, ksc)
    qT_d = dram("qT", [1024, NT], BF16, ksi)
    kT_d = dram("kT", [128, NT], BF16, ksi)
    ikT_d = dram("ikT", [128, NT], BF16, ksi)
    iqT_d = dram("iqT", [1024, NT], BF16, ksi)
    rqT_d = dram("rqT", [512, NT], BF16, ksi)
    rkT_d = dram("rkT", [512, NT], BF16, ksi)
    rgT_d = dram("rgT", [1024, NT], BF16, ksi)
    v_d = dram("v", [NT, 128], BF16, ksi)
    iw_d = dram("iw", [NT, 16], F32, ksi)
    rk_d = dram("rk", [NT, 512], BF16, ksi)
    rv_d = dram("rv", [NT, 1024], BF16, ksi)
    mixT_d = dram("mixT", [D, NT], BF16, ksc)

    with ExitStack() as es:
        fw = FW(nc, es)
        nc._fw = fw

        uid = [0]

        def sb(ctx, name, shape, dt):
            uid[0] += 1
            return ctx.enter_context(nc.sbuf_tensor(f"s{uid[0]}_{name}", shape, dt))

        def pst(ctx, name, shape, dt):
            uid[0] += 1
            return ctx.enter_context(nc.psum_tensor(f"p{uid[0]}_{name}", shape, dt))

        identf = sb(es, "identf", [128, 128], F32)
        identb = sb(es, "identb", [128, 128], BF16)
        onesb = sb(es, "onesb", [128, 128], BF16)
        gains = {n: sb(es, "t_" + n, [128, DEPTH * 16], F32) for n in GAIN_NAMES}
        gn_t = sb(es, "t_gn", [128, DEPTH * 8], F32)
        cbuf = Buf()
        fw.dma("sync", identf[:], cd["c_identf"], writes=[cbuf])
        fw.dma("sync", identb[:], cd["c_identb"], writes=[cbuf])
        for n in GAIN_NAMES:
            for l in range(DEPTH):
                fw.dma("sync", gains[n][:, l * 16:(l + 1) * 16], gains_d[n][l], writes=[cbuf])
        for l in range(DEPTH):
            fw.dma("sync", gn_t[:, l * 8:(l + 1) * 8], gn_d[l], writes=[cbuf])
        fw.op("vector", lambda e: e.memset(onesb[:], 1.0), writes=[cbuf])

        wbuf = {}

        def conv(key, dst, src):
            b = wbuf.setdefault(key, Buf())
            fw.dma("gpsimd", dst, src, writes=[b])

        def conv_layer(l):
            for g, pieces in enumerate(IN_GROUPS):
                for (s0, wd, d0) in pieces:
                    dst = wb_in[l, g].rearrange("p (k c) -> p k c", c=512)[:, :, d0:d0 + wd]
                    src = w_in_d[l][:, s0:s0 + wd].rearrange("(k p) c -> p k c", p=128)
                    conv(("in", l, g), dst, src)
            for g in range(4):
                dst = wb_out[l, g].rearrange("p (k c) -> p k c", c=512)
                src = w_out_d[l][:, g * 512:(g + 1) * 512].rearrange("(k p) c -> p k c", p=128)
                conv(("out", l, g), dst, src)
            for g in range(16):
                dst = wb_ff1[l, g].rearrange("p (k c) -> p k c", c=512)
                src = w_ff1_d[l][:, g * 512:(g + 1) * 512].rearrange("(k p) c -> p k c", p=128)
                conv(("ff1", l, g), dst, src)
            for q in range(4):
                for og in range(4):
                    dst = wb_ff2[l, q * 4 + og].rearrange("p (k c) -> p k c", c=512)
                    src = w_ff2_d[l][q * 2048:(q + 1) * 2048, og * 512:(og + 1) * 512].rearrange(
                        "(k p) c -> p k c", p=128)
                    conv(("ff2", l, q * 4 + og), dst, src)
            for g in range(4):
                dst = wb_ple[l, g].rearrange("p (k c) -> p k c", c=512)
                src = w_ple_d[l][:, g * 512:(g + 1) * 512].rearrange("(k p) c -> p k c", p=128)
                conv(("ple", l, g), dst, src)
                dst = wb_gate[l, g].rearrange("p (k c) -> p k c", c=512)
                src = w_gate_d[l][:, g * 512:(g + 1) * 512].rearrange("(k p) c -> p k c", p=128)
                conv(("gate", l, g), dst, src)

        if not only_seq:
            conv_layer(0)

        def tables_phase():
            with ExitStack() as ph:
                invf = sb(ph, "invf", [128, 3], F32)
                posi = sb(ph, "posi", [128, S], I32)
                posf = sb(ph, "posf", [128, S], F32)
                ys = sb(ph, "ys", [128, S], F32)
                ki = sb(ph, "ki", [128, S], I32)
                kf = sb(ph, "kf", [128, S], F32)
                fr = sb(ph, "fr", [128, S], F32)
                tb = [sb(ph, f"tb{i}", [128, S], F32) for i in range(2)]
                b_invf, b_posi, b_posf, b_ys, b_ki, b_kf, b_fr = bufs(7)
                b_tb = bufs(2)
                fw.dma("sync", invf[:], cd["c_invf"], writes=[b_invf])
                n = 0
                for s in range(nseq):
                    fw.dma("sync", posi[:], pos_d[0:1, s * S:(s + 1) * S].partition_broadcast(128),
                           writes=[b_posi])
                    fw.op("vector", lambda e: e.tensor_copy(out=posf[:], in_=posi[:]),
                          reads=[b_posi], writes=[b_posf])
                    for t in range(3):
                        for cs in range(2):
                            if cs == 0:
                                fw.op("vector", lambda e, t=t: e.tensor_scalar(
                                    out=ys[:], in0=posf[:], scalar1=invf[:, t:t + 1], scalar2=0.25,
                                    op0=ALU.mult, op1=ALU.add), reads=[b_posf, b_invf], writes=[b_ys])
                            else:
                                fw.op("vector", lambda e, t=t: e.tensor_scalar(
                                    out=ys[:], in0=posf[:], scalar1=invf[:, t:t + 1], scalar2=None,
                                    op0=ALU.mult), reads=[b_posf, b_invf], writes=[b_ys])
                            fw.op("vector", lambda e: e.tensor_copy(out=ki[:], in_=ys[:]),
                                  reads=[b_ys], writes=[b_ki])
                            fw.op("vector", lambda e: e.tensor_copy(out=kf[:], in_=ki[:]),
                                  reads=[b_ki], writes=[b_kf])
                            fw.op("vector", lambda e: e.tensor_tensor(out=fr[:], in0=ys[:], in1=kf[:],
                                                                      op=ALU.subtract),
                                  reads=[b_ys, b_kf], writes=[b_fr])
                            fw.op("vector", lambda e: e.tensor_scalar(out=kf[:], in0=fr[:], scalar1=0.0,
                                                                      scalar2=None, op0=ALU.is_lt),
                                  reads=[b_fr], writes=[b_kf])
                            fw.op("vector", lambda e: e.tensor_tensor(out=fr[:], in0=fr[:], in1=kf[:],
                                                                      op=ALU.add),
                                  reads=[b_fr, b_kf], writes=[b_fr])
                            tt = tb[n % 2]
                            bt = b_tb[n % 2]
                            n += 1
                            fw.op("scalar", lambda e, tt=tt: e.activation(
                                out=tt[:], in_=fr[:], func=AF.Sin, scale=-2.0 * np.pi, bias=pib[:, 0:1]),
                                reads=[b_fr, cbuf], writes=[bt])
                            fw.dma("sync", tabs_d[2 * t + cs][:, s * S:(s + 1) * S], tt[:], reads=[bt])
                fw.barrier()

        pib = sb(es, "pib", [128, 2], F32)
        fw.op("vector", lambda e: e.memset(pib[:, 0:1], float(np.pi)), writes=[cbuf])
        fw.op("vector", lambda e: e.memset(pib[:, 1:2], 1e-6), writes=[cbuf])
        epsb = sb(es, "epsb", [128, 1], F32)
        fw.op("vector", lambda e: e.memset(epsb[:], 1e-5), writes=[cbuf])
        if only_seq:
            seq_phase_holder = []
        else:
            tables_phase()
        if stop_after == "tables":
            fw.barrier()
            return nc

        hbuf = bufs(NTILE)

        def dense_phase(l_prev, l_next):
            with ExitStack() as ph:
                hT = sb(ph, "hT", [128, 16 * TT], F32)
                yT = sb(ph, "yT", [128, 16 * TT], F32)
                act = sb(ph, "act", [128, 16 * TT], BF16)
                uT = sb(ph, "uT", [128, 16 * TT], BF16)
                NW = 3
                Wt = [sb(ph, f"Wt{i}", [128, 8192], BF16) for i in range(NW)]
                Wp = [sb(ph, f"Wp{i}", [128, 1024], BF16) for i in range(2)]
                tabs = sb(ph, "tabs_t", [128, 6 * TT], F32)
                rstd = sb(ph, "rstd", [128, TT], F32)
                sqr = [sb(ph, f"sqr{i}", [128, TT], BF16) for i in range(2)]
                tmp = [sb(ph, f"tmp{i}", [128, TT], F32) for i in range(4)]
                qb = [sb(ph, f"qb{i}", [128, TT], BF16) for i in range(2)]
                stg = [sb(ph, f"stg{i}", [128, TT], BF16) for i in range(3)]
                tok = [sb(ph, f"tok{i}", [128, 512], BF16) for i in range(3)]
                iwst = sb(ph, "iwst", [128, 64], F32)
                pin = sb(ph, "pin", [128, 1024], F32)
                pbt = sb(ph, "pbt", [128, 1024], BF16)
                pT = sb(ph, "pT", [128, 1024], BF16)
                pm = sb(ph, "pm", [128, 3 * 128], BF16)
                acc = [pst(ph, f"acc{i}", [128, 512], F32) for i in range(4)]
                ssb = pst(ph, "ssb", [128, 512], F32)
                ppb = [pst(ph, f"ppb{i}", [128, 512], F32) for i in range(2)]
                trb = pst(ph, "trb", [128, 512], F32)
                trb16 = trb[:].bitcast(BF16)

                b_hT, b_yT, b_act = bufs(16), bufs(16), bufs(16)
                b_uT = bufs(16)
                b_W, b_Wp = bufs(NW), bufs(2)
                b_tabs, b_rstd, b_ss, b_trb, b_pin, b_pbt, b_pT, b_pm, b_iwst = bufs(9)
                b_sqr, b_tmp, b_qb, b_stg, b_tok = bufs(2), bufs(4), bufs(2), bufs(3), bufs(3)
                b_acc, b_pp = bufs(4), bufs(2)
                ctr = dict(w=0, acc=0, sqr=0, tmp=0, qb=0, stg=0, tok=0, pp=0, wp=0)

                def nxt(k, n):
                    i = ctr[k] % n
                    ctr[k] += 1
                    return i

                for t in range(3):
                    fw.dma("sync", pm[:, t * 128:(t + 1) * 128], cd["c_pm"][t], writes=[b_pm])

                def hc(c):
                    return hT[:, c * TT:(c + 1) * TT]

                def yc(c):
                    return yT[:, c * TT:(c + 1) * TT]

                def ac(c):
                    return act[:, c * TT:(c + 1) * TT]

                def uc(c):
                    return uT[:, c * TT:(c + 1) * TT]

                plan = []
                for ti_p in range(NTILE):
                    if l_prev is not None:
                        lp = l_prev
                        plan += [(wb_out[lp, og], ("out", lp, og)) for og in range(4)]
                        for q in range(4):
                            plan += [(wb_ff1[lp, q * 4 + g1], ("ff1", lp, q * 4 + g1)) for g1 in range(4)]
                            plan += [(wb_ff2[lp, q * 4 + og], ("ff2", lp, q * 4 + og)) for og in range(4)]
                        plan += [(wb_gate[lp, og], ("gate", lp, og)) for og in range(4)]
                    if l_next is not None:
                        plan += [(wb_in[l_next, g], ("in", l_next, g)) for g in range(11)]
                wstate = dict(issue=0, use=0)

                def wload(src, key):
                    k = wstate["use"]
                    assert plan[k][1] == key, (plan[k][1], key)
                    while wstate["issue"] < min(len(plan), k + NW):
                        ki = wstate["issue"]
                        fw.dma("sync", Wt[ki % NW][:], plan[ki][0], reads=[wbuf[plan[ki][1]]],
                               writes=[b_W[ki % NW]])
                        wstate["issue"] += 1
                    wstate["use"] += 1
                    return Wt[k % NW], b_W[k % NW]

                def gemm_f(W, bW, j, rhs_fn, rhs_bufs, nk):
                    i = nxt("acc", 4)
                    for k in range(nk):
                        fw.op("tensor", lambda e, k=k: e.matmul(
                            acc[i][:], lhsT=W[:, k * 512 + j * 128:k * 512 + (j + 1) * 128], rhs=rhs_fn(k),
                            start=(k == 0), stop=(k == nk - 1)),
                            reads=[bW] + rhs_bufs, writes=[b_acc[i]], signal=(k == nk - 1))
                    return acc[i], b_acc[i]

                def rstd_from(src_fn, src_bufs):
                    for c in range(16):
                        i = nxt("sqr", 2)
                        fw.op("scalar", lambda e, c=c, i=i: e.activation(out=sqr[i][:], in_=src_fn(c),
                                                                         func=AF.Square),
                              reads=[src_bufs[c]], writes=[b_sqr[i]])
                        fw.op("tensor", lambda e, c=c, i=i: e.matmul(ssb[:], lhsT=onesb[:], rhs=sqr[i][:],
                                                                     start=(c == 0), stop=(c == 15)),
                              reads=[b_sqr[i], cbuf], writes=[b_ss], signal=True)
                    fw.op("scalar", lambda e: e.activation(out=rstd[:], in_=ssb[:], func=AF.Sqrt,
                                                           scale=1.0 / D, bias=pib[:, 1:2]),
                          reads=[b_ss, cbuf], writes=[b_rstd])
                    fw.op("vector", lambda e: e.reciprocal(out=rstd[:], in_=rstd[:]),
                          reads=[b_rstd], writes=[b_rstd])

                def norm_to_act(gname, l):
                    rstd_from(hc, b_hT)
                    g = gains[gname]
                    for c in range(16):
                        fw.op("vector", lambda e, c=c: e.scalar_tensor_tensor(
                            out=ac(c), in0=hc(c), scalar=g[:, l * 16 + c:l * 16 + c + 1], in1=rstd[:],
                            op0=ALU.mult, op1=ALU.mult),
                            reads=[b_hT[c], b_rstd, cbuf], writes=[b_act[c]])

                def norm_add(gname, l):
                    rstd_from(yc, b_yT)
                    g = gains[gname]
                    for c in range(16):
                        i = nxt("tmp", 4)
                        fw.op("vector", lambda e, c=c, i=i: e.scalar_tensor_tensor(
                            out=tmp[i][:], in0=yc(c), scalar=g[:, l * 16 + c:l * 16 + c + 1], in1=rstd[:],
                            op0=ALU.mult, op1=ALU.mult),
                            reads=[b_yT[c], b_rstd, cbuf], writes=[b_tmp[i]])
                        fw.op("gpsimd", lambda e, c=c, i=i: e.tensor_tensor(out=hc(c), in0=hc(c), in1=tmp[i][:],
                                                                            op=ALU.add),
                              reads=[b_tmp[i], b_hT[c]], writes=[b_hT[c]])

                def p1(l, ti):
                    tsl = slice(ti * TT, (ti + 1) * TT)
                    fw.dma("sync", tabs[:].rearrange("p (a t) -> p a t", t=TT),
                           tabs_d[:, :, tsl].rearrange("a p t -> p a t"), writes=[b_tabs])
                    norm_to_act("g_premix", l)

                    pend = []

                    def flush():
                        while pend:
                            pend.pop(0)()

                    def rope_epi(a, ba, typ, scale, dst, after=None):
                        i = nxt("qb", 2)
                        fw.op("scalar", lambda e: e.activation(out=qb[i][:], in_=a[:], func=AF.Copy,
                                                               scale=float(scale)),
                              reads=[ba], writes=[b_qb[i]])
                        ip = nxt("pp", 2)
                        i1 = nxt("tmp", 4)
                        i2 = nxt("tmp", 4)
                        si = nxt("stg", 3)
                        Ct = tabs[:, (2 * typ) * TT:(2 * typ + 1) * TT]
                        St = tabs[:, (2 * typ + 1) * TT:(2 * typ + 2) * TT]
                        fw.op("gpsimd", lambda e: e.tensor_tensor(out=tmp[i1][:], in0=qb[i][:], in1=Ct,
                                                                  op=ALU.mult),
                              reads=[b_qb[i], b_tabs], writes=[b_tmp[i1]])

                        def rest():
                            fw.op("tensor", lambda e: e.matmul(ppb[ip][:], lhsT=pm[:, typ * 128:(typ + 1) * 128],
                                                               rhs=qb[i][:], start=True, stop=True),
                                  reads=[b_qb[i], b_pm], writes=[b_pp[ip]])
                            fw.op("vector", lambda e: e.tensor_tensor(out=tmp[i2][:], in0=ppb[ip][:], in1=St,
                                                                      op=ALU.mult),
                                  reads=[b_pp[ip], b_tabs], writes=[b_tmp[i2]])
                            fw.op("gpsimd", lambda e: e.tensor_tensor(out=stg[si][:], in0=tmp[i1][:],
                                                                      in1=tmp[i2][:], op=ALU.add),
                                  reads=[b_tmp[i1], b_tmp[i2]], writes=[b_stg[si]])
                            fw.dma("sync", dst, stg[si][:], reads=[b_stg[si]])
                            if after is not None:
                                after(si)
                        pend.append(rest)
                        return si

                    rhs_act = lambda k: ac(k)

                    def gemm_p1(W, bW, j):
                        r_ = gemm_f(W, bW, j, lambda k: ac(k), b_act, 16)
                        flush()
                        return r_

                    for g in range(11):
                        W, bW = wload(wb_in[l, g], ("in", l, g))
                        if g in (0, 1):
                            for j in range(4):
                                h = g * 4 + j
                                a, ba = gemm_p1(W, bW, j)
                                rope_epi(a, ba, 0, 128.0 ** -0.5, qT_d[h * 128:(h + 1) * 128, tsl])
                        elif g == 2:
                            a, ba = gemm_p1(W, bW, 0)
                            rope_epi(a, ba, 0, 1.0, kT_d[:, tsl])
                            a, ba = gemm_p1(W, bW, 1)
                            rope_epi(a, ba, 1, 1.0, ikT_d[:, tsl])
                            ti_ = nxt("tok", 3)
                            for tb in range(4):
                                i = nxt("acc", 4)
                                for k in range(16):
                                    fw.op("tensor", lambda e, k=k, tb=tb, i=i: e.matmul(
                                        acc[i][:, 0:144], lhsT=act[:, k * TT + tb * 128:k * TT + (tb + 1) * 128],
                                        rhs=W[:, k * 512 + 256:k * 512 + 400], start=(k == 0), stop=(k == 15)),
                                        reads=[bW] + b_act, writes=[b_acc[i]], signal=(k == 15))
                                fw.op("scalar", lambda e, tb=tb, i=i: e.activation(
                                    out=tok[ti_][:, tb * 128:(tb + 1) * 128], in_=acc[i][:, 0:128], func=AF.Copy),
                                    reads=[b_acc[i]], writes=[b_tok[ti_]])
                                fw.op("scalar", lambda e, tb=tb, i=i: e.activation(
                                    out=iwst[:, tb * 16:(tb + 1) * 16], in_=acc[i][:, 128:144], func=AF.Copy,
                                    scale=1.0 / 32.0),
                                    reads=[b_acc[i]], writes=[b_iwst])
                            fw.dma("sync", v_d[tsl, :].rearrange("(b p) f -> p b f", p=128),
                                   tok[ti_][:].rearrange("p (b f) -> p b f", f=128), reads=[b_tok[ti_]])
                            fw.dma("sync", iw_d[tsl, :].rearrange("(b p) f -> p b f", p=128),
                                   iwst[:].rearrange("p (b f) -> p b f", f=16), reads=[b_iwst])
                        elif g in (3, 4):
                            for j in range(4):
                                pr = (g - 3) * 4 + j
                                a, ba = gemm_p1(W, bW, j)
                                rope_epi(a, ba, 1, 1.0, iqT_d[pr * 128:(pr + 1) * 128, tsl])
                        elif g == 5:
                            for j in range(4):
                                a, ba = gemm_p1(W, bW, j)
                                rope_epi(a, ba, 2, 1.0, rqT_d[j * 128:(j + 1) * 128, tsl])
                        elif g == 6:
                            for j in range(4):
                                a, ba = gemm_p1(W, bW, j)
                                def rk_after(si, j=j):
                                    for tb in range(4):
                                        fw.op("tensor", lambda e, tb=tb: e.transpose(
                                            out=trb16[:, tb * 128:(tb + 1) * 128],
                                            in_=stg[si][:, tb * 128:(tb + 1) * 128], identity=identb[:]),
                                            reads=[b_stg[si], cbuf], writes=[b_trb], signal=(tb == 3))
                                    ti_ = nxt("tok", 3)
                                    fw.op("scalar", lambda e: e.activation(out=tok[ti_][:], in_=trb16[:, 0:512],
                                                                           func=AF.Copy),
                                          reads=[b_trb], writes=[b_tok[ti_]])
                                    fw.dma("sync",
                                           rk_d[tsl, j * 128:(j + 1) * 128].rearrange("(b p) f -> p b f", p=128),
                                           tok[ti_][:].rearrange("p (b f) -> p b f", f=128), reads=[b_tok[ti_]])
                                rope_epi(a, ba, 2, 0.125, rkT_d[j * 128:(j + 1) * 128, tsl], after=rk_after)
                        elif g in (7, 8):
                            for tb in range(4):
                                i = nxt("acc", 4)
                                for k in range(16):
                                    fw.op("tensor", lambda e, k=k, tb=tb, i=i: e.matmul(
                                        acc[i][:], lhsT=act[:, k * TT + tb * 128:k * TT + (tb + 1) * 128],
                                        rhs=W[:, k * 512:(k + 1) * 512], start=(k == 0), stop=(k == 15)),
                                        reads=[bW] + b_act, writes=[b_acc[i]], signal=(k == 15))
                                ti_ = nxt("tok", 3)
                                fw.op("scalar", lambda e, i=i: e.activation(out=tok[ti_][:], in_=acc[i][:],
                                                                            func=AF.Copy),
                                      reads=[b_acc[i]], writes=[b_tok[ti_]])
                                r0 = ti * TT + tb * 128
                                fw.dma("sync", rv_d[r0:r0 + 128, (g - 7) * 512:(g - 6) * 512], tok[ti_][:],
                                       reads=[b_tok[ti_]])
                        else:
                            for j in range(4):
                                hh = (g - 9) * 4 + j
                                a, ba = gemm_p1(W, bW, j)
                                si = nxt("stg", 3)
                                fw.op("scalar", lambda e: e.activation(out=stg[si][:], in_=a[:], func=AF.Silu),
                                      reads=[ba], writes=[b_stg[si]])
                                fw.dma("sync", rgT_d[hh * 128:(hh + 1) * 128, tsl], stg[si][:], reads=[b_stg[si]])
                    flush()

                def p3(l, ti):
                    tsl = slice(ti * TT, (ti + 1) * TT)
                    fw.dma("sync", act[:].rearrange("p (c t) -> p c t", t=TT),
                           mixT_d[:, tsl].rearrange("(c p) t -> p c t", p=128), writes=b_act)
                    rhs_act = lambda k: ac(k)
                    for og in range(4):
                        W, bW = wload(wb_out[l, og], ("out", l, og))
                        for j in range(4):
                            oc = og * 4 + j
                            a, ba = gemm_f(W, bW, j, rhs_act, b_act, 16)
                            fw.op("scalar", lambda e, oc=oc: e.activation(out=yc(oc), in_=a[:], func=AF.Copy),
                                  reads=[ba], writes=[b_yT[oc]])
                    norm_add("g_postmix", l)
                    norm_to_act("g_preff", l)
                    for q in range(4):
                        for g1 in range(4):
                            W, bW = wload(wb_ff1[l, q * 4 + g1], ("ff1", l, q * 4 + g1))
                            for j in range(4):
                                ucx = g1 * 4 + j
                                a, ba = gemm_f(W, bW, j, rhs_act, b_act, 16)
                                i = nxt("tmp", 4)
                                fw.op("scalar", lambda e, i=i: e.activation(out=tmp[i][:], in_=a[:], func=AF.Relu),
                                      reads=[ba], writes=[b_tmp[i]])
                                fw.op("gpsimd", lambda e, i=i, ucx=ucx: e.tensor_tensor(
                                    out=uc(ucx), in0=tmp[i][:], in1=tmp[i][:], op=ALU.mult),
                                    reads=[b_tmp[i]], writes=[b_uT[ucx]])
                        for og in range(4):
                            W, bW = wload(wb_ff2[l, q * 4 + og], ("ff2", l, q * 4 + og))
                            for j in range(4):
                                oc = og * 4 + j
                                a, ba = gemm_f(W, bW, j, lambda k: uc(k), b_uT, 16)
                                if q == 0:
                                    fw.op("scalar", lambda e, oc=oc: e.activation(out=yc(oc), in_=a[:],
                                                                                  func=AF.Copy),
                                          reads=[ba], writes=[b_yT[oc]])
                                else:
                                    fw.op("vector", lambda e, oc=oc: e.tensor_tensor(out=yc(oc), in0=yc(oc),
                                                                                     in1=a[:], op=ALU.add),
                                          reads=[ba, b_yT[oc]], writes=[b_yT[oc]])
                    norm_add("g_postff", l)
                    fw.dma("sync", pin[:].rearrange("p (b f) -> p b f", f=256),
                           p_d[l][tsl, :].rearrange("(b p) f -> p b f", p=128), writes=[b_pin])
                    fw.op("vector", lambda e: e.tensor_copy(out=pbt[:], in_=pin[:]), reads=[b_pin], writes=[b_pbt])
                    for k2 in range(2):
                        for tb in range(4):
                            fw.op("tensor", lambda e, k2=k2, tb=tb: e.transpose(
                                out=trb16[:, tb * 128:(tb + 1) * 128],
                                in_=pbt[:, tb * 256 + k2 * 128:tb * 256 + (k2 + 1) * 128], identity=identb[:]),
                                reads=[b_pbt, cbuf], writes=[b_trb], signal=(tb == 3))
                        fw.op("scalar", lambda e, k2=k2: e.activation(out=pT[:, k2 * 512:(k2 + 1) * 512],
                                                                      in_=trb16[:, 0:512], func=AF.Copy),
                              reads=[b_trb], writes=[b_pT])
                    for c in range(16):
                        fw.op("gpsimd", lambda e, c=c: e.tensor_copy(out=ac(c), in_=hc(c)),
                              reads=[b_hT[c]], writes=[b_act[c]])
                    for og in range(4):
                        W, bW = wload(wb_gate[l, og], ("gate", l, og))
                        ip = nxt("wp", 2)
                        fw.dma("sync", Wp[ip][:], wb_ple[l, og], reads=[wbuf[("ple", l, og)]], writes=[b_Wp[ip]])
                        for j in range(4):
                            oc = og * 4 + j
                            a, ba = gemm_f(W, bW, j, rhs_act, b_act, 16)
                            a2, ba2 = gemm_f(Wp[ip], b_Wp[ip], j, lambda k: pT[:, k * 512:(k + 1) * 512], [b_pT], 2)
                            i = nxt("tmp", 4)
                            fw.op("scalar", lambda e, i=i: e.activation(out=tmp[i][:], in_=a[:], func=AF.Sigmoid),
                                  reads=[ba], writes=[b_tmp[i]])
                            fw.op("vector", lambda e, i=i, oc=oc: e.tensor_tensor(out=yc(oc), in0=tmp[i][:],
                                                                                  in1=a2[:], op=ALU.mult),
                                  reads=[b_tmp[i], ba2], writes=[b_yT[oc]])
                    norm_add("g_ple", l)

                for ti in range(NTILE):
                    tsl = slice(ti * TT, (ti + 1) * TT)
                    if l_prev is None:
                        for tb in range(4):
                            r0 = ti * TT + tb * 128
                            fw.dma("sync", yT[:, tb * 2048:(tb + 1) * 2048], x_d[r0:r0 + 128, :],
                                   writes=b_yT[tb * 4:(tb + 1) * 4])
                        for c in range(16):
                            for tb in range(4):
                                fw.op("tensor", lambda e, c=c, tb=tb: e.transpose(
                                    out=trb[:, tb * 128:(tb + 1) * 128],
                                    in_=yT[:, tb * 2048 + c * 128:tb * 2048 + (c + 1) * 128], identity=identf[:]),
                                    reads=b_yT[tb * 4:(tb + 1) * 4] + [cbuf], writes=[b_trb], signal=(tb == 3))
                            fw.op("scalar", lambda e, c=c: e.activation(out=hc(c), in_=trb[:], func=AF.Copy),
                                  reads=[b_trb], writes=[b_hT[c]])
                    else:
                        fw.dma("sync", hT[:].rearrange("p (c t) -> p c t", t=TT),
                               hT_d[:, tsl].rearrange("(c p) t -> p c t", p=128),
                               reads=[hbuf[ti]], writes=b_hT)
                        p3(l_prev, ti)
                    if l_next is not None:
                        fw.dma("sync", hT_d[:, tsl].rearrange("(c p) t -> p c t", p=128),
                               hT[:].rearrange("p (c t) -> p c t", t=TT), reads=b_hT, writes=[hbuf[ti]])
                        p1(l_next, ti)
                    else:
                        for tb in range(4):
                            for c4 in range(4):
                                for cc in range(4):
                                    c = c4 * 4 + cc
                                    fw.op("tensor", lambda e, c=c, cc=cc, tb=tb: e.transpose(
                                        out=trb[:, cc * 128:(cc + 1) * 128],
                                        in_=hT[:, c * TT + tb * 128:c * TT + (tb + 1) * 128], identity=identf[:]),
                                        reads=[b_hT[c], cbuf], writes=[b_trb], signal=(cc == 3))
                                fw.op("scalar", lambda e, c4=c4, tb=tb: e.activation(
                                    out=yT[:, tb * 2048 + c4 * 512:tb * 2048 + (c4 + 1) * 512], in_=trb[:],
                                    func=AF.Copy), reads=[b_trb], writes=[b_yT[tb * 4 + c4]])
                            r0 = ti * TT + tb * 128
                            fw.dma("sync", out_d[r0:r0 + 128, :], yT[:, tb * 2048:(tb + 1) * 2048],
                                   reads=b_yT[tb * 4:(tb + 1) * 4])
                fw.barrier()

        def seq_phase(l):
            with ExitStack() as ph:
                NBUF = 2
                irep = sb(ph, "irep", [128, 512], BF16)
                causb = sb(ph, "causb", [128, 128], BF16)
                causf = sb(ph, "causf", [128, 128], F32)
                dtt = sb(ph, "dtt", [128, 1024], F32)
                xit = sb(ph, "xit", [128, 512], F32)
                ztt = sb(ph, "ztt", [128, 512], F32)
                gtt = sb(ph, "gtt", [128, 4], F32)
                pS = pst(ph, "pS", [128, 1024], F32)
                pO = pst(ph, "pO", [128, 1024], F32)
                pD = pst(ph, "pD", [128, 1024], F32)
                pD16 = pD[:].bitcast(BF16)
                b_c = Buf()
                for tdst, nm in [(irep, "c_irep"), (causb, "c_causb"), (causf, "c_causf"), (dtt, "c_dt"),
                                 (xit, "c_xi"), (ztt, "c_zt"), (gtt, "c_gt")]:
                    fw.dma("sync", tdst[:], cd[nm], writes=[b_c])
                b_pS, b_pO, b_pD = Buf(), Buf(), Buf()

                class St:
                    pass

                sts = []
                for s in range(nseq):
                    z = St()
                    z.s = s
                    z.kTs = sb(ph, "kTs", [128, S], BF16)
                    z.ikTs = sb(ph, "ikTs", [128, S], BF16)
                    z.Vs = sb(ph, "Vs", [128, S], BF16)
                    z.qTb = [sb(ph, "qTb", [128, 1024], BF16) for i in range(NBUF)]
                    z.iqTb = [sb(ph, "iqTb", [128, 1024], BF16) for i in range(NBUF)]
                    z.iwb = [sb(ph, "iwb", [128, 16], F32) for i in range(NBUF)]
                    z.rqTb = [sb(ph, "rqTb", [128, 512], BF16) for i in range(NBUF)]
                    z.rkTb = [sb(ph, "rkTb", [128, 512], BF16) for i in range(NBUF)]
                    z.rkb = [sb(ph, "rkb", [128, 512], BF16) for i in range(NBUF)]
                    z.rvb = [sb(ph, "rvb", [128, 1024], BF16) for i in range(NBUF)]
                    z.rgTb = [sb(ph, "rgTb", [128, 1024], BF16) for i in range(NBUF)]
                    z.sacc = sb(ph, "sacc", [128, S], F32)
                    z.work = sb(ph, "work", [128, S], F32)
                    z.mb = sb(ph, "mb", [128, S], BF16)
                    z.mx = sb(ph, "mx", [128, 8], F32)
                    z.rr = [sb(ph, "rr", [128, 512], F32) for i in range(2)]
                    z.ET = [sb(ph, "ET", [128, 1024], BF16) for i in range(2)]
                    z.rec = sb(ph, "rec", [128, 1024], F32)
                    z.ast = sb(ph, "ast", [128, 1024], BF16)
                    z.Aall = sb(ph, "Aall", [128, 1024], BF16)
                    z.rqxi = sb(ph, "rqxi", [128, 512], BF16)
                    z.rkz = sb(ph, "rkz", [128, 512], BF16)
                    z.Rst = sb(ph, "Rst", [128, 512], F32)
                    z.Rb = sb(ph, "Rb", [128, 512], BF16)
                    z.ocp = sb(ph, "ocp", [128, 1024], F32)
                    z.osq = sb(ph, "osq", [128, 1024], F32)
                    z.st = sb(ph, "st", [128, 64], F32)
                    z.yb = sb(ph, "yb", [128, 1024], BF16)
                    z.rst = sb(ph, "rst", [128, 1024], BF16)
                    z.pA = pst(ph, "pA", [128, 512], F32)
                    (z.b_kTs, z.b_ikTs, z.b_Vs, z.b_sacc, z.b_work, z.b_mb, z.b_mx, z.b_rec, z.b_ast, z.b_Aall,
                     z.b_rqxi, z.b_rkz, z.b_R, z.b_Rb, z.b_ocp, z.b_osq, z.b_st, z.b_yb, z.b_rst, z.b_pA) = bufs(20)
                    z.b_blk = bufs(NBUF)
                    z.b_rr, z.b_ET = bufs(2), bufs(2)
                    z.ctr = dict(rr=0, et=0)
                    sts.append(z)

                def nxt(z, k, n):
                    i = z.ctr[k] % n
                    z.ctr[k] += 1
                    return i

                def seq_setup(z):
                    ssl = slice(z.s * S, (z.s + 1) * S)
                    fw.dma("sync", z.kTs[:], kT_d[:, ssl], writes=[z.b_kTs])
                    fw.dma("sync", z.ikTs[:], ikT_d[:, ssl], writes=[z.b_ikTs])
                    fw.dma("sync", z.Vs[:].rearrange("p (b f) -> p b f", f=128),
                           v_d[ssl, :].rearrange("(b p) f -> p b f", p=128), writes=[z.b_Vs])
                    fw.op("vector", lambda e: e.memset(z.Rst[:], 0.0), writes=[z.b_R])
                    fw.op("vector", lambda e: e.memset(z.Rb[:], 0.0), writes=[z.b_Rb])

                def blk(z, j):
                    bi = j % NBUF
                    bb = z.b_blk[bi]
                    qTb, iqTb, iwb, rqTb, rkTb, rkb, rvb, rgTb = (z.qTb[bi], z.iqTb[bi], z.iwb[bi], z.rqTb[bi],
                                                                 z.rkTb[bi], z.rkb[bi], z.rvb[bi], z.rgTb[bi])
                    sacc, work, mb, mx = z.sacc, z.work, z.mb, z.mx
                    t0 = z.s * S + j * 128
                    bsl = slice(t0, t0 + 128)
                    nk = (j + 1) * 128
                    for (dst, src, w_) in [(qTb, qT_d, 128), (iqTb, iqT_d, 128), (rqTb, rqT_d, 128),
                                           (rkTb, rkT_d, 128), (rgTb, rgT_d, 128)]:
                        fw.dma("sync", dst[:].rearrange("p (h t) -> p h t", t=128),
                               src[:, bsl].rearrange("(h p) t -> p h t", p=128), writes=[bb])
                    fw.dma("sync", iwb[:], iw_d[bsl, :], writes=[bb])
                    fw.dma("sync", rkb[:], rk_d[bsl, :], writes=[bb])
                    fw.dma("sync", rvb[:], rv_d[bsl, :], writes=[bb])
                    yield
                    if j >= 2:
                        npc = (nk + 511) // 512
                        for pc in range(npc):
                            w = min(512, nk - pc * 512)
                            csl = slice(pc * 512, pc * 512 + w)
                            for h in range(16):
                                base = 64 * (h % 2)
                                pr = h // 2
                                fw.op("tensor", lambda e: e.matmul(
                                    z.pA[:, 0:w], lhsT=iqTb[base:base + 64, pr * 128:(pr + 1) * 128],
                                    rhs=z.ikTs[base:base + 64, csl], start=True, stop=True),
                                    reads=[bb, z.b_ikTs], writes=[z.b_pA])
                                ir = nxt(z, "rr", 2)
                                fw.op("scalar", lambda e: e.activation(out=z.rr[ir][:, 0:w], in_=z.pA[:, 0:w],
                                                                       func=AF.Relu),
                                      reads=[z.b_pA], writes=[z.b_rr[ir]])
                                if h == 0:
                                    fw.op("vector", lambda e: e.tensor_scalar(
                                        out=sacc[:, csl], in0=z.rr[ir][:, 0:w], scalar1=iwb[:, 0:1],
                                        scalar2=None, op0=ALU.mult),
                                        reads=[z.b_rr[ir], bb], writes=[z.b_sacc])
                                else:
                                    fw.op("vector", lambda e: e.scalar_tensor_tensor(
                                        out=sacc[:, csl], in0=z.rr[ir][:, 0:w], scalar=iwb[:, h:h + 1],
                                        in1=sacc[:, csl], op0=ALU.mult, op1=ALU.add),
                                        reads=[z.b_rr[ir], bb, z.b_sacc], writes=[z.b_sacc])
                                yield
                        dsl = slice(j * 128, (j + 1) * 128)
                        fw.op("vector", lambda e: e.tensor_tensor(out=sacc[:, dsl], in0=sacc[:, dsl],
                                                                  in1=causf[:], op=ALU.add),
                              reads=[z.b_sacc, b_c], writes=[z.b_sacc])
                        yield
                        src, bsrc = sacc, z.b_sacc
                        for r in range(32):
                            fw.op("vector", lambda e: e.max(out=mx[:], in_=src[:, 0:nk]),
                                  reads=[bsrc], writes=[z.b_mx])
                            yield
                            if r < 31:
                                fw.op("vector", lambda e: e.match_replace(
                                    out=work[:, 0:nk], in_to_replace=mx[:], in_values=src[:, 0:nk],
                                    imm_value=NEG), reads=[bsrc, z.b_mx], writes=[z.b_work])
                                src, bsrc = work, z.b_work
                                yield
                        fw.op("vector", lambda e: e.tensor_scalar(
                            out=mb[:, 0:nk], in0=sacc[:, 0:nk], scalar1=mx[:, 7:8], scalar2=-30000.0,
                            op0=ALU.is_lt, op1=ALU.mult), reads=[z.b_sacc, z.b_mx], writes=[z.b_mb])
                    else:
                        if j == 1:
                            fw.op("vector", lambda e: e.memset(mb[:, 0:128], 0.0), writes=[z.b_mb])
                        dsl = slice(j * 128, (j + 1) * 128)
                        fw.op("vector", lambda e: e.tensor_copy(out=mb[:, dsl], in_=causb[:]),
                              reads=[b_c], writes=[z.b_mb])
                    yield
                    for kc in range(j + 1):
                        ksl = slice(kc * 128, (kc + 1) * 128)
                        for half in range(2):
                            hs = slice(half * 512, (half + 1) * 512)
                            fw.op("tensor", lambda e: e.matmul(
                                pS[:, hs], lhsT=z.kTs[:, ksl], rhs=qTb[:, hs], start=True, stop=False),
                                reads=[z.b_kTs, bb], writes=[b_pS], signal=False)
                            fw.op("tensor", lambda e: e.matmul(
                                pS[:, hs], lhsT=mb[:, ksl], rhs=irep[:], start=False, stop=True),
                                reads=[z.b_mb, b_c], writes=[b_pS], signal=(half == 1))
                        ie = nxt(z, "et", 2)
                        fw.op("scalar", lambda e: e.activation(out=z.ET[ie][:], in_=pS[:], func=AF.Exp),
                              reads=[b_pS], writes=[z.b_ET[ie]])
                        for half in range(2):
                            hs = slice(half * 512, (half + 1) * 512)
                            fw.op("tensor", lambda e: e.matmul(
                                pO[:, hs], lhsT=z.Vs[:, ksl], rhs=z.ET[ie][:, hs], start=(kc == 0), stop=(kc == j)),
                                reads=[z.b_Vs, z.b_ET[ie]], writes=[b_pO], signal=False)
                            fw.op("tensor", lambda e: e.matmul(
                                pD[:, hs], lhsT=onesb[:], rhs=z.ET[ie][:, hs], start=(kc == 0), stop=(kc == j)),
                                reads=[cbuf, z.b_ET[ie]], writes=[b_pD], signal=(half == 1))
                    fw.op("vector", lambda e: e.reciprocal(out=z.rec[:], in_=pD[:]), reads=[b_pD], writes=[z.b_rec])
                    fw.op("vector", lambda e: e.tensor_tensor(out=z.ast[:], in0=pO[:], in1=z.rec[:], op=ALU.mult),
                          reads=[b_pO, z.b_rec], writes=[z.b_ast])
                    fw.dma("sync", mixT_d[0:1024, bsl].rearrange("(h p) t -> p h t", p=128),
                           z.ast[:].rearrange("p (h t) -> p h t", t=128), reads=[z.b_ast])
                    yield
                    for h in range(8):
                        base = 64 * (h % 2)
                        c = h // 2
                        fw.op("tensor", lambda e: e.matmul(
                            pD[:, (h % 2) * 512 + c * 128:(h % 2) * 512 + (c + 1) * 128],
                            lhsT=rkTb[base:base + 64, c * 128:(c + 1) * 128],
                            rhs=rqTb[base:base + 64, c * 128:(c + 1) * 128], start=True, stop=True),
                            reads=[bb], writes=[b_pD], signal=(h == 7))
                    fw.op("vector", lambda e: e.tensor_tensor(out=z.Aall[:], in0=pD[:], in1=dtt[:], op=ALU.mult),
                          reads=[b_pD, b_c], writes=[z.b_Aall])
                    fw.op("gpsimd", lambda e: e.tensor_tensor(out=z.rqxi[:], in0=rqTb[:], in1=xit[:], op=ALU.mult),
                          reads=[bb, b_c], writes=[z.b_rqxi])
                    fw.op("gpsimd", lambda e: e.tensor_tensor(out=z.rkz[:], in0=rkb[:], in1=ztt[:], op=ALU.mult),
                          reads=[bb, b_c], writes=[z.b_rkz])
                    yield
                    for h in range(8):
                        base = 64 * (h % 2)
                        c = h // 2
                        hsl = slice(h * 128, (h + 1) * 128)
                        asl = slice((h % 2) * 512 + c * 128, (h % 2) * 512 + (c + 1) * 128)
                        fw.op("tensor", lambda e: e.matmul(
                            pS[:, hsl], lhsT=z.Aall[:, asl], rhs=rvb[:, hsl], start=True, stop=False),
                            reads=[z.b_Aall, bb], writes=[b_pS], signal=False)
                        fw.op("tensor", lambda e: e.matmul(
                            pS[:, hsl], lhsT=z.rqxi[base:base + 64, c * 128:(c + 1) * 128],
                            rhs=z.Rb[base:base + 64, c * 128:(c + 1) * 128], start=False, stop=True),
                            reads=[z.b_rqxi, z.b_Rb], writes=[b_pS], signal=(h == 7))
                    fw.op("scalar", lambda e: e.activation(out=z.ocp[:], in_=pS[:], func=AF.Copy),
                          reads=[b_pS], writes=[z.b_ocp])
                    fw.op("scalar", lambda e: e.activation(out=z.osq[:], in_=pS[:], func=AF.Square),
                          reads=[b_pS], writes=[z.b_osq])
                    for c in range(4):
                        fw.op("tensor", lambda e: e.matmul(
                            pO[:, c * 256:(c + 1) * 256], lhsT=z.rkz[:, c * 128:(c + 1) * 128],
                            rhs=rvb[:, c * 256:(c + 1) * 256], start=True, stop=True),
                            reads=[z.b_rkz, bb], writes=[b_pO], signal=(c == 3))
                    for c in range(4):
                        for hh in range(2):
                            ps_ = slice(hh * 64, (hh + 1) * 64)
                            fw.op("vector", lambda e: e.scalar_tensor_tensor(
                                out=z.Rst[ps_, c * 128:(c + 1) * 128], in0=z.Rst[ps_, c * 128:(c + 1) * 128],
                                scalar=gtt[ps_, c:c + 1],
                                in1=pO[ps_, c * 256 + hh * 128:c * 256 + (hh + 1) * 128],
                                op0=ALU.mult, op1=ALU.add),
                                reads=[z.b_R, b_pO, b_c], writes=[z.b_R])
                    fw.op("vector", lambda e: e.tensor_copy(out=z.Rb[:], in_=z.Rst[:]), reads=[z.b_R], writes=[z.b_Rb])
                    yield
                    st = z.st
                    fw.op("vector", lambda e: e.tensor_reduce(
                        out=st[:, 0:8], in_=z.ocp[:].rearrange("p (h f) -> p h f", f=128), axis=AX.X, op=ALU.add),
                        reads=[z.b_ocp], writes=[z.b_st])
                    fw.op("vector", lambda e: e.tensor_reduce(
                        out=st[:, 8:16], in_=z.osq[:].rearrange("p (h f) -> p h f", f=128), axis=AX.X, op=ALU.add),
                        reads=[z.b_osq], writes=[z.b_st])
                    yield
                    fw.op("vector", lambda e: e.tensor_scalar(out=st[:, 16:24], in0=st[:, 0:8],
                                                              scalar1=1.0 / 128, scalar2=None, op0=ALU.mult),
                          reads=[z.b_st], writes=[z.b_st])
                    yield
                    fw.op("vector", lambda e: e.tensor_tensor(out=st[:, 24:32], in0=st[:, 16:24],
                                                              in1=st[:, 16:24], op=ALU.mult),
                          reads=[z.b_st], writes=[z.b_st])
                    yield
                    fw.op("vector", lambda e: e.scalar_tensor_tensor(
                        out=st[:, 32:40], in0=st[:, 8:16], scalar=1.0 / 128, in1=st[:, 24:32],
                        op0=ALU.mult, op1=ALU.subtract), reads=[z.b_st], writes=[z.b_st])
                    fw.op("scalar", lambda e: e.activation(out=st[:, 40:48], in_=st[:, 32:40], func=AF.Sqrt,
                                                           bias=epsb[:, 0:1]),
                          reads=[z.b_st, cbuf], writes=[z.b_st])
                    yield
                    fw.op("vector", lambda e: e.reciprocal(out=st[:, 40:48], in_=st[:, 40:48]),
                          reads=[z.b_st], writes=[z.b_st])
                    yield
                    fw.op("vector", lambda e: e.scalar_tensor_tensor(
                        out=st[:, 48:56], in0=st[:, 16:24], scalar=-1.0, in1=st[:, 40:48],
                        op0=ALU.mult, op1=ALU.mult), reads=[z.b_st], writes=[z.b_st])
                    for h in range(8):
                        hsl = slice(h * 128, (h + 1) * 128)
                        fw.op("gpsimd", lambda e: e.tensor_scalar(
                            out=z.yb[:, hsl], in0=z.ocp[:, hsl], scalar1=st[:, 40 + h:41 + h],
                            scalar2=st[:, 48 + h:49 + h], op0=ALU.mult, op1=ALU.add),
                            reads=[z.b_ocp, z.b_st], writes=[z.b_yb])
                    yield
                    for h in range(8):
                        hsl = slice(h * 128, (h + 1) * 128)
                        fw.op("tensor", lambda e: e.transpose(out=pD16[:, hsl], in_=z.yb[:, hsl],
                                                              identity=identb[:]),
                              reads=[z.b_yb, cbuf], writes=[b_pD], signal=(h == 7))
                    for h in range(8):
                        hsl = slice(h * 128, (h + 1) * 128)
                        fw.op("vector", lambda e: e.scalar_tensor_tensor(
                            out=z.rst[:, hsl], in0=pD16[:, hsl], scalar=gn_t[:, l * 8 + h:l * 8 + h + 1],
                            in1=rgTb[:, hsl], op0=ALU.mult, op1=ALU.mult),
                            reads=[b_pD, bb, cbuf], writes=[z.b_rst])
                    fw.dma("sync", mixT_d[1024:2048, bsl].rearrange("(h p) t -> p h t", p=128),
                           z.rst[:].rearrange("p (h t) -> p h t", t=128), reads=[z.b_rst])
                    yield

                for z in sts:
                    seq_setup(z)
                for j in range(nblk):
                    gens = [blk(z, j) for z in sts]
                    while gens:
                        for g in list(gens):
                            try:
                                next(g)
                            except StopIteration:
                                gens.remove(g)
                fw.barrier()


        if only_seq:
            seq_phase(0)
            fw.barrier()
            return nc
        dense_phase(None, 0)
        if stop_after == "p1":
            return nc
        for l in range(nlayers):
            if l + 1 < nlayers:
                conv_layer(l + 1)
            seq_phase(l)
            if stop_after == f"p2_{l}":
                return nc
            dense_phase(l, l + 1 if l + 1 < nlayers else None)
        fw.barrier()
    return nc


_NC_CACHE = {}


def _prep_core_inputs(inputs, c, nseq, consts):
    b0 = c * nseq
    m = {}
    m["x"] = np.ascontiguousarray(inputs["x"][b0:b0 + nseq]).reshape(nseq * S, D)
    m["p"] = np.ascontiguousarray(inputs["p"][:, b0:b0 + nseq]).reshape(DEPTH, nseq * S, 256)
    m["pos"] = np.ascontiguousarray(inputs["positions"][b0:b0 + nseq]).reshape(1, nseq * S).astype(np.int32)
    return m


def _shared_inputs(inputs):
    m = {}
    for k_, n_ in [("w_in", "w_in"), ("w_out", "w_out"), ("w_ff1", "w_ff1"), ("w_ff2", "w_ff2"),
                   ("w_ple", "w_ple"), ("w_ple_gate", "w_gate")]:
        m[n_] = np.ascontiguousarray(np.asarray(inputs[k_], dtype=np.float32))
    for k_, n_ in [("pre_mix_norm", "g_premix"), ("post_mix_norm", "g_postmix"), ("pre_ff_norm", "g_preff"),
                   ("post_ff_norm", "g_postff"), ("ple_norm", "g_ple")]:
        g = np.asarray(inputs[k_], dtype=np.float32)
        m[n_] = np.ascontiguousarray(g.reshape(DEPTH, 16, 128).transpose(0, 2, 1))
    g = np.asarray(inputs["ret_gn"], dtype=np.float32)
    m["g_gn"] = np.ascontiguousarray(g.reshape(DEPTH, 8, 128).transpose(0, 2, 1))
    m.update(_consts())
    return m


def kernel(**inputs):
    inputs = {k: np.asarray(v) for k, v in inputs.items()}
    B = inputs["x"].shape[0]
    ncores = 8
    nseq = B // ncores
    if "nc" not in _NC_CACHE:
        _NC_CACHE["nc"] = build(nseq=nseq)
    nc = _NC_CACHE["nc"]
    shared = _shared_inputs(inputs)
    in_maps = []
    for c in range(ncores):
        m = dict(shared)
        m.update(_prep_core_inputs(inputs, c, nseq, None))
        in_maps.append(m)
    res = run_bass_kernel_spmd(nc, in_maps, core_ids=list(range(ncores)))
    outs = [np.asarray(r["out"]).reshape(nseq, S, D) for r in res.results]
    return np.concatenate(outs, axis=0).astype(np.float32)
```

```python
import numpy as np
import ml_dtypes
from contextlib import ExitStack
import concourse.bass as bass
import concourse.mybir as mybir
from concourse.bass_utils import run_bass_kernel_spmd

F32 = mybir.dt.float32
BF16 = mybir.dt.bfloat16
I32 = mybir.dt.int32
AF = mybir.ActivationFunctionType
ALU = mybir.AluOpType
AX = mybir.AxisListType

D = 2048
S = 2048
DEPTH = 2
KC = 16
TT = 512
NB = S // 128
AQ, AK, AV, IQ, IK, IW, RQ, RK, RV, RG, INW = 0, 1024, 1152, 1280, 2304, 2368, 2384, 2896, 3408, 4432, 5456
IN_GROUPS = [
    [(AQ, 512, 0)], [(AQ + 512, 512, 0)],
    [(AK, 128, 0), (IK, 64, 128), (IK, 64, 192), (AV, 128, 256), (IW, 16, 384), (AV, 112, 400)],
    [(IQ, 512, 0)], [(IQ + 512, 512, 0)],
    [(RQ, 512, 0)], [(RK, 512, 0)], [(RV, 512, 0)], [(RV + 512, 512, 0)],
    [(RG, 512, 0)], [(RG + 512, 512, 0)],
]
NEG = -1.0e30
SEM_LIMIT = 60000
KBIS = 22


class Buf:
    __slots__ = ("w", "r")

    def __init__(self):
        self.w = None
        self.r = {}


def bufs(n):
    return [Buf() for _ in range(n)]


class FW:
    def __init__(self, nc, es):
        self.nc = nc
        self.es = es
        self.nsem = 0
        self.E = {}
        for name in ["tensor", "vector", "scalar", "gpsimd", "sync"]:
            self.E[name] = dict(h=getattr(nc, name), sem=self._sem(), count=0, seen={},
                                selfsync=(name != "tensor"), epoch=0, last=None)
        self.pool = {q: [dict(sem=self._sem(), val=0) for _ in range(n)]
                     for q, n in [("sync", 24), ("gpsimd", 40), ("scalar", 2)]}
        self.rr = {q: 0 for q in self.pool}
        self.log = {n: [] for n in self.E}

    def _sem(self):
        self.nsem += 1
        return self.es.enter_context(self.nc.semaphore(f"sm{self.nsem}"))

    def _deps(self, reads, writes):
        d = []
        for b in reads:
            if b.w is not None:
                d.append(b.w)
        for b in writes:
            if b.w is not None:
                d.append(b.w)
            d.extend(b.r.values())
        return d

    def _wait(self, ename, deps):
        E = self.E[ename]
        need = {}
        for (key, sem, val, owner) in deps:
            if owner == ename and not E["selfsync"]:
                continue
            if E["seen"].get(key, 0) >= val:
                continue
            if key not in need or need[key][1] < val:
                need[key] = (sem, val)
        for key, (sem, val) in need.items():
            E["h"].wait_ge(sem, val)
            self.log[ename].append(("wait", id(sem), val))
            E["seen"][key] = val

    def _record(self, tok, reads, writes):
        for b in reads:
            o = b.r.get(tok[0])
            if o is None or o[2] < tok[2]:
                b.r[tok[0]] = tok
        for b in writes:
            b.w = tok
            b.r = {}

    def op(self, ename, fn, reads=(), writes=(), signal=True):
        E = self.E[ename]
        self._wait(ename, self._deps(reads, writes))
        if E["count"] >= SEM_LIMIT:
            E["sem"] = self._sem()
            E["count"] = 0
            E["epoch"] += 1
        ins = fn(E["h"])
        key = (ename, E["epoch"])
        if signal:
            E["count"] += 1
            ins.then_inc(E["sem"], 1)
            self.log[ename].append(("inc", id(E["sem"]), 1))
            tok = (key, E["sem"], E["count"], ename)
            E["last"] = tok
        else:
            tok = (key, E["sem"], E["count"] + 1, ename)
        self._record(tok, reads, writes)
        return ins

    def dma(self, q, out, in_, reads=(), writes=(), **kw):
        E = self.E[q]
        pool = self.pool[q]
        i = self.rr[q]
        self.rr[q] = (i + 1) % len(pool)
        slot = pool[i]
        if slot["val"] + 16 > SEM_LIMIT:
            slot["sem"] = self._sem()
            slot["val"] = 0
        deps = self._deps(reads, writes)
        key = ("dma", id(slot["sem"]))
        if slot["val"] > 0:
            deps.append((key, slot["sem"], slot["val"], "dma"))
        self._wait(q, deps)
        ins = E["h"].dma_start(out=out, in_=in_, **kw)
        slot["val"] += 16
        ins.then_inc(slot["sem"], 16)
        self.log[q].append(("inc", id(slot["sem"]), 16))
        tok = (key, slot["sem"], slot["val"], "dma")
        self._record(tok, reads, writes)
        return ins

    def all_tokens(self):
        toks = []
        for n, E in self.E.items():
            if E["last"] is not None:
                toks.append(E["last"])
        for q, pool in self.pool.items():
            for s in pool:
                if s["val"] > 0:
                    toks.append((("dma", id(s["sem"])), s["sem"], s["val"], "dma"))
        return toks

    def barrier(self, engines=None):
        toks = self.all_tokens()
        for n in (engines or list(self.E.keys())):
            E = self.E[n]
            ss = E["selfsync"]
            E["selfsync"] = True
            self._wait(n, toks)
            E["selfsync"] = ss


def _consts():
    c = {}
    c["c_identf"] = np.eye(128, dtype=np.float32)
    c["c_identb"] = np.eye(128, dtype=np.float32).astype(ml_dtypes.bfloat16)
    c["c_irep"] = np.tile(np.eye(128, dtype=np.float32), (1, 4)).astype(ml_dtypes.bfloat16)
    pm = np.zeros((3, 128, 128), np.float32)
    invf = np.zeros((128, 3), np.float64)
    for i in range(16):
        pm[0, i + 16, i] = -1.0
        pm[0, i, i + 16] = 1.0
        invf[i, 0] = invf[i + 16, 0] = 500000.0 ** (-i / 16.0)
    for o in (0, 64):
        for i in range(8):
            pm[1, o + i + 8, o + i] = -1.0
            pm[1, o + i, o + i + 8] = 1.0
            invf[o + i, 1] = invf[o + i + 8, 1] = 500000.0 ** (-i / 8.0)
    for o in (0, 64):
        for i in range(32):
            pm[2, o + i + 32, o + i] = -1.0
            pm[2, o + i, o + i + 32] = 1.0
            invf[o + i, 2] = invf[o + i + 32, 2] = 10000.0 ** (-i / 32.0)
    c["c_pm"] = pm.astype(ml_dtypes.bfloat16)
    c["c_invf"] = (invf.astype(np.float32).astype(np.float64) / (2 * np.pi)).astype(np.float32)
    q = np.arange(128)[:, None]
    k = np.arange(128)[None, :]
    c["c_causb"] = np.where(k <= q, 0.0, -30000.0).astype(np.float32).astype(ml_dtypes.bfloat16)
    c["c_causf"] = np.where(k <= q, 0.0, NEG).astype(np.float32)
    H = 8
    C = 128
    gamma = (1.0 - 2.0 ** (-5.0 - np.arange(H, dtype=np.float32))).astype(np.float32)
    log_g = np.log(gamma).astype(np.float32)
    i = np.arange(C, dtype=np.float32)
    dt = np.zeros((128, H, 128), np.float32)
    for h in range(H):
        diff = i[None, :] - i[:, None]
        dt[:, h, :] = np.where(diff >= 0, np.exp(np.maximum(diff, 0.0) * log_g[h]), 0.0)
    c["c_dt"] = np.ascontiguousarray(dt.reshape(128, 4, 2, 128).transpose(0, 2, 1, 3)).reshape(128, 1024).astype(np.float32)
    zeta = np.exp((C - 1.0 - i)[None, :] * log_g[:, None]).astype(np.float32)
    xi = np.exp((i + 1.0)[None, :] * log_g[:, None]).astype(np.float32)
    gch = np.exp(C * log_g).astype(np.float32)
    xit = np.zeros((128, 4, 128), np.float32)
    gt = np.zeros((128, 4), np.float32)
    for cc in range(4):
        for hh in range(2):
            xit[hh * 64:(hh + 1) * 64, cc, :] = xi[2 * cc + hh][None, :]
            gt[hh * 64:(hh + 1) * 64, cc] = gch[2 * cc + hh]
    c["c_xi"] = xit.reshape(128, 512)
    c["c_gt"] = gt
    zt = np.zeros((128, H, 64), np.float32)
    for h in range(H):
        zt[:, h, :] = zeta[h][:, None]
    c["c_zt"] = zt.reshape(128, 512)
    c["c_bis"] = np.tile((2.0 ** -(np.arange(KBIS, dtype=np.float32) + 2.0))[None, :], (128, 1)).astype(np.float32)
    return c


CONST_SPECS = {
    "c_identf": ([128, 128], F32), "c_identb": ([128, 128], BF16), "c_irep": ([128, 512], BF16),
    "c_pm": ([3, 128, 128], BF16), "c_invf": ([128, 3], F32), "c_causb": ([128, 128], BF16),
    "c_causf": ([128, 128], F32), "c_dt": ([128, 1024], F32), "c_xi": ([128, 512], F32),
    "c_gt": ([128, 4], F32), "c_zt": ([128, 512], F32), "c_bis": ([128, KBIS], F32),
}
GAIN_NAMES = ["g_premix", "g_postmix", "g_preff", "g_postff", "g_ple"]


def build(nseq=2, dbg=False, stop_after=None, nlayers=DEPTH, only_seq=False, nblk=NB, parts=("idx", "attn", "ret"), retlvl=9):
    NT = nseq * S
    NTILE = NT // TT
    nc = bass.Bass("TRN2", target_bir_lowering=False)
    kin = "ExternalInput"
    ksc = "ExternalOutput" if dbg else "Internal"

    def dram(name, shape, dt, kind):
        return nc.dram_tensor(name, shape, dt, kind=kind).ap()

    kin_ = kin
    if only_seq:
        kin = "Internal"
    x_d = dram("x", [NT, D], F32, kin)
    p_d = dram("p", [DEPTH, NT, 256], F32, kin)
    pos_d = dram("pos", [1, NT], I32, kin)
    w_in_d = dram("w_in", [DEPTH, D, INW], F32, kin)
    w_out_d = dram("w_out", [DEPTH, D, D], F32, kin)
    w_ff1_d = dram("w_ff1", [DEPTH, D, 4 * D], F32, kin)
    w_ff2_d = dram("w_ff2", [DEPTH, 4 * D, D], F32, kin)
    w_ple_d = dram("w_ple", [DEPTH, 256, D], F32, kin)
    w_gate_d = dram("w_gate", [DEPTH, D, D], F32, kin)
    kin = kin_
    gains_d = {n: dram(n, [DEPTH, 128, 16], F32, kin) for n in GAIN_NAMES}
    gn_d = dram("g_gn", [DEPTH, 128, 8], F32, kin)
    cd = {n: dram(n, sh, dt, kin) for n, (sh, dt) in CONST_SPECS.items()}
    out_d = dram("out", [NT, D], F32, "ExternalOutput")

    wb_in = dram("wb_in", [DEPTH, 11, 128, 8192], BF16, "Internal")
    wb_out = dram("wb_out", [DEPTH, 4, 128, 8192], BF16, "Internal")
    wb_ff1 = dram("wb_ff1", [DEPTH, 16, 128, 8192], BF16, "Internal")
    wb_ff2 = dram("wb_ff2", [DEPTH, 16, 128, 8192], BF16, "Internal")
    wb_gate = dram("wb_gate", [DEPTH, 4, 128, 8192], BF16, "Internal")
    wb_ple = dram("wb_ple", [DEPTH, 4, 128, 1024], BF16, "Internal")
    ksi = kin if only_seq else ksc
    hT_d = dram("hT", [D, NT], F32, ksc)
    tabs_d = dram("tabs", [6, 128, NT], F32, ksc)
    qT_d = dram("qT", [1024, NT], BF16, ksi)
    kT_d = dram("kT", [128, NT], BF16, ksi)
    ikT_d = dram("ikT", [128, NT], BF16, ksi)
    iqT_d = dram("iqT", [1024, NT], BF16, ksi)
    rqT_d = dram("rqT", [512, NT], BF16, ksi)
    rkT_d = dram("rkT", [512, NT], BF16, ksi)
    rgT_d = dram("rgT", [1024, NT], BF16, ksi)
    v_d = dram("v", [NT, 128], BF16, ksi)
    iw_d = dram("iw", [NT, 16], F32, ksi)
    rk_d = dram("rk", [NT, 512], BF16, ksi)
    rv_d = dram("rv", [NT, 1024], BF16, ksi)
    mixT_d = dram("mixT", [D, NT], BF16, ksc)

    with ExitStack() as es:
        fw = FW(nc, es)
        nc._fw = fw

        uid = [0]

        def sb(ctx, name, shape, dt):
            uid[0] += 1
            return ctx.enter_context(nc.sbuf_tensor(f"s{uid[0]}_{name}", shape, dt))

        def pst(ctx, name, shape, dt):
            uid[0] += 1
            return ctx.enter_context(nc.psum_tensor(f"p{uid[0]}_{name}", shape, dt))

        identf = sb(es, "identf", [128, 128], F32)
        identb = sb(es, "identb", [128, 128], BF16)
        onesb = sb(es, "onesb", [128, 128], BF16)
        gains = {n: sb(es, "t_" + n, [128, DEPTH * 16], F32) for n in GAIN_NAMES}
        gn_t = sb(es, "t_gn", [128, DEPTH * 8], F32)
        cbuf = Buf()
        fw.dma("sync", identf[:], cd["c_identf"], writes=[cbuf])
        fw.dma("sync", identb[:], cd["c_identb"], writes=[cbuf])
        for n in GAIN_NAMES:
            for l in range(DEPTH):
                fw.dma("sync", gains[n][:, l * 16:(l + 1) * 16], gains_d[n][l], writes=[cbuf])
        for l in range(DEPTH):
            fw.dma("sync", gn_t[:, l * 8:(l + 1) * 8], gn_d[l], writes=[cbuf])
        fw.op("vector", lambda e: e.memset(onesb[:], 1.0), writes=[cbuf])

        wbuf = {}

        def conv(key, dst, src):
            b = wbuf.setdefault(key, Buf())
            fw.dma("gpsimd", dst, src, writes=[b])

        def conv_layer(l):
            for g, pieces in enumerate(IN_GROUPS):
                for (s0, wd, d0) in pieces:
                    dst = wb_in[l, g].rearrange("p (k c) -> p k c", c=512)[:, :, d0:d0 + wd]
                    src = w_in_d[l][:, s0:s0 + wd].rearrange("(k p) c -> p k c", p=128)
                    conv(("in", l, g), dst, src)
            for g in range(4):
                dst = wb_out[l, g].rearrange("p (k c) -> p k c", c=512)
                src = w_out_d[l][:, g * 512:(g + 1) * 512].rearrange("(k p) c -> p k c", p=128)
                conv(("out", l, g), dst, src)
            for g in range(16):
                dst = wb_ff1[l, g].rearrange("p (k c) -> p k c", c=512)
                src = w_ff1_d[l][:, g * 512:(g + 1) * 512].rearrange("(k p) c -> p k c", p=128)
                conv(("ff1", l, g), dst, src)
            for q in range(4):
                for og in range(4):
                    dst = wb_ff2[l, q * 4 + og].rearrange("p (k c) -> p k c", c=512)
                    src = w_ff2_d[l][q * 2048:(q + 1) * 2048, og * 512:(og + 1) * 512].rearrange(
                        "(k p) c -> p k c", p=128)
                    conv(("ff2", l, q * 4 + og), dst, src)
            for g in range(4):
                dst = wb_ple[l, g].rearrange("p (k c) -> p k c", c=512)
                src = w_ple_d[l][:, g * 512:(g + 1) * 512].rearrange("(k p) c -> p k c", p=128)
                conv(("ple", l, g), dst, src)
                dst = wb_gate[l, g].rearrange("p (k c) -> p k c", c=512)
                src = w_gate_d[l][:, g * 512:(g + 1) * 512].rearrange("(k p) c -> p k c", p=128)
                conv(("gate", l, g), dst, src)

        if not only_seq:
            conv_layer(0)

        def tables_phase():
            with ExitStack() as ph:
                invf = sb(ph, "invf", [128, 3], F32)
                posi = sb(ph, "posi", [128, S], I32)
                posf = sb(ph, "posf", [128, S], F32)
                ys = sb(ph, "ys", [128, S], F32)
                ki = sb(ph, "ki", [128, S], I32)
                kf = sb(ph, "kf", [128, S], F32)
                fr = sb(ph, "fr", [128, S], F32)
                tb = [sb(ph, f"tb{i}", [128, S], F32) for i in range(2)]
                b_invf, b_posi, b_posf, b_ys, b_ki, b_kf, b_fr = bufs(7)
                b_tb = bufs(2)
                fw.dma("sync", invf[:], cd["c_invf"], writes=[b_invf])
                n = 0
                for s in range(nseq):
                    fw.dma("sync", posi[:], pos_d[0:1, s * S:(s + 1) * S].partition_broadcast(128),
                           writes=[b_posi])
                    fw.op("vector", lambda e: e.tensor_copy(out=posf[:], in_=posi[:]),
                          reads=[b_posi], writes=[b_posf])
                    for t in range(3):
                        for cs in range(2):
                            if cs == 0:
                                fw.op("vector", lambda e, t=t: e.tensor_scalar(
                                    out=ys[:], in0=posf[:], scalar1=invf[:, t:t + 1], scalar2=0.25,
                                    op0=ALU.mult, op1=ALU.add), reads=[b_posf, b_invf], writes=[b_ys])
                            else:
                                fw.op("vector", lambda e, t=t: e.tensor_scalar(
                                    out=ys[:], in0=posf[:], scalar1=invf[:, t:t + 1], scalar2=None,
                                    op0=ALU.mult), reads=[b_posf, b_invf], writes=[b_ys])
                            fw.op("vector", lambda e: e.tensor_copy(out=ki[:], in_=ys[:]),
                                  reads=[b_ys], writes=[b_ki])
                            fw.op("vector", lambda e: e.tensor_copy(out=kf[:], in_=ki[:]),
                                  reads=[b_ki], writes=[b_kf])
                            fw.op("vector", lambda e: e.tensor_tensor(out=fr[:], in0=ys[:], in1=kf[:],
                                                                      op=ALU.subtract),
                                  reads=[b_ys, b_kf], writes=[b_fr])
                            fw.op("vector", lambda e: e.tensor_scalar(out=kf[:], in0=fr[:], scalar1=0.0,
                                                                      scalar2=None, op0=ALU.is_lt),
                                  reads=[b_fr], writes=[b_kf])
                            fw.op("vector", lambda e: e.tensor_tensor(out=fr[:], in0=fr[:], in1=kf[:],
                                                                      op=ALU.add),
                                  reads=[b_fr, b_kf], writes=[b_fr])
                            tt = tb[n % 2]
                            bt = b_tb[n % 2]
                            n += 1
                            fw.op("scalar", lambda e, tt=tt: e.activation(
                                out=tt[:], in_=fr[:], func=AF.Sin, scale=-2.0 * np.pi, bias=pib[:, 0:1]),
                                reads=[b_fr, cbuf], writes=[bt])
                            fw.dma("sync", tabs_d[2 * t + cs][:, s * S:(s + 1) * S], tt[:], reads=[bt])
                fw.barrier()

        pib = sb(es, "pib", [128, 2], F32)
        fw.op("vector", lambda e: e.memset(pib[:, 0:1], float(np.pi)), writes=[cbuf])
        fw.op("vector", lambda e: e.memset(pib[:, 1:2], 1e-6), writes=[cbuf])
        epsb = sb(es, "epsb", [128, 1], F32)
        fw.op("vector", lambda e: e.memset(epsb[:], 1e-5), writes=[cbuf])
        if only_seq:
            seq_phase_holder = []
        else:
            tables_phase()
        if stop_after == "tables":
            fw.barrier()
            return nc

        hbuf = bufs(NTILE)

        def dense_phase(l_prev, l_next):
            with ExitStack() as ph:
                hT = sb(ph, "hT", [128, 16 * TT], F32)
                yT = sb(ph, "yT", [128, 16 * TT], F32)
                act = sb(ph, "act", [128, 16 * TT], BF16)
                uT = sb(ph, "uT", [128, 16 * TT], BF16)
                NW = 3
                Wt = [sb(ph, f"Wt{i}", [128, 8192], BF16) for i in range(NW)]
                Wp = [sb(ph, f"Wp{i}", [128, 1024], BF16) for i in range(2)]
                tabs = sb(ph, "tabs_t", [128, 6 * TT], F32)
                rstd = sb(ph, "rstd", [128, TT], F32)
                sqr = [sb(ph, f"sqr{i}", [128, TT], BF16) for i in range(2)]
                tmp = [sb(ph, f"tmp{i}", [128, TT], F32) for i in range(4)]
                qb = [sb(ph, f"qb{i}", [128, TT], BF16) for i in range(2)]
                stg = [sb(ph, f"stg{i}", [128, TT], BF16) for i in range(3)]
                tok = [sb(ph, f"tok{i}", [128, 512], BF16) for i in range(3)]
                iwst = sb(ph, "iwst", [128, 64], F32)
                pin = sb(ph, "pin", [128, 1024], F32)
                pbt = sb(ph, "pbt", [128, 1024], BF16)
                pT = sb(ph, "pT", [128, 1024], BF16)
                pm = sb(ph, "pm", [128, 3 * 128], BF16)
                acc = [pst(ph, f"acc{i}", [128, 512], F32) for i in range(4)]
                ssb = pst(ph, "ssb", [128, 512], F32)
                ppb = [pst(ph, f"ppb{i}", [128, 512], F32) for i in range(2)]
                trb = pst(ph, "trb", [128, 512], F32)
                trb16 = trb[:].bitcast(BF16)

                b_hT, b_yT, b_act = bufs(16), bufs(16), bufs(16)
                b_uT = bufs(16)
                b_W, b_Wp = bufs(NW), bufs(2)
                b_tabs, b_rstd, b_ss, b_trb, b_pin, b_pbt, b_pT, b_pm, b_iwst = bufs(9)
                b_sqr, b_tmp, b_qb, b_stg, b_tok = bufs(2), bufs(4), bufs(2), bufs(3), bufs(3)
                b_acc, b_pp = bufs(4), bufs(2)
                ctr = dict(w=0, acc=0, sqr=0, tmp=0, qb=0, stg=0, tok=0, pp=0, wp=0)

                def nxt(k, n):
                    i = ctr[k] % n
                    ctr[k] += 1
                    return i

                for t in range(3):
                    fw.dma("sync", pm[:, t * 128:(t + 1) * 128], cd["c_pm"][t], writes=[b_pm])

                def hc(c):
                    return hT[:, c * TT:(c + 1) * TT]

                def yc(c):
                    return yT[:, c * TT:(c + 1) * TT]

                def ac(c):
                    return act[:, c * TT:(c + 1) * TT]

                def uc(c):
                    return uT[:, c * TT:(c + 1) * TT]

                plan = []
                for ti_p in range(NTILE):
                    if l_prev is not None:
                        lp = l_prev
                        plan += [(wb_out[lp, og], ("out", lp, og)) for og in range(4)]
                        for q in range(4):
                            plan += [(wb_ff1[lp, q * 4 + g1], ("ff1", lp, q * 4 + g1)) for g1 in range(4)]
                            plan += [(wb_ff2[lp, q * 4 + og], ("ff2", lp, q * 4 + og)) for og in range(4)]
                        plan += [(wb_gate[lp, og], ("gate", lp, og)) for og in range(4)]
                    if l_next is not None:
                        plan += [(wb_in[l_next, g], ("in", l_next, g)) for g in range(11)]
                wstate = dict(issue=0, use=0)

                def wload(src, key):
                    k = wstate["use"]
                    assert plan[k][1] == key, (plan[k][1], key)
                    while wstate["issue"] < min(len(plan), k + NW):
                        ki = wstate["issue"]
                        fw.dma("sync", Wt[ki % NW][:], plan[ki][0], reads=[wbuf[plan[ki][1]]],
                               writes=[b_W[ki % NW]])
                        wstate["issue"] += 1
                    wstate["use"] += 1
                    return Wt[k % NW], b_W[k % NW]

                def gemm_f(W, bW, j, rhs_fn, rhs_bufs, nk):
                    i = nxt("acc", 4)
                    for k in range(nk):
                        fw.op("tensor", lambda e, k=k: e.matmul(
                            acc[i][:], lhsT=W[:, k * 512 + j * 128:k * 512 + (j + 1) * 128], rhs=rhs_fn(k),
                            start=(k == 0), stop=(k == nk - 1)),
                            reads=[bW] + rhs_bufs, writes=[b_acc[i]], signal=(k == nk - 1))
                    return acc[i], b_acc[i]

                def rstd_from(src_fn, src_bufs):
                    for c in range(16):
                        i = nxt("sqr", 2)
                        fw.op("scalar", lambda e, c=c, i=i: e.activation(out=sqr[i][:], in_=src_fn(c),
                                                                         func=AF.Square),
                              reads=[src_bufs[c]], writes=[b_sqr[i]])
                        fw.op("tensor", lambda e, c=c, i=i: e.matmul(ssb[:], lhsT=onesb[:], rhs=sqr[i][:],
                                                                     start=(c == 0), stop=(c == 15)),
                              reads=[b_sqr[i], cbuf], writes=[b_ss], signal=True)
                    fw.op("scalar", lambda e: e.activation(out=rstd[:], in_=ssb[:], func=AF.Sqrt,
                                                           scale=1.0 / D, bias=pib[:, 1:2]),
                          reads=[b_ss, cbuf], writes=[b_rstd])
                    fw.op("vector", lambda e: e.reciprocal(out=rstd[:], in_=rstd[:]),
                          reads=[b_rstd], writes=[b_rstd])

                def norm_to_act(gname, l):
                    rstd_from(hc, b_hT)
                    g = gains[gname]
                    for c in range(16):
                        fw.op("vector", lambda e, c=c: e.scalar_tensor_tensor(
                            out=ac(c), in0=hc(c), scalar=g[:, l * 16 + c:l * 16 + c + 1], in1=rstd[:],
                            op0=ALU.mult, op1=ALU.mult),
                            reads=[b_hT[c], b_rstd, cbuf], writes=[b_act[c]])

                def norm_add(gname, l):
                    rstd_from(yc, b_yT)
                    g = gains[gname]
                    for c in range(16):
                        i = nxt("tmp", 4)
                        fw.op("vector", lambda e, c=c, i=i: e.scalar_tensor_tensor(
                            out=tmp[i][:], in0=yc(c), scalar=g[:, l * 16 + c:l * 16 + c + 1], in1=rstd[:],
                            op0=ALU.mult, op1=ALU.mult),
                            reads=[b_yT[c], b_rstd, cbuf], writes=[b_tmp[i]])
                        fw.op("gpsimd", lambda e, c=c, i=i: e.tensor_tensor(out=hc(c), in0=hc(c), in1=tmp[i][:],
                                                                            op=ALU.add),
                              reads=[b_tmp[i], b_hT[c]], writes=[b_hT[c]])

                def p1(l, ti):
                    tsl = slice(ti * TT, (ti + 1) * TT)
                    fw.dma("sync", tabs[:].rearrange("p (a t) -> p a t", t=TT),
                           tabs_d[:, :, tsl].rearrange("a p t -> p a t"), writes=[b_tabs])
                    norm_to_act("g_premix", l)

                    pend = []

                    def flush():
                        while pend:
                            pend.pop(0)()

                    def rope_epi(a, ba, typ, scale, dst, after=None):
                        i = nxt("qb", 2)
                        fw.op("scalar", lambda e: e.activation(out=qb[i][:], in_=a[:], func=AF.Copy,
                                                               scale=float(scale)),
                              reads=[ba], writes=[b_qb[i]])
                        ip = nxt("pp", 2)
                        i1 = nxt("tmp", 4)
                        i2 = nxt("tmp", 4)
                        si = nxt("stg", 3)
                        Ct = tabs[:, (2 * typ) * TT:(2 * typ + 1) * TT]
                        St = tabs[:, (2 * typ + 1) * TT:(2 * typ + 2) * TT]
                        fw.op("gpsimd", lambda e: e.tensor_tensor(out=tmp[i1][:], in0=qb[i][:], in1=Ct,
                                                                  op=ALU.mult),
                              reads=[b_qb[i], b_tabs], writes=[b_tmp[i1]])

                        def rest():
                            fw.op("tensor", lambda e: e.matmul(ppb[ip][:], lhsT=pm[:, typ * 128:(typ + 1) * 128],
                                                               rhs=qb[i][:], start=True, stop=True),
                                  reads=[b_qb[i], b_pm], writes=[b_pp[ip]])
                            fw.op("vector", lambda e: e.tensor_tensor(out=tmp[i2][:], in0=ppb[ip][:], in1=St,
                                                                      op=ALU.mult),
                                  reads=[b_pp[ip], b_tabs], writes=[b_tmp[i2]])
                            fw.op("gpsimd", lambda e: e.tensor_tensor(out=stg[si][:], in0=tmp[i1][:],
                                                                      in1=tmp[i2][:], op=ALU.add),
                                  reads=[b_tmp[i1], b_tmp[i2]], writes=[b_stg[si]])
                            fw.dma("sync", dst, stg[si][:], reads=[b_stg[si]])
                            if after is not None:
                                after(si)
                        pend.append(rest)
                        return si

                    rhs_act = lambda k: ac(k)

                    def gemm_p1(W, bW, j):
                        r_ = gemm_f(W, bW, j, lambda k: ac(k), b_act, 16)
                        flush()
                        return r_

                    for g in range(11):
                        W, bW = wload(wb_in[l, g], ("in", l, g))
                        if g in (0, 1):
                            for j in range(4):
                                h = g * 4 + j
                                a, ba = gemm_p1(W, bW, j)
                                rope_epi(a, ba, 0, 128.0 ** -0.5, qT_d[h * 128:(h + 1) * 128, tsl])
                        elif g == 2:
                            a, ba = gemm_p1(W, bW, 0)
                            rope_epi(a, ba, 0, 1.0, kT_d[:, tsl])
                            a, ba = gemm_p1(W, bW, 1)
                            rope_epi(a, ba, 1, 1.0, ikT_d[:, tsl])
                            ti_ = nxt("tok", 3)
                            for tb in range(4):
                                i = nxt("acc", 4)
                                for k in range(16):
                                    fw.op("tensor", lambda e, k=k, tb=tb, i=i: e.matmul(
                                        acc[i][:, 0:144], lhsT=act[:, k * TT + tb * 128:k * TT + (tb + 1) * 128],
                                        rhs=W[:, k * 512 + 256:k * 512 + 400], start=(k == 0), stop=(k == 15)),
                                        reads=[bW] + b_act, writes=[b_acc[i]], signal=(k == 15))
                                fw.op("scalar", lambda e, tb=tb, i=i: e.activation(
                                    out=tok[ti_][:, tb * 128:(tb + 1) * 128], in_=acc[i][:, 0:128], func=AF.Copy),
                                    reads=[b_acc[i]], writes=[b_tok[ti_]])
                                fw.op("scalar", lambda e, tb=tb, i=i: e.activation(
                                    out=iwst[:, tb * 16:(tb + 1) * 16], in_=acc[i][:, 128:144], func=AF.Copy,
                                    scale=1.0 / 32.0),
                                    reads=[b_acc[i]], writes=[b_iwst])
                            fw.dma("sync", v_d[tsl, :].rearrange("(b p) f -> p b f", p=128),
                                   tok[ti_][:].rearrange("p (b f) -> p b f", f=128), reads=[b_tok[ti_]])
                            fw.dma("sync", iw_d[tsl, :].rearrange("(b p) f -> p b f", p=128),
                                   iwst[:].rearrange("p (b f) -> p b f", f=16), reads=[b_iwst])
                        elif g in (3, 4):
                            for j in range(4):
                                pr = (g - 3) * 4 + j
                                a, ba = gemm_p1(W, bW, j)
                                rope_epi(a, ba, 1, 1.0, iqT_d[pr * 128:(pr + 1) * 128, tsl])
                        elif g == 5:
                            for j in range(4):
                                a, ba = gemm_p1(W, bW, j)
                                rope_epi(a, ba, 2, 1.0, rqT_d[j * 128:(j + 1) * 128, tsl])
                        elif g == 6:
                            for j in range(4):
                                a, ba = gemm_p1(W, bW, j)
                                def rk_after(si, j=j):
                                    for tb in range(4):
                                        fw.op("tensor", lambda e, tb=tb: e.transpose(
                                            out=trb16[:, tb * 128:(tb + 1) * 128],
                                            in_=stg[si][:, tb * 128:(tb + 1) * 128], identity=identb[:]),
                                            reads=[b_stg[si], cbuf], writes=[b_trb], signal=(tb == 3))
                                    ti_ = nxt("tok", 3)
                                    fw.op("scalar", lambda e: e.activation(out=tok[ti_][:], in_=trb16[:, 0:512],
                                                                           func=AF.Copy),
                                          reads=[b_trb], writes=[b_tok[ti_]])
                                    fw.dma("sync",
                                           rk_d[tsl, j * 128:(j + 1) * 128].rearrange("(b p) f -> p b f", p=128),
                                           tok[ti_][:].rearrange("p (b f) -> p b f", f=128), reads=[b_tok[ti_]])
                                rope_epi(a, ba, 2, 0.125, rkT_d[j * 128:(j + 1) * 128, tsl], after=rk_after)
                        elif g in (7, 8):
                            for tb in range(4):
                                i = nxt("acc", 4)
                                for k in range(16):
                                    fw.op("tensor", lambda e, k=k, tb=tb, i=i: e.matmul(
                                        acc[i][:], lhsT=act[:, k * TT + tb * 128:k * TT + (tb + 1) * 128],
                                        rhs=W[:, k * 512:(k + 1) * 512], start=(k == 0), stop=(k == 15)),
                                        reads=[bW] + b_act, writes=[b_acc[i]], signal=(k == 15))
                                ti_ = nxt("tok", 3)
                                fw.op("scalar", lambda e, i=i: e.activation(out=tok[ti_][:], in_=acc[i][:],
                                                                            func=AF.Copy),
                                      reads=[b_acc[i]], writes=[b_tok[ti_]])
                                r0 = ti * TT + tb * 128
                                fw.dma("sync", rv_d[r0:r0 + 128, (g - 7) * 512:(g - 6) * 512], tok[ti_][:],
                                       reads=[b_tok[ti_]])
                        else:
                            for j in range(4):
                                hh = (g - 9) * 4 + j
                                a, ba = gemm_p1(W, bW, j)
                                si = nxt("stg", 3)
                                fw.op("scalar", lambda e: e.activation(out=stg[si][:], in_=a[:], func=AF.Silu),
                                      reads=[ba], writes=[b_stg[si]])
                                fw.dma("sync", rgT_d[hh * 128:(hh + 1) * 128, tsl], stg[si][:], reads=[b_stg[si]])
                    flush()

                def p3(l, ti):
                    tsl = slice(ti * TT, (ti + 1) * TT)
                    fw.dma("sync", act[:].rearrange("p (c t) -> p c t", t=TT),
                           mixT_d[:, tsl].rearrange("(c p) t -> p c t", p=128), writes=b_act)
                    rhs_act = lambda k: ac(k)
                    for og in range(4):
                        W, bW = wload(wb_out[l, og], ("out", l, og))
                        for j in range(4):
                            oc = og * 4 + j
                            a, ba = gemm_f(W, bW, j, rhs_act, b_act, 16)
                            fw.op("scalar", lambda e, oc=oc: e.activation(out=yc(oc), in_=a[:], func=AF.Copy),
                                  reads=[ba], writes=[b_yT[oc]])
                    norm_add("g_postmix", l)
                    norm_to_act("g_preff", l)
                    for q in range(4):
                        for g1 in range(4):
                            W, bW = wload(wb_ff1[l, q * 4 + g1], ("ff1", l, q * 4 + g1))
                            for j in range(4):
                                ucx = g1 * 4 + j
                                a, ba = gemm_f(W, bW, j, rhs_act, b_act, 16)
                                i = nxt("tmp", 4)
                                fw.op("scalar", lambda e, i=i: e.activation(out=tmp[i][:], in_=a[:], func=AF.Relu),
                                      reads=[ba], writes=[b_tmp[i]])
                                fw.op("gpsimd", lambda e, i=i, ucx=ucx: e.tensor_tensor(
                                    out=uc(ucx), in0=tmp[i][:], in1=tmp[i][:], op=ALU.mult),
                                    reads=[b_tmp[i]], writes=[b_uT[ucx]])
                        for og in range(4):
                            W, bW = wload(wb_ff2[l, q * 4 + og], ("ff2", l, q * 4 + og))
                            for j in range(4):
                                oc = og * 4 + j
                                a, ba = gemm_f(W, bW, j, lambda k: uc(k), b_uT, 16)
                                if q == 0:
                                    fw.op("scalar", lambda e, oc=oc: e.activation(out=yc(oc), in_=a[:],
                                                                                  func=AF.Copy),
                                          reads=[ba], writes=[b_yT[oc]])
                                else:
                                    fw.op("vector", lambda e, oc=oc: e.tensor_tensor(out=yc(oc), in0=yc(oc),
                                                                                     in1=a[:], op=ALU.add),
                                          reads=[ba, b_yT[oc]], writes=[b_yT[oc]])
                    norm_add("g_postff", l)
                    fw.dma("sync", pin[:].rearrange("p (b f) -> p b f", f=256),
                           p_d[l][tsl, :].rearrange("(b p) f -> p b f", p=128), writes=[b_pin])
                    fw.op("vector", lambda e: e.tensor_copy(out=pbt[:], in_=pin[:]), reads=[b_pin], writes=[b_pbt])
                    for k2 in range(2):
                        for tb in range(4):
                            fw.op("tensor", lambda e, k2=k2, tb=tb: e.transpose(
                                out=trb16[:, tb * 128:(tb + 1) * 128],
                                in_=pbt[:, tb * 256 + k2 * 128:tb * 256 + (k2 + 1) * 128], identity=identb[:]),
                                reads=[b_pbt, cbuf], writes=[b_trb], signal=(tb == 3))
                        fw.op("scalar", lambda e, k2=k2: e.activation(out=pT[:, k2 * 512:(k2 + 1) * 512],
                                                                      in_=trb16[:, 0:512], func=AF.Copy),
                              reads=[b_trb], writes=[b_pT])
                    for c in range(16):
                        fw.op("gpsimd", lambda e, c=c: e.tensor_copy(out=ac(c), in_=hc(c)),
                              reads=[b_hT[c]], writes=[b_act[c]])
                    for og in range(4):
                        W, bW = wload(wb_gate[l, og], ("gate", l, og))
                        ip = nxt("wp", 2)
                        fw.dma("sync", Wp[ip][:], wb_ple[l, og], reads=[wbuf[("ple", l, og)]], writes=[b_Wp[ip]])
                        for j in range(4):
                            oc = og * 4 + j
                            a, ba = gemm_f(W, bW, j, rhs_act, b_act, 16)
                            a2, ba2 = gemm_f(Wp[ip], b_Wp[ip], j, lambda k: pT[:, k * 512:(k + 1) * 512], [b_pT], 2)
                            i = nxt("tmp", 4)
                            fw.op("scalar", lambda e, i=i: e.activation(out=tmp[i][:], in_=a[:], func=AF.Sigmoid),
                                  reads=[ba], writes=[b_tmp[i]])
                            fw.op("vector", lambda e, i=i, oc=oc: e.tensor_tensor(out=yc(oc), in0=tmp[i][:],
                                                                                  in1=a2[:], op=ALU.mult),
                                  reads=[b_tmp[i], ba2], writes=[b_yT[oc]])
                    norm_add("g_ple", l)

                for ti in range(NTILE):
                    tsl = slice(ti * TT, (ti + 1) * TT)
                    if l_prev is None:
                        for tb in range(4):
                            r0 = ti * TT + tb * 128
                            fw.dma("sync", yT[:, tb * 2048:(tb + 1) * 2048], x_d[r0:r0 + 128, :],
                                   writes=b_yT[tb * 4:(tb + 1) * 4])
                        for c in range(16):
                            for tb in range(4):
                                fw.op("tensor", lambda e, c=c, tb=tb: e.transpose(
                                    out=trb[:, tb * 128:(tb + 1) * 128],
                                    in_=yT[:, tb * 2048 + c * 128:tb * 2048 + (c + 1) * 128], identity=identf[:]),
                                    reads=b_yT[tb * 4:(tb + 1) * 4] + [cbuf], writes=[b_trb], signal=(tb == 3))
                            fw.op("scalar", lambda e, c=c: e.activation(out=hc(c), in_=trb[:], func=AF.Copy),
                                  reads=[b_trb], writes=[b_hT[c]])
                    else:
                        fw.dma("sync", hT[:].rearrange("p (c t) -> p c t", t=TT),
                               hT_d[:, tsl].rearrange("(c p) t -> p c t", p=128),
                               reads=[hbuf[ti]], writes=b_hT)
                        p3(l_prev, ti)
                    if l_next is not None:
                        fw.dma("sync", hT_d[:, tsl].rearrange("(c p) t -> p c t", p=128),
                               hT[:].rearrange("p (c t) -> p c t", t=TT), reads=b_hT, writes=[hbuf[ti]])
                        p1(l_next, ti)
                    else:
                        for tb in range(4):
                            for c4 in range(4):
                                for cc in range(4):
                                    c = c4 * 4 + cc
                                    fw.op("tensor", lambda e, c=c, cc=cc, tb=tb: e.transpose(
                                        out=trb[:, cc * 128:(cc + 1) * 128],
                                        in_=hT[:, c * TT + tb * 128:c * TT + (tb + 1) * 128], identity=identf[:]),
                                        reads=[b_hT[c], cbuf], writes=[b_trb], signal=(cc == 3))
                                fw.op("scalar", lambda e, c4=c4, tb=tb: e.activation(
                                    out=yT[:, tb * 2048 + c4 * 512:tb * 2048 + (c4 + 1) * 512], in_=trb[:],
                                    func=AF.Copy), reads=[b_trb], writes=[b_yT[tb * 4 + c4]])
                            r0 = ti * TT + tb * 128
                            fw.dma("sync", out_d[r0:r0 + 128, :], yT[:, tb * 2048:(tb + 1) * 2048],
                                   reads=b_yT[tb * 4:(tb + 1) * 4])
                fw.barrier()

        def seq_phase(l):
            with ExitStack() as ph:
                NBUF = 2
                irep = sb(ph, "irep", [128, 512], BF16)
                causb = sb(ph, "causb", [128, 128], BF16)
                causf = sb(ph, "causf", [128, 128], F32)
                dtt = sb(ph, "dtt", [128, 1024], F32)
                xit = sb(ph, "xit", [128, 512], F32)
                ztt = sb(ph, "ztt", [128, 512], F32)
                gtt = sb(ph, "gtt", [128, 4], F32)
                bist = sb(ph, "bist", [128, KBIS], F32)
                pS = pst(ph, "pS", [128, 1024], F32)
                pO = pst(ph, "pO", [128, 1024], F32)
                pD = pst(ph, "pD", [128, 1024], F32)
                pD16 = pD[:].bitcast(BF16)
                b_c = Buf()
                for tdst, nm in [(irep, "c_irep"), (causb, "c_causb"), (causf, "c_causf"), (dtt, "c_dt"),
                                 (xit, "c_xi"), (ztt, "c_zt"), (gtt, "c_gt"), (bist, "c_bis")]:
                    fw.dma("sync", tdst[:], cd[nm], writes=[b_c])
                b_pS, b_pO, b_pD = Buf(), Buf(), Buf()

                class St:
                    pass

                sts = []
                for s in range(nseq):
                    z = St()
                    z.s = s
                    z.kTs = sb(ph, "kTs", [128, S], BF16)
                    z.ikTs = sb(ph, "ikTs", [128, S], BF16)
                    z.Vs = sb(ph, "Vs", [128, S], BF16)
                    z.qTb = [sb(ph, "qTb", [128, 1024], BF16) for i in range(NBUF)]
                    z.iqTb = [sb(ph, "iqTb", [128, 1024], BF16) for i in range(NBUF)]
                    z.iwb = [sb(ph, "iwb", [128, 16], F32) for i in range(NBUF)]
                    z.rqTb = [sb(ph, "rqTb", [128, 512], BF16) for i in range(NBUF)]
                    z.rkTb = [sb(ph, "rkTb", [128, 512], BF16) for i in range(NBUF)]
                    z.rkb = [sb(ph, "rkb", [128, 512], BF16) for i in range(NBUF)]
                    z.rvb = [sb(ph, "rvb", [128, 1024], BF16) for i in range(NBUF)]
                    z.rgTb = [sb(ph, "rgTb", [128, 1024], BF16) for i in range(NBUF)]
                    z.sacc = sb(ph, "sacc", [128, S], F32)
                    z.work = sb(ph, "work", [128, S], F32)
                    z.mb = sb(ph, "mb", [128, S], BF16)
                    z.mx = sb(ph, "mx", [128, 8], F32)
                    z.steps = sb(ph, "steps", [128, KBIS], F32)
                    z.rr = [sb(ph, "rr", [128, 512], F32) for i in range(2)]
                    z.ET = [sb(ph, "ET", [128, 1024], BF16) for i in range(2)]
                    z.rec = sb(ph, "rec", [128, 1024], F32)
                    z.ast = sb(ph, "ast", [128, 1024], BF16)
                    z.Aall = sb(ph, "Aall", [128, 1024], BF16)
                    z.rqxi = sb(ph, "rqxi", [128, 512], BF16)
                    z.rkz = sb(ph, "rkz", [128, 512], BF16)
                    z.Rst = sb(ph, "Rst", [128, 512], F32)
                    z.Rb = sb(ph, "Rb", [128, 512], BF16)
                    z.ocp = sb(ph, "ocp", [128, 1024], F32)
                    z.osq = sb(ph, "osq", [128, 1024], F32)
                    z.st = sb(ph, "st", [128, 64], F32)
                    z.yb = sb(ph, "yb", [128, 1024], BF16)
                    z.rst = sb(ph, "rst", [128, 1024], BF16)
                    z.pA = pst(ph, "pA", [128, 512], F32)
                    (z.b_kTs, z.b_ikTs, z.b_Vs, z.b_sacc, z.b_work, z.b_mb, z.b_mx, z.b_rec, z.b_ast, z.b_Aall,
                     z.b_rqxi, z.b_rkz, z.b_R, z.b_Rb, z.b_ocp, z.b_osq, z.b_st, z.b_yb, z.b_rst, z.b_pA) = bufs(20)
                    z.b_blk = bufs(NBUF)
                    z.b_rr, z.b_ET = bufs(2), bufs(2)
                    z.ctr = dict(rr=0, et=0)
                    sts.append(z)

                def nxt(z, k, n):
                    i = z.ctr[k] % n
                    z.ctr[k] += 1
                    return i

                def seq_setup(z):
                    ssl = slice(z.s * S, (z.s + 1) * S)
                    fw.dma("sync", z.kTs[:], kT_d[:, ssl], writes=[z.b_kTs])
                    fw.dma("sync", z.ikTs[:], ikT_d[:, ssl], writes=[z.b_ikTs])
                    fw.dma("sync", z.Vs[:].rearrange("p (b f) -> p b f", f=128),
                           v_d[ssl, :].rearrange("(b p) f -> p b f", p=128), writes=[z.b_Vs])
                    fw.op("vector", lambda e: e.memset(z.Rst[:], 0.0), writes=[z.b_R])
                    fw.op("vector", lambda e: e.memset(z.Rb[:], 0.0), writes=[z.b_Rb])

                def blk(z, j):
                    bi = j % NBUF
                    bb = z.b_blk[bi]
                    qTb, iqTb, iwb, rqTb, rkTb, rkb, rvb, rgTb = (z.qTb[bi], z.iqTb[bi], z.iwb[bi], z.rqTb[bi],
                                                                 z.rkTb[bi], z.rkb[bi], z.rvb[bi], z.rgTb[bi])
                    sacc, work, mb, mx = z.sacc, z.work, z.mb, z.mx
                    t0 = z.s * S + j * 128
                    bsl = slice(t0, t0 + 128)
                    nk = (j + 1) * 128
                    for (dst, src, w_) in [(qTb, qT_d, 128), (iqTb, iqT_d, 128), (rqTb, rqT_d, 128),
                                           (rkTb, rkT_d, 128), (rgTb, rgT_d, 128)]:
                        fw.dma("sync", dst[:].rearrange("p (h t) -> p h t", t=128),
                               src[:, bsl].rearrange("(h p) t -> p h t", p=128), writes=[bb])
                    fw.dma("sync", iwb[:], iw_d[bsl, :], writes=[bb])
                    fw.dma("sync", rkb[:], rk_d[bsl, :], writes=[bb])
                    fw.dma("sync", rvb[:], rv_d[bsl, :], writes=[bb])
                    yield
                    if j >= 2:
                        npc = (nk + 511) // 512
                        for pc in range(npc):
                            w = min(512, nk - pc * 512)
                            csl = slice(pc * 512, pc * 512 + w)
                            for h in range(16):
                                base = 64 * (h % 2)
                                pr = h // 2
                                fw.op("tensor", lambda e: e.matmul(
                                    z.pA[:, 0:w], lhsT=iqTb[base:base + 64, pr * 128:(pr + 1) * 128],
                                    rhs=z.ikTs[base:base + 64, csl], start=True, stop=True),
                                    reads=[bb, z.b_ikTs], writes=[z.b_pA])
                                ir = nxt(z, "rr", 2)
                                fw.op("scalar", lambda e: e.activation(out=z.rr[ir][:, 0:w], in_=z.pA[:, 0:w],
                                                                       func=AF.Relu),
                                      reads=[z.b_pA], writes=[z.b_rr[ir]])
                                if h == 0:
                                    fw.op("vector", lambda e: e.tensor_scalar(
                                        out=sacc[:, csl], in0=z.rr[ir][:, 0:w], scalar1=iwb[:, 0:1],
                                        scalar2=None, op0=ALU.mult),
                                        reads=[z.b_rr[ir], bb], writes=[z.b_sacc])
                                else:
                                    fw.op("vector", lambda e: e.scalar_tensor_tensor(
                                        out=sacc[:, csl], in0=z.rr[ir][:, 0:w], scalar=iwb[:, h:h + 1],
                                        in1=sacc[:, csl], op0=ALU.mult, op1=ALU.add),
                                        reads=[z.b_rr[ir], bb, z.b_sacc], writes=[z.b_sacc])
                                yield
                        fw.op("vector", lambda e: e.tensor_reduce(out=mx[:, 1:2], in_=sacc[:, 0:nk], axis=AX.X,
                                                                  op=ALU.min),
                              reads=[z.b_sacc], writes=[z.b_mx])
                        yield
                        dsl = slice(j * 128, (j + 1) * 128)
                        fw.op("vector", lambda e: e.tensor_tensor(out=sacc[:, dsl], in0=sacc[:, dsl],
                                                                  in1=causf[:], op=ALU.add),
                              reads=[z.b_sacc, b_c], writes=[z.b_sacc])
                        yield
                        fw.op("vector", lambda e: e.tensor_reduce(out=mx[:, 0:1], in_=sacc[:, 0:nk], axis=AX.X,
                                                                  op=ALU.max),
                              reads=[z.b_sacc], writes=[z.b_mx])
                        yield
                        fw.op("vector", lambda e: e.tensor_tensor(out=mx[:, 2:3], in0=mx[:, 0:1], in1=mx[:, 1:2],
                                                                  op=ALU.subtract),
                              reads=[z.b_mx], writes=[z.b_mx])
                        yield
                        fw.op("vector", lambda e: e.tensor_scalar(out=z.steps[:], in0=bist[:], scalar1=mx[:, 2:3],
                                                                  scalar2=None, op0=ALU.mult),
                              reads=[z.b_mx, b_c], writes=[z.b_mx])
                        fw.op("vector", lambda e: e.scalar_tensor_tensor(
                            out=mx[:, 3:4], in0=mx[:, 2:3], scalar=0.5, in1=mx[:, 1:2], op0=ALU.mult, op1=ALU.add),
                            reads=[z.b_mx], writes=[z.b_mx])
                        yield
                        for k in range(KBIS):
                            fw.op("vector", lambda e: e.tensor_scalar(
                                out=work[:, 0:nk], in0=sacc[:, 0:nk], scalar1=mx[:, 3:4], scalar2=None,
                                op0=ALU.is_ge, op1=ALU.add, accum_out=mx[:, 4:5]),
                                reads=[z.b_sacc, z.b_mx], writes=[z.b_work, z.b_mx])
                            yield
                            fw.op("vector", lambda e: e.tensor_scalar(
                                out=mx[:, 5:6], in0=mx[:, 4:5], scalar1=255.5, scalar2=2.0,
                                op0=ALU.is_ge, op1=ALU.mult), reads=[z.b_mx], writes=[z.b_mx])
                            yield
                            fw.op("vector", lambda e: e.tensor_scalar(
                                out=mx[:, 5:6], in0=mx[:, 5:6], scalar1=-1.0, scalar2=z.steps[:, k:k + 1],
                                op0=ALU.add, op1=ALU.mult), reads=[z.b_mx], writes=[z.b_mx])
                            yield
                            fw.op("vector", lambda e: e.tensor_tensor(out=mx[:, 3:4], in0=mx[:, 3:4], in1=mx[:, 5:6],
                                                                      op=ALU.add),
                                  reads=[z.b_mx], writes=[z.b_mx])
                            yield
                        fw.op("vector", lambda e: e.tensor_tensor(out=mx[:, 7:8], in0=mx[:, 3:4],
                                                                  in1=z.steps[:, KBIS - 1:KBIS], op=ALU.subtract),
                              reads=[z.b_mx], writes=[z.b_mx])
                        yield
                        fw.op("vector", lambda e: e.tensor_scalar(
                            out=mb[:, 0:nk], in0=sacc[:, 0:nk], scalar1=mx[:, 7:8], scalar2=-30000.0,
                            op0=ALU.is_lt, op1=ALU.mult), reads=[z.b_sacc, z.b_mx], writes=[z.b_mb])
                    else:
                        if j == 1:
                            fw.op("vector", lambda e: e.memset(mb[:, 0:128], 0.0), writes=[z.b_mb])
                        dsl = slice(j * 128, (j + 1) * 128)
                        fw.op("vector", lambda e: e.tensor_copy(out=mb[:, dsl], in_=causb[:]),
                              reads=[b_c], writes=[z.b_mb])
                    yield
                    for kc in range(j + 1):
                        ksl = slice(kc * 128, (kc + 1) * 128)
                        for half in range(2):
                            hs = slice(half * 512, (half + 1) * 512)
                            fw.op("tensor", lambda e: e.matmul(
                                pS[:, hs], lhsT=z.kTs[:, ksl], rhs=qTb[:, hs], start=True, stop=False),
                                reads=[z.b_kTs, bb], writes=[b_pS], signal=False)
                            fw.op("tensor", lambda e: e.matmul(
                                pS[:, hs], lhsT=mb[:, ksl], rhs=irep[:], start=False, stop=True),
                                reads=[z.b_mb, b_c], writes=[b_pS], signal=(half == 1))
                        ie = nxt(z, "et", 2)
                        fw.op("scalar", lambda e: e.activation(out=z.ET[ie][:], in_=pS[:], func=AF.Exp),
                              reads=[b_pS], writes=[z.b_ET[ie]])
                        for half in range(2):
                            hs = slice(half * 512, (half + 1) * 512)
                            fw.op("tensor", lambda e: e.matmul(
                                pO[:, hs], lhsT=z.Vs[:, ksl], rhs=z.ET[ie][:, hs], start=(kc == 0), stop=(kc == j)),
                                reads=[z.b_Vs, z.b_ET[ie]], writes=[b_pO], signal=False)
                            fw.op("tensor", lambda e: e.matmul(
                                pD[:, hs], lhsT=onesb[:], rhs=z.ET[ie][:, hs], start=(kc == 0), stop=(kc == j)),
                                reads=[cbuf, z.b_ET[ie]], writes=[b_pD], signal=(half == 1))
                    fw.op("vector", lambda e: e.reciprocal(out=z.rec[:], in_=pD[:]), reads=[b_pD], writes=[z.b_rec])
                    fw.op("vector", lambda e: e.tensor_tensor(out=z.ast[:], in0=pO[:], in1=z.rec[:], op=ALU.mult),
                          reads=[b_pO, z.b_rec], writes=[z.b_ast])
                    fw.dma("sync", mixT_d[0:1024, bsl].rearrange("(h p) t -> p h t", p=128),
                           z.ast[:].rearrange("p (h t) -> p h t", t=128), reads=[z.b_ast])
                    yield
                    for h in range(8):
                        base = 64 * (h % 2)
                        c = h // 2
                        fw.op("tensor", lambda e: e.matmul(
                            pD[:, (h % 2) * 512 + c * 128:(h % 2) * 512 + (c + 1) * 128],
                            lhsT=rkTb[base:base + 64, c * 128:(c + 1) * 128],
                            rhs=rqTb[base:base + 64, c * 128:(c + 1) * 128], start=True, stop=True),
                            reads=[bb], writes=[b_pD], signal=(h == 7))
                    fw.op("vector", lambda e: e.tensor_tensor(out=z.Aall[:], in0=pD[:], in1=dtt[:], op=ALU.mult),
                          reads=[b_pD, b_c], writes=[z.b_Aall])
                    fw.op("gpsimd", lambda e: e.tensor_tensor(out=z.rqxi[:], in0=rqTb[:], in1=xit[:], op=ALU.mult),
                          reads=[bb, b_c], writes=[z.b_rqxi])
                    fw.op("gpsimd", lambda e: e.tensor_tensor(out=z.rkz[:], in0=rkb[:], in1=ztt[:], op=ALU.mult),
                          reads=[bb, b_c], writes=[z.b_rkz])
                    yield
                    for h in range(8):
                        base = 64 * (h % 2)
                        c = h // 2
                        hsl = slice(h * 128, (h + 1) * 128)
                        asl = slice((h % 2) * 512 + c * 128, (h % 2) * 512 + (c + 1) * 128)
                        fw.op("tensor", lambda e: e.matmul(
                            pS[:, hsl], lhsT=z.Aall[:, asl], rhs=rvb[:, hsl], start=True, stop=False),
                            reads=[z.b_Aall, bb], writes=[b_pS], signal=False)
                        fw.op("tensor", lambda e: e.matmul(
                            pS[:, hsl], lhsT=z.rqxi[base:base + 64, c * 128:(c + 1) * 128],
                            rhs=z.Rb[base:base + 64, c * 128:(c + 1) * 128], start=False, stop=True),
                            reads=[z.b_rqxi, z.b_Rb], writes=[b_pS], signal=(h == 7))
                    fw.op("scalar", lambda e: e.activation(out=z.ocp[:], in_=pS[:], func=AF.Copy),
                          reads=[b_pS], writes=[z.b_ocp])
                    fw.op("scalar", lambda e: e.activation(out=z.osq[:], in_=pS[:], func=AF.Square),
                          reads=[b_pS], writes=[z.b_osq])
                    for c in range(4):
                        fw.op("tensor", lambda e: e.matmul(
                            pO[:, c * 256:(c + 1) * 256], lhsT=z.rkz[:, c * 128:(c + 1) * 128],
                            rhs=rvb[:, c * 256:(c + 1) * 256], start=True, stop=True),
                            reads=[z.b_rkz, bb], writes=[b_pO], signal=(c == 3))
                    for c in range(4):
                        for hh in range(2):
                            ps_ = slice(hh * 64, (hh + 1) * 64)
                            fw.op("vector", lambda e: e.scalar_tensor_tensor(
                                out=z.Rst[ps_, c * 128:(c + 1) * 128], in0=z.Rst[ps_, c * 128:(c + 1) * 128],
                                scalar=gtt[ps_, c:c + 1],
                                in1=pO[ps_, c * 256 + hh * 128:c * 256 + (hh + 1) * 128],
                                op0=ALU.mult, op1=ALU.add),
                                reads=[z.b_R, b_pO, b_c], writes=[z.b_R])
                    fw.op("vector", lambda e: e.tensor_copy(out=z.Rb[:], in_=z.Rst[:]), reads=[z.b_R], writes=[z.b_Rb])
                    yield
                    st = z.st
                    fw.op("vector", lambda e: e.tensor_reduce(
                        out=st[:, 0:8], in_=z.ocp[:].rearrange("p (h f) -> p h f", f=128), axis=AX.X, op=ALU.add),
                        reads=[z.b_ocp], writes=[z.b_st])
                    fw.op("vector", lambda e: e.tensor_reduce(
                        out=st[:, 8:16], in_=z.osq[:].rearrange("p (h f) -> p h f", f=128), axis=AX.X, op=ALU.add),
                        reads=[z.b_osq], writes=[z.b_st])
                    yield
                    fw.op("vector", lambda e: e.tensor_scalar(out=st[:, 16:24], in0=st[:, 0:8],
                                                              scalar1=1.0 / 128, scalar2=None, op0=ALU.mult),
                          reads=[z.b_st], writes=[z.b_st])
                    yield
                    fw.op("vector", lambda e: e.tensor_tensor(out=st[:, 24:32], in0=st[:, 16:24],
                                                              in1=st[:, 16:24], op=ALU.mult),
                          reads=[z.b_st], writes=[z.b_st])
                    yield
                    fw.op("vector", lambda e: e.scalar_tensor_tensor(
                        out=st[:, 32:40], in0=st[:, 8:16], scalar=1.0 / 128, in1=st[:, 24:32],
                        op0=ALU.mult, op1=ALU.subtract), reads=[z.b_st], writes=[z.b_st])
                    fw.op("scalar", lambda e: e.activation(out=st[:, 40:48], in_=st[:, 32:40], func=AF.Sqrt,
                                                           bias=epsb[:, 0:1]),
                          reads=[z.b_st, cbuf], writes=[z.b_st])
                    yield
                    fw.op("vector", lambda e: e.reciprocal(out=st[:, 40:48], in_=st[:, 40:48]),
                          reads=[z.b_st], writes=[z.b_st])
                    yield
                    fw.op("vector", lambda e: e.scalar_tensor_tensor(
                        out=st[:, 48:56], in0=st[:, 16:24], scalar=-1.0, in1=st[:, 40:48],
                        op0=ALU.mult, op1=ALU.mult), reads=[z.b_st], writes=[z.b_st])
                    for h in range(8):
                        hsl = slice(h * 128, (h + 1) * 128)
                        fw.op("gpsimd", lambda e: e.tensor_scalar(
                            out=z.yb[:, hsl], in0=z.ocp[:, hsl], scalar1=st[:, 40 + h:41 + h],
                            scalar2=st[:, 48 + h:49 + h], op0=ALU.mult, op1=ALU.add),
                            reads=[z.b_ocp, z.b_st], writes=[z.b_yb])
                    yield
                    for h in range(8):
                        hsl = slice(h * 128, (h + 1) * 128)
                        fw.op("tensor", lambda e: e.transpose(out=pD16[:, hsl], in_=z.yb[:, hsl],
                                                              identity=identb[:]),
                              reads=[z.b_yb, cbuf], writes=[b_pD], signal=(h == 7))
                    for h in range(8):
                        hsl = slice(h * 128, (h + 1) * 128)
                        fw.op("vector", lambda e: e.scalar_tensor_tensor(
                            out=z.rst[:, hsl], in0=pD16[:, hsl], scalar=gn_t[:, l * 8 + h:l * 8 + h + 1],
                            in1=rgTb[:, hsl], op0=ALU.mult, op1=ALU.mult),
                            reads=[b_pD, bb, cbuf], writes=[z.b_rst])
                    fw.dma("sync", mixT_d[1024:2048, bsl].rearrange("(h p) t -> p h t", p=128),
                           z.rst[:].rearrange("p (h t) -> p h t", t=128), reads=[z.b_rst])
                    yield

                for z in sts:
                    seq_setup(z)
                for j in range(nblk):
                    gens = [blk(z, j) for z in sts]
                    while gens:
                        for g in list(gens):
                            try:
                                next(g)
                            except StopIteration:
                                gens.remove(g)
                fw.barrier()


        if only_seq:
            seq_phase(0)
            fw.barrier()
            return nc
        dense_phase(None, 0)
        if stop_after == "p1":
            return nc
        for l in range(nlayers):
            if l + 1 < nlayers:
                conv_layer(l + 1)
            seq_phase(l)
            if stop_after == f"p2_{l}":
                return nc
            dense_phase(l, l + 1 if l + 1 < nlayers else None)
        fw.barrier()
    return nc


_NC_CACHE = {}


def _prep_core_inputs(inputs, c, nseq, consts):
    b0 = c * nseq
    m = {}
    m["x"] = np.ascontiguousarray(inputs["x"][b0:b0 + nseq]).reshape(nseq * S, D)
    m["p"] = np.ascontiguousarray(inputs["p"][:, b0:b0 + nseq]).reshape(DEPTH, nseq * S, 256)
    m["pos"] = np.ascontiguousarray(inputs["positions"][b0:b0 + nseq]).reshape(1, nseq * S).astype(np.int32)
    return m


def _shared_inputs(inputs):
    m = {}
    for k_, n_ in [("w_in", "w_in"), ("w_out", "w_out"), ("w_ff1", "w_ff1"), ("w_ff2", "w_ff2"),
                   ("w_ple", "w_ple"), ("w_ple_gate", "w_gate")]:
        m[n_] = np.ascontiguousarray(np.asarray(inputs[k_], dtype=np.float32))
    for k_, n_ in [("pre_mix_norm", "g_premix"), ("post_mix_norm", "g_postmix"), ("pre_ff_norm", "g_preff"),
                   ("post_ff_norm", "g_postff"), ("ple_norm", "g_ple")]:
        g = np.asarray(inputs[k_], dtype=np.float32)
        m[n_] = np.ascontiguousarray(g.reshape(DEPTH, 16, 128).transpose(0, 2, 1))
    g = np.asarray(inputs["ret_gn"], dtype=np.float32)
    m["g_gn"] = np.ascontiguousarray(g.reshape(DEPTH, 8, 128).transpose(0, 2, 1))
    m.update(_consts())
    return m


def kernel(**inputs):
    inputs = {k: np.asarray(v) for k, v in inputs.items()}
    B = inputs["x"].shape[0]
    ncores = 8
    nseq = B // ncores
    if "nc" not in _NC_CACHE:
        _NC_CACHE["nc"] = build(nseq=nseq)
    nc = _NC_CACHE["nc"]
    shared = _shared_inputs(inputs)
    in_maps = []
    for c in range(ncores):
        m = dict(shared)
        m.update(_prep_core_inputs(inputs, c, nseq, None))
        in_maps.append(m)
    res = run_bass_kernel_spmd(nc, in_maps, core_ids=list(range(ncores)))
    outs = [np.asarray(r["out"]).reshape(nseq, S, D) for r in res.results]
    return np.concatenate(outs, axis=0).astype(np.float32)
```

```python
import numpy as np
import ml_dtypes
from contextlib import ExitStack
import concourse.bass as bass
import concourse.mybir as mybir
from concourse.bass_utils import run_bass_kernel_spmd

F32 = mybir.dt.float32
BF16 = mybir.dt.bfloat16
I32 = mybir.dt.int32
AF = mybir.ActivationFunctionType
ALU = mybir.AluOpType
AX = mybir.AxisListType

D = 2048
S = 2048
DEPTH = 2
KC = 16
TT = 512
NB = S // 128
AQ, AK, AV, IQ, IK, IW, RQ, RK, RV, RG, INW = 0, 1024, 1152, 1280, 2304, 2368, 2384, 2896, 3408, 4432, 5456
IN_GROUPS = [
    [(AQ, 512, 0)], [(AQ + 512, 512, 0)],
    [(AK, 128, 0), (IK, 64, 128), (IK, 64, 192), (AV, 128, 256), (IW, 16, 384), (AV, 112, 400)],
    [(IQ, 512, 0)], [(IQ + 512, 512, 0)],
    [(RQ, 512, 0)], [(RK, 512, 0)], [(RV, 512, 0)], [(RV + 512, 512, 0)],
    [(RG, 512, 0)], [(RG + 512, 512, 0)],
]
NEG = -1.0e30
SEM_LIMIT = 60000
KBIS = 22


class Buf:
    __slots__ = ("w", "r")

    def __init__(self):
        self.w = None
        self.r = {}


def bufs(n):
    return [Buf() for _ in range(n)]


class FW:
    def __init__(self, nc, es):
        self.nc = nc
        self.es = es
        self.nsem = 0
        self.E = {}
        for name in ["tensor", "vector", "scalar", "gpsimd", "sync"]:
            self.E[name] = dict(h=getattr(nc, name), sem=self._sem(), count=0, seen={},
                                selfsync=(name != "tensor"), epoch=0, last=None)
        self.pool = {q: [dict(sem=self._sem(), val=0) for _ in range(n)]
                     for q, n in [("sync", 24), ("gpsimd", 40), ("scalar", 2)]}
        self.rr = {q: 0 for q in self.pool}
        self.log = {n: [] for n in self.E}

    def _sem(self):
        self.nsem += 1
        return self.es.enter_context(self.nc.semaphore(f"sm{self.nsem}"))

    def _deps(self, reads, writes):
        d = []
        for b in reads:
            if b.w is not None:
                d.append(b.w)
        for b in writes:
            if b.w is not None:
                d.append(b.w)
            d.extend(b.r.values())
        return d

    def _wait(self, ename, deps):
        E = self.E[ename]
        need = {}
        for (key, sem, val, owner) in deps:
            if owner == ename and not E["selfsync"]:
                continue
            if E["seen"].get(key, 0) >= val:
                continue
            if key not in need or need[key][1] < val:
                need[key] = (sem, val)
        for key, (sem, val) in need.items():
            E["h"].wait_ge(sem, val)
            self.log[ename].append(("wait", id(sem), val))
            E["seen"][key] = val

    def _record(self, tok, reads, writes):
        for b in reads:
            o = b.r.get(tok[0])
            if o is None or o[2] < tok[2]:
                b.r[tok[0]] = tok
        for b in writes:
            b.w = tok
            b.r = {}

    def op(self, ename, fn, reads=(), writes=(), signal=True):
        E = self.E[ename]
        self._wait(ename, self._deps(reads, writes))
        if E["count"] >= SEM_LIMIT:
            E["sem"] = self._sem()
            E["count"] = 0
            E["epoch"] += 1
        ins = fn(E["h"])
        key = (ename, E["epoch"])
        if signal:
            E["count"] += 1
            ins.then_inc(E["sem"], 1)
            self.log[ename].append(("inc", id(E["sem"]), 1))
            tok = (key, E["sem"], E["count"], ename)
            E["last"] = tok
        else:
            tok = (key, E["sem"], E["count"] + 1, ename)
        self._record(tok, reads, writes)
        return ins

    def dma(self, q, out, in_, reads=(), writes=(), **kw):
        E = self.E[q]
        pool = self.pool[q]
        i = self.rr[q]
        self.rr[q] = (i + 1) % len(pool)
        slot = pool[i]
        if slot["val"] + 16 > SEM_LIMIT:
            slot["sem"] = self._sem()
            slot["val"] = 0
        deps = self._deps(reads, writes)
        key = ("dma", id(slot["sem"]))
        if slot["val"] > 0:
            deps.append((key, slot["sem"], slot["val"], "dma"))
        self._wait(q, deps)
        ins = E["h"].dma_start(out=out, in_=in_, **kw)
        slot["val"] += 16
        ins.then_inc(slot["sem"], 16)
        self.log[q].append(("inc", id(slot["sem"]), 16))
        tok = (key, slot["sem"], slot["val"], "dma")
        self._record(tok, reads, writes)
        return ins

    def all_tokens(self):
        toks = []
        for n, E in self.E.items():
            if E["last"] is not None:
                toks.append(E["last"])
        for q, pool in self.pool.items():
            for s in pool:
                if s["val"] > 0:
                    toks.append((("dma", id(s["sem"])), s["sem"], s["val"], "dma"))
        return toks

    def barrier(self, engines=None):
        toks = self.all_tokens()
        for n in (engines or list(self.E.keys())):
            E = self.E[n]
            ss = E["selfsync"]
            E["selfsync"] = True
            self._wait(n, toks)
            E["selfsync"] = ss


def _consts():
    c = {}
    c["c_identf"] = np.eye(128, dtype=np.float32)
    c["c_identb"] = np.eye(128, dtype=np.float32).astype(ml_dtypes.bfloat16)
    c["c_irep"] = np.tile(np.eye(128, dtype=np.float32), (1, 4)).astype(ml_dtypes.bfloat16)
    pm = np.zeros((3, 128, 128), np.float32)
    invf = np.zeros((128, 3), np.float64)
    for i in range(16):
        pm[0, i + 16, i] = -1.0
        pm[0, i, i + 16] = 1.0
        invf[i, 0] = invf[i + 16, 0] = 500000.0 ** (-i / 16.0)
    for o in (0, 64):
        for i in range(8):
            pm[1, o + i + 8, o + i] = -1.0
            pm[1, o + i, o + i + 8] = 1.0
            invf[o + i, 1] = invf[o + i + 8, 1] = 500000.0 ** (-i / 8.0)
    for o in (0, 64):
        for i in range(32):
            pm[2, o + i + 32, o + i] = -1.0
            pm[2, o + i, o + i + 32] = 1.0
            invf[o + i, 2] = invf[o + i + 32, 2] = 10000.0 ** (-i / 32.0)
    c["c_pm"] = pm.astype(ml_dtypes.bfloat16)
    c["c_invf"] = (invf.astype(np.float32).astype(np.float64) / (2 * np.pi)).astype(np.float32)
    q = np.arange(128)[:, None]
    k = np.arange(128)[None, :]
    c["c_causb"] = np.where(k <= q, 0.0, -30000.0).astype(np.float32).astype(ml_dtypes.bfloat16)
    c["c_causf"] = np.where(k <= q, 0.0, NEG).astype(np.float32)
    H = 8
    C = 128
    gamma = (1.0 - 2.0 ** (-5.0 - np.arange(H, dtype=np.float32))).astype(np.float32)
    log_g = np.log(gamma).astype(np.float32)
    i = np.arange(C, dtype=np.float32)
    dt = np.zeros((128, H, 128), np.float32)
    for h in range(H):
        diff = i[None, :] - i[:, None]
        dt[:, h, :] = np.where(diff >= 0, np.exp(np.maximum(diff, 0.0) * log_g[h]), 0.0)
    c["c_dt"] = np.ascontiguousarray(dt.reshape(128, 4, 2, 128).transpose(0, 2, 1, 3)).reshape(128, 1024).astype(np.float32)
    zeta = np.exp((C - 1.0 - i)[None, :] * log_g[:, None]).astype(np.float32)
    xi = np.exp((i + 1.0)[None, :] * log_g[:, None]).astype(np.float32)
    gch = np.exp(C * log_g).astype(np.float32)
    xit = np.zeros((128, 4, 128), np.float32)
    gt = np.zeros((128, 4), np.float32)
    for cc in range(4):
        for hh in range(2):
            xit[hh * 64:(hh + 1) * 64, cc, :] = xi[2 * cc + hh][None, :]
            gt[hh * 64:(hh + 1) * 64, cc] = gch[2 * cc + hh]
    c["c_xi"] = xit.reshape(128, 512)
    c["c_gt"] = gt
    zt = np.zeros((128, H, 64), np.float32)
    for h in range(H):
        zt[:, h, :] = zeta[h][:, None]
    c["c_zt"] = zt.reshape(128, 512)
    c["c_bis"] = np.tile((2.0 ** -(np.arange(KBIS, dtype=np.float32) + 2.0))[None, :], (128, 1)).astype(np.float32)
    return c


CONST_SPECS = {
    "c_identf": ([128, 128], F32), "c_identb": ([128, 128], BF16), "c_irep": ([128, 512], BF16),
    "c_pm": ([3, 128, 128], BF16), "c_invf": ([128, 3], F32), "c_causb": ([128, 128], BF16),
    "c_causf": ([128, 128], F32), "c_dt": ([128, 1024], F32), "c_xi": ([128, 512], F32),
    "c_gt": ([128, 4], F32), "c_zt": ([128, 512], F32), "c_bis": ([128, KBIS], F32),
}
GAIN_NAMES = ["g_premix", "g_postmix", "g_preff", "g_postff", "g_ple"]


def build(nseq=2, dbg=False, stop_after=None, nlayers=DEPTH, only_seq=False, nblk=NB, parts=("idx", "attn", "ret"), retlvl=9):
    NT = nseq * S
    NTILE = NT // TT
    nc = bass.Bass("TRN2", target_bir_lowering=False)
    kin = "ExternalInput"
    ksc = "ExternalOutput" if dbg else "Internal"

    def dram(name, shape, dt, kind):
        return nc.dram_tensor(name, shape, dt, kind=kind).ap()

    kin_ = kin
    if only_seq:
        kin = "Internal"
    x_d = dram("x", [NT, D], F32, kin)
    p_d = dram("p", [DEPTH, NT, 256], F32, kin)
    pos_d = dram("pos", [1, NT], I32, kin)
    w_in_d = dram("w_in", [DEPTH, D, INW], F32, kin)
    w_out_d = dram("w_out", [DEPTH, D, D], F32, kin)
    w_ff1_d = dram("w_ff1", [DEPTH, D, 4 * D], F32, kin)
    w_ff2_d = dram("w_ff2", [DEPTH, 4 * D, D], F32, kin)
    w_ple_d = dram("w_ple", [DEPTH, 256, D], F32, kin)
    w_gate_d = dram("w_gate", [DEPTH, D, D], F32, kin)
    kin = kin_
    gains_d = {n: dram(n, [DEPTH, 128, 16], F32, kin) for n in GAIN_NAMES}
    gn_d = dram("g_gn", [DEPTH, 128, 8], F32, kin)
    cd = {n: dram(n, sh, dt, kin) for n, (sh, dt) in CONST_SPECS.items()}
    out_d = dram("out", [NT, D], F32, "ExternalOutput")

    wb_in = dram("wb_in", [DEPTH, 11, 128, 8192], BF16, "Internal")
    wb_out = dram("wb_out", [DEPTH, 4, 128, 8192], BF16, "Internal")
    wb_ff1 = dram("wb_ff1", [DEPTH, 16, 128, 8192], BF16, "Internal")
    wb_ff2 = dram("wb_ff2", [DEPTH, 16, 128, 8192], BF16, "Internal")
    wb_gate = dram("wb_gate", [DEPTH, 4, 128, 8192], BF16, "Internal")
    wb_ple = dram("wb_ple", [DEPTH, 4, 128, 1024], BF16, "Internal")
    ksi = kin if only_seq else ksc
    hT_d = dram("hT", [D, NT], F32, ksc)
    tabs_d = dram("tabs", [6, 128, NT], F32, ksc)
    qT_d = dram("qT", [1024, NT], BF16, ksi)
    kT_d = dram("kT", [128, NT], BF16, ksi)
    ikT_d = dram("ikT", [128, NT], BF16, ksi)
    iqT_d = dram("iqT", [1024, NT], BF16, ksi)
    rqT_d = dram("rqT", [512, NT], BF16, ksi)
    rkT_d = dram("rkT", [512, NT], BF16, ksi)
    rgT_d = dram("rgT", [1024, NT], BF16, ksi)
    v_d = dram("v", [NT, 128], BF16, ksi)
    iw_d = dram("iw", [NT, 16], F32, ksi)
    rk_d = dram("rk", [NT, 512], BF16, ksi)
    rv_d = dram("rv", [NT, 1024], BF16, ksi)
    mixT_d = dram("mixT", [D, NT], BF16, ksc)

    with ExitStack() as es:
        fw = FW(nc, es)
        nc._fw = fw

        uid = [0]

        def sb(ctx, name, shape, dt):
            uid[0] += 1
            return ctx.enter_context(nc.sbuf_tensor(f"s{uid[0]}_{name}", shape, dt))

        def pst(ctx, name, shape, dt):
            uid[0] += 1
            return ctx.enter_context(nc.psum_tensor(f"p{uid[0]}_{name}", shape, dt))

        identf = sb(es, "identf", [128, 128], F32)
        identb = sb(es, "identb", [128, 128], BF16)
        onesb = sb(es, "onesb", [128, 128], BF16)
        gains = {n: sb(es, "t_" + n, [128, DEPTH * 16], F32) for n in GAIN_NAMES}
        gn_t = sb(es, "t_gn", [128, DEPTH * 8], F32)
        cbuf = Buf()
        fw.dma("sync", identf[:], cd["c_identf"], writes=[cbuf])
        fw.dma("sync", identb[:], cd["c_identb"], writes=[cbuf])
        for n in GAIN_NAMES:
            for l in range(DEPTH):
                fw.dma("sync", gains[n][:, l * 16:(l + 1) * 16], gains_d[n][l], writes=[cbuf])
        for l in range(DEPTH):
            fw.dma("sync", gn_t[:, l * 8:(l + 1) * 8], gn_d[l], writes=[cbuf])
        fw.op("vector", lambda e: e.memset(onesb[:], 1.0), writes=[cbuf])

        wbuf = {}

        def conv(key, dst, src):
            b = wbuf.setdefault(key, Buf())
            fw.dma("gpsimd", dst, src, writes=[b])

        def conv_layer(l):
            for g, pieces in enumerate(IN_GROUPS):
                for (s0, wd, d0) in pieces:
                    dst = wb_in[l, g].rearrange("p (k c) -> p k c", c=512)[:, :, d0:d0 + wd]
                    src = w_in_d[l][:, s0:s0 + wd].rearrange("(k p) c -> p k c", p=128)
                    conv(("in", l, g), dst, src)
            for g in range(4):
                dst = wb_out[l, g].rearrange("p (k c) -> p k c", c=512)
                src = w_out_d[l][:, g * 512:(g + 1) * 512].rearrange("(k p) c -> p k c", p=128)
                conv(("out", l, g), dst, src)
            for g in range(16):
                dst = wb_ff1[l, g].rearrange("p (k c) -> p k c", c=512)
                src = w_ff1_d[l][:, g * 512:(g + 1) * 512].rearrange("(k p) c -> p k c", p=128)
                conv(("ff1", l, g), dst, src)
            for q in range(4):
                for og in range(4):
                    dst = wb_ff2[l, q * 4 + og].rearrange("p (k c) -> p k c", c=512)
                    src = w_ff2_d[l][q * 2048:(q + 1) * 2048, og * 512:(og + 1) * 512].rearrange(
                        "(k p) c -> p k c", p=128)
                    conv(("ff2", l, q * 4 + og), dst, src)
            for g in range(4):
                dst = wb_ple[l, g].rearrange("p (k c) -> p k c", c=512)
                src = w_ple_d[l][:, g * 512:(g + 1) * 512].rearrange("(k p) c -> p k c", p=128)
                conv(("ple", l, g), dst, src)
                dst = wb_gate[l, g].rearrange("p (k c) -> p k c", c=512)
                src = w_gate_d[l][:, g * 512:(g + 1) * 512].rearrange("(k p) c -> p k c", p=128)
                conv(("gate", l, g), dst, src)

        if not only_seq:
            conv_layer(0)

        def tables_phase():
            with ExitStack() as ph:
                invf = sb(ph, "invf", [128, 3], F32)
                posi = sb(ph, "posi", [128, S], I32)
                posf = sb(ph, "posf", [128, S], F32)
                ys = sb(ph, "ys", [128, S], F32)
                ki = sb(ph, "ki", [128, S], I32)
                kf = sb(ph, "kf", [128, S], F32)
                fr = sb(ph, "fr", [128, S], F32)
                tb = [sb(ph, f"tb{i}", [128, S], F32) for i in range(2)]
                b_invf, b_posi, b_posf, b_ys, b_ki, b_kf, b_fr = bufs(7)
                b_tb = bufs(2)
                fw.dma("sync", invf[:], cd["c_invf"], writes=[b_invf])
                n = 0
                for s in range(nseq):
                    fw.dma("sync", posi[:], pos_d[0:1, s * S:(s + 1) * S].partition_broadcast(128),
                           writes=[b_posi])
                    fw.op("vector", lambda e: e.tensor_copy(out=posf[:], in_=posi[:]),
                          reads=[b_posi], writes=[b_posf])
                    for t in range(3):
                        for cs in range(2):
                            if cs == 0:
                                fw.op("vector", lambda e, t=t: e.tensor_scalar(
                                    out=ys[:], in0=posf[:], scalar1=invf[:, t:t + 1], scalar2=0.25,
                                    op0=ALU.mult, op1=ALU.add), reads=[b_posf, b_invf], writes=[b_ys])
                            else:
                                fw.op("vector", lambda e, t=t: e.tensor_scalar(
                                    out=ys[:], in0=posf[:], scalar1=invf[:, t:t + 1], scalar2=None,
                                    op0=ALU.mult), reads=[b_posf, b_invf], writes=[b_ys])
                            fw.op("vector", lambda e: e.tensor_copy(out=ki[:], in_=ys[:]),
                                  reads=[b_ys], writes=[b_ki])
                            fw.op("vector", lambda e: e.tensor_copy(out=kf[:], in_=ki[:]),
                                  reads=[b_ki], writes=[b_kf])
                            fw.op("vector", lambda e: e.tensor_tensor(out=fr[:], in0=ys[:], in1=kf[:],
                                                                      op=ALU.subtract),
                                  reads=[b_ys, b_kf], writes=[b_fr])
                            fw.op("vector", lambda e: e.tensor_scalar(out=kf[:], in0=fr[:], scalar1=0.0,
                                                                      scalar2=None, op0=ALU.is_lt),
                                  reads=[b_fr], writes=[b_kf])
                            fw.op("vector", lambda e: e.tensor_tensor(out=fr[:], in0=fr[:], in1=kf[:],
                                                                      op=ALU.add),
                                  reads=[b_fr, b_kf], writes=[b_fr])
                            tt = tb[n % 2]
                            bt = b_tb[n % 2]
                            n += 1
                            fw.op("scalar", lambda e, tt=tt: e.activation(
                                out=tt[:], in_=fr[:], func=AF.Sin, scale=-2.0 * np.pi, bias=pib[:, 0:1]),
                                reads=[b_fr, cbuf], writes=[bt])
                            fw.dma("sync", tabs_d[2 * t + cs][:, s * S:(s + 1) * S], tt[:], reads=[bt])
                fw.barrier()

        pib = sb(es, "pib", [128, 2], F32)
        fw.op("vector", lambda e: e.memset(pib[:, 0:1], float(np.pi)), writes=[cbuf])
        fw.op("vector", lambda e: e.memset(pib[:, 1:2], 1e-6), writes=[cbuf])
        epsb = sb(es, "epsb", [128, 1], F32)
        fw.op("vector", lambda e: e.memset(epsb[:], 1e-5), writes=[cbuf])
        if only_seq:
            seq_phase_holder = []
        else:
            tables_phase()
        if stop_after == "tables":
            fw.barrier()
            return nc

        hbuf = bufs(NTILE)

        def dense_phase(l_prev, l_next):
            with ExitStack() as ph:
                hT = sb(ph, "hT", [128, 16 * TT], F32)
                yT = sb(ph, "yT", [128, 16 * TT], F32)
                act = sb(ph, "act", [128, 16 * TT], BF16)
                uT = sb(ph, "uT", [128, 16 * TT], BF16)
                NW = 3
                Wt = [sb(ph, f"Wt{i}", [128, 8192], BF16) for i in range(NW)]
                Wp = [sb(ph, f"Wp{i}", [128, 1024], BF16) for i in range(2)]
                tabs = sb(ph, "tabs_t", [128, 6 * TT], F32)
                rstd = sb(ph, "rstd", [128, TT], F32)
                sqr = [sb(ph, f"sqr{i}", [128, TT], BF16) for i in range(2)]
                tmp = [sb(ph, f"tmp{i}", [128, TT], F32) for i in range(4)]
                qb = [sb(ph, f"qb{i}", [128, TT], BF16) for i in range(2)]
                stg = [sb(ph, f"stg{i}", [128, TT], BF16) for i in range(3)]
                tok = [sb(ph, f"tok{i}", [128, 512], BF16) for i in range(3)]
                iwst = sb(ph, "iwst", [128, 64], F32)
                pin = sb(ph, "pin", [128, 1024], F32)
                pbt = sb(ph, "pbt", [128, 1024], BF16)
                pT = sb(ph, "pT", [128, 1024], BF16)
                pm = sb(ph, "pm", [128, 3 * 128], BF16)
                acc = [pst(ph, f"acc{i}", [128, 512], F32) for i in range(4)]
                ssb = pst(ph, "ssb", [128, 512], F32)
                ppb = [pst(ph, f"ppb{i}", [128, 512], F32) for i in range(2)]
                trb = pst(ph, "trb", [128, 512], F32)
                trb16 = trb[:].bitcast(BF16)

                b_hT, b_yT, b_act = bufs(16), bufs(16), bufs(16)
                b_uT = bufs(16)
                b_W, b_Wp = bufs(NW), bufs(2)
                b_tabs, b_rstd, b_ss, b_trb, b_pin, b_pbt, b_pT, b_pm, b_iwst = bufs(9)
                b_sqr, b_tmp, b_qb, b_stg, b_tok = bufs(2), bufs(4), bufs(2), bufs(3), bufs(3)
                b_acc, b_pp = bufs(4), bufs(2)
                ctr = dict(w=0, acc=0, sqr=0, tmp=0, qb=0, stg=0, tok=0, pp=0, wp=0)

                def nxt(k, n):
                    i = ctr[k] % n
                    ctr[k] += 1
                    return i

                for t in range(3):
                    fw.dma("sync", pm[:, t * 128:(t + 1) * 128], cd["c_pm"][t], writes=[b_pm])

                def hc(c):
                    return hT[:, c * TT:(c + 1) * TT]

                def yc(c):
                    return yT[:, c * TT:(c + 1) * TT]

                def ac(c):
                    return act[:, c * TT:(c + 1) * TT]

                def uc(c):
                    return uT[:, c * TT:(c + 1) * TT]

                plan = []
                for ti_p in range(NTILE):
                    if l_prev is not None:
                        lp = l_prev
                        plan += [(wb_out[lp, og], ("out", lp, og)) for og in range(4)]
                        for q in range(4):
                            plan += [(wb_ff1[lp, q * 4 + g1], ("ff1", lp, q * 4 + g1)) for g1 in range(4)]
                            plan += [(wb_ff2[lp, q * 4 + og], ("ff2", lp, q * 4 + og)) for og in range(4)]
                        plan += [(wb_gate[lp, og], ("gate", lp, og)) for og in range(4)]
                    if l_next is not None:
                        plan += [(wb_in[l_next, g], ("in", l_next, g)) for g in range(11)]
                wstate = dict(issue=0, use=0)

                def wload(src, key):
                    k = wstate["use"]
                    assert plan[k][1] == key, (plan[k][1], key)
                    while wstate["issue"] < min(len(plan), k + NW):
                        ki = wstate["issue"]
                        fw.dma("sync", Wt[ki % NW][:], plan[ki][0], reads=[wbuf[plan[ki][1]]],
                               writes=[b_W[ki % NW]])
                        wstate["issue"] += 1
                    wstate["use"] += 1
                    return Wt[k % NW], b_W[k % NW]

                def gemm_f(W, bW, j, rhs_fn, rhs_bufs, nk):
                    i = nxt("acc", 4)
                    for k in range(nk):
                        fw.op("tensor", lambda e, k=k: e.matmul(
                            acc[i][:], lhsT=W[:, k * 512 + j * 128:k * 512 + (j + 1) * 128], rhs=rhs_fn(k),
                            start=(k == 0), stop=(k == nk - 1)),
                            reads=[bW] + ([rhs_bufs[k]] if len(rhs_bufs) == nk else rhs_bufs),
                            writes=[b_acc[i]], signal=(k == nk - 1))
                    return acc[i], b_acc[i]

                def rstd_from(src_fn, src_bufs):
                    for c in range(16):
                        i = nxt("sqr", 2)
                        fw.op("scalar", lambda e, c=c, i=i: e.activation(out=sqr[i][:], in_=src_fn(c),
                                                                         func=AF.Square),
                              reads=[src_bufs[c]], writes=[b_sqr[i]])
                        fw.op("tensor", lambda e, c=c, i=i: e.matmul(ssb[:], lhsT=onesb[:], rhs=sqr[i][:],
                                                                     start=(c == 0), stop=(c == 15)),
                              reads=[b_sqr[i], cbuf], writes=[b_ss], signal=True)
                    fw.op("scalar", lambda e: e.activation(out=rstd[:], in_=ssb[:], func=AF.Sqrt,
                                                           scale=1.0 / D, bias=pib[:, 1:2]),
                          reads=[b_ss, cbuf], writes=[b_rstd])
                    fw.op("vector", lambda e: e.reciprocal(out=rstd[:], in_=rstd[:]),
                          reads=[b_rstd], writes=[b_rstd])

                def norm_to_act(gname, l):
                    rstd_from(hc, b_hT)
                    g = gains[gname]
                    for c in range(16):
                        fw.op("vector", lambda e, c=c: e.scalar_tensor_tensor(
                            out=ac(c), in0=hc(c), scalar=g[:, l * 16 + c:l * 16 + c + 1], in1=rstd[:],
                            op0=ALU.mult, op1=ALU.mult),
                            reads=[b_hT[c], b_rstd, cbuf], writes=[b_act[c]])

                def norm_add(gname, l):
                    rstd_from(yc, b_yT)
                    g = gains[gname]
                    for c in range(16):
                        i = nxt("tmp", 4)
                        fw.op("vector", lambda e, c=c, i=i: e.scalar_tensor_tensor(
                            out=tmp[i][:], in0=yc(c), scalar=g[:, l * 16 + c:l * 16 + c + 1], in1=rstd[:],
                            op0=ALU.mult, op1=ALU.mult),
                            reads=[b_yT[c], b_rstd, cbuf], writes=[b_tmp[i]])
                        fw.op("gpsimd", lambda e, c=c, i=i: e.tensor_tensor(out=hc(c), in0=hc(c), in1=tmp[i][:],
                                                                            op=ALU.add),
                              reads=[b_tmp[i], b_hT[c]], writes=[b_hT[c]])

                def p1(l, ti):
                    tsl = slice(ti * TT, (ti + 1) * TT)
                    fw.dma("sync", tabs[:].rearrange("p (a t) -> p a t", t=TT),
                           tabs_d[:, :, tsl].rearrange("a p t -> p a t"), writes=[b_tabs])
                    norm_to_act("g_premix", l)

                    pend = []

                    def flush():
                        while pend:
                            pend.pop(0)()

                    def rope_epi(a, ba, typ, scale, dst, after=None):
                        i = nxt("qb", 2)
                        fw.op("scalar", lambda e: e.activation(out=qb[i][:], in_=a[:], func=AF.Copy,
                                                               scale=float(scale)),
                              reads=[ba], writes=[b_qb[i]])
                        ip = nxt("pp", 2)
                        i1 = nxt("tmp", 4)
                        i2 = nxt("tmp", 4)
                        si = nxt("stg", 3)
                        Ct = tabs[:, (2 * typ) * TT:(2 * typ + 1) * TT]
                        St = tabs[:, (2 * typ + 1) * TT:(2 * typ + 2) * TT]
                        fw.op("gpsimd", lambda e: e.tensor_tensor(out=tmp[i1][:], in0=qb[i][:], in1=Ct,
                                                                  op=ALU.mult),
                              reads=[b_qb[i], b_tabs], writes=[b_tmp[i1]])

                        def rest():
                            fw.op("tensor", lambda e: e.matmul(ppb[ip][:], lhsT=pm[:, typ * 128:(typ + 1) * 128],
                                                               rhs=qb[i][:], start=True, stop=True),
                                  reads=[b_qb[i], b_pm], writes=[b_pp[ip]])
                            fw.op("vector", lambda e: e.tensor_tensor(out=tmp[i2][:], in0=ppb[ip][:], in1=St,
                                                                      op=ALU.mult),
                                  reads=[b_pp[ip], b_tabs], writes=[b_tmp[i2]])
                            fw.op("gpsimd", lambda e: e.tensor_tensor(out=stg[si][:], in0=tmp[i1][:],
                                                                      in1=tmp[i2][:], op=ALU.add),
                                  reads=[b_tmp[i1], b_tmp[i2]], writes=[b_stg[si]])
                            fw.dma("sync", dst, stg[si][:], reads=[b_stg[si]])
                            if after is not None:
                                after(si)
                        pend.append(rest)
                        return si

                    rhs_act = lambda k: ac(k)

                    def gemm_p1(W, bW, j):
                        r_ = gemm_f(W, bW, j, lambda k: ac(k), b_act, 16)
                        flush()
                        return r_

                    for g in range(11):
                        W, bW = wload(wb_in[l, g], ("in", l, g))
                        if g in (0, 1):
                            for j in range(4):
                                h = g * 4 + j
                                a, ba = gemm_p1(W, bW, j)
                                rope_epi(a, ba, 0, 128.0 ** -0.5, qT_d[h * 128:(h + 1) * 128, tsl])
                        elif g == 2:
                            a, ba = gemm_p1(W, bW, 0)
                            rope_epi(a, ba, 0, 1.0, kT_d[:, tsl])
                            a, ba = gemm_p1(W, bW, 1)
                            rope_epi(a, ba, 1, 1.0, ikT_d[:, tsl])
                            ti_ = nxt("tok", 3)
                            for tb in range(4):
                                i = nxt("acc", 4)
                                for k in range(16):
                                    fw.op("tensor", lambda e, k=k, tb=tb, i=i: e.matmul(
                                        acc[i][:, 0:144], lhsT=act[:, k * TT + tb * 128:k * TT + (tb + 1) * 128],
                                        rhs=W[:, k * 512 + 256:k * 512 + 400], start=(k == 0), stop=(k == 15)),
                                        reads=[bW] + b_act, writes=[b_acc[i]], signal=(k == 15))
                                fw.op("scalar", lambda e, tb=tb, i=i: e.activation(
                                    out=tok[ti_][:, tb * 128:(tb + 1) * 128], in_=acc[i][:, 0:128], func=AF.Copy),
                                    reads=[b_acc[i]], writes=[b_tok[ti_]])
                                fw.op("scalar", lambda e, tb=tb, i=i: e.activation(
                                    out=iwst[:, tb * 16:(tb + 1) * 16], in_=acc[i][:, 128:144], func=AF.Copy,
                                    scale=1.0 / 32.0),
                                    reads=[b_acc[i]], writes=[b_iwst])
                            fw.dma("sync", v_d[tsl, :].rearrange("(b p) f -> p b f", p=128),
                                   tok[ti_][:].rearrange("p (b f) -> p b f", f=128), reads=[b_tok[ti_]])
                            fw.dma("sync", iw_d[tsl, :].rearrange("(b p) f -> p b f", p=128),
                                   iwst[:].rearrange("p (b f) -> p b f", f=16), reads=[b_iwst])
                        elif g in (3, 4):
                            for j in range(4):
                                pr = (g - 3) * 4 + j
                                a, ba = gemm_p1(W, bW, j)
                                rope_epi(a, ba, 1, 1.0, iqT_d[pr * 128:(pr + 1) * 128, tsl])
                        elif g == 5:
                            for j in range(4):
                                a, ba = gemm_p1(W, bW, j)
                                rope_epi(a, ba, 2, 1.0, rqT_d[j * 128:(j + 1) * 128, tsl])
                        elif g == 6:
                            for j in range(4):
                                a, ba = gemm_p1(W, bW, j)
                                def rk_after(si, j=j):
                                    for tb in range(4):
                                        fw.op("tensor", lambda e, tb=tb: e.transpose(
                                            out=trb16[:, tb * 128:(tb + 1) * 128],
                                            in_=stg[si][:, tb * 128:(tb + 1) * 128], identity=identb[:]),
                                            reads=[b_stg[si], cbuf], writes=[b_trb], signal=(tb == 3))
                                    ti_ = nxt("tok", 3)
                                    fw.op("scalar", lambda e: e.activation(out=tok[ti_][:], in_=trb16[:, 0:512],
                                                                           func=AF.Copy),
                                          reads=[b_trb], writes=[b_tok[ti_]])
                                    fw.dma("sync",
                                           rk_d[tsl, j * 128:(j + 1) * 128].rearrange("(b p) f -> p b f", p=128),
                                           tok[ti_][:].rearrange("p (b f) -> p b f", f=128), reads=[b_tok[ti_]])
                                rope_epi(a, ba, 2, 0.125, rkT_d[j * 128:(j + 1) * 128, tsl], after=rk_after)
                        elif g in (7, 8):
                            for tb in range(4):
                                i = nxt("acc", 4)
                                for k in range(16):
                                    fw.op("tensor", lambda e, k=k, tb=tb, i=i: e.matmul(
                                        acc[i][:], lhsT=act[:, k * TT + tb * 128:k * TT + (tb + 1) * 128],
                                        rhs=W[:, k * 512:(k + 1) * 512], start=(k == 0), stop=(k == 15)),
                                        reads=[bW] + b_act, writes=[b_acc[i]], signal=(k == 15))
                                ti_ = nxt("tok", 3)
                                fw.op("scalar", lambda e, i=i: e.activation(out=tok[ti_][:], in_=acc[i][:],
                                                                            func=AF.Copy),
                                      reads=[b_acc[i]], writes=[b_tok[ti_]])
                                r0 = ti * TT + tb * 128
                                fw.dma("sync", rv_d[r0:r0 + 128, (g - 7) * 512:(g - 6) * 512], tok[ti_][:],
                                       reads=[b_tok[ti_]])
                        else:
                            for j in range(4):
                                hh = (g - 9) * 4 + j
                                a, ba = gemm_p1(W, bW, j)
                                si = nxt("stg", 3)
                                fw.op("scalar", lambda e: e.activation(out=stg[si][:], in_=a[:], func=AF.Silu),
                                      reads=[ba], writes=[b_stg[si]])
                                fw.dma("sync", rgT_d[hh * 128:(hh + 1) * 128, tsl], stg[si][:], reads=[b_stg[si]])
                    flush()

                def p3(l, ti):
                    tsl = slice(ti * TT, (ti + 1) * TT)
                    fw.dma("sync", act[:].rearrange("p (c t) -> p c t", t=TT),
                           mixT_d[:, tsl].rearrange("(c p) t -> p c t", p=128), writes=b_act)
                    rhs_act = lambda k: ac(k)
                    for og in range(4):
                        W, bW = wload(wb_out[l, og], ("out", l, og))
                        for j in range(4):
                            oc = og * 4 + j
                            a, ba = gemm_f(W, bW, j, rhs_act, b_act, 16)
                            fw.op("scalar", lambda e, oc=oc: e.activation(out=yc(oc), in_=a[:], func=AF.Copy),
                                  reads=[ba], writes=[b_yT[oc]])
                    norm_add("g_postmix", l)
                    norm_to_act("g_preff", l)
                    for q in range(4):
                        for g1 in range(4):
                            W, bW = wload(wb_ff1[l, q * 4 + g1], ("ff1", l, q * 4 + g1))
                            for j in range(4):
                                ucx = g1 * 4 + j
                                a, ba = gemm_f(W, bW, j, rhs_act, b_act, 16)
                                i = nxt("tmp", 4)
                                fw.op("scalar", lambda e, i=i: e.activation(out=tmp[i][:], in_=a[:], func=AF.Relu),
                                      reads=[ba], writes=[b_tmp[i]])
                                fw.op("gpsimd", lambda e, i=i, ucx=ucx: e.tensor_tensor(
                                    out=uc(ucx), in0=tmp[i][:], in1=tmp[i][:], op=ALU.mult),
                                    reads=[b_tmp[i]], writes=[b_uT[ucx]])
                        for og in range(4):
                            W, bW = wload(wb_ff2[l, q * 4 + og], ("ff2", l, q * 4 + og))
                            for j in range(4):
                                oc = og * 4 + j
                                a, ba = gemm_f(W, bW, j, lambda k: uc(k), b_uT, 16)
                                if q == 0:
                                    fw.op("scalar", lambda e, oc=oc: e.activation(out=yc(oc), in_=a[:],
                                                                                  func=AF.Copy),
                                          reads=[ba], writes=[b_yT[oc]])
                                else:
                                    fw.op("vector", lambda e, oc=oc: e.tensor_tensor(out=yc(oc), in0=yc(oc),
                                                                                     in1=a[:], op=ALU.add),
                                          reads=[ba, b_yT[oc]], writes=[b_yT[oc]])
                    norm_add("g_postff", l)
                    fw.dma("sync", pin[:].rearrange("p (b f) -> p b f", f=256),
                           p_d[l][tsl, :].rearrange("(b p) f -> p b f", p=128), writes=[b_pin])
                    fw.op("vector", lambda e: e.tensor_copy(out=pbt[:], in_=pin[:]), reads=[b_pin], writes=[b_pbt])
                    for k2 in range(2):
                        for tb in range(4):
                            fw.op("tensor", lambda e, k2=k2, tb=tb: e.transpose(
                                out=trb16[:, tb * 128:(tb + 1) * 128],
                                in_=pbt[:, tb * 256 + k2 * 128:tb * 256 + (k2 + 1) * 128], identity=identb[:]),
                                reads=[b_pbt, cbuf], writes=[b_trb], signal=(tb == 3))
                        fw.op("scalar", lambda e, k2=k2: e.activation(out=pT[:, k2 * 512:(k2 + 1) * 512],
                                                                      in_=trb16[:, 0:512], func=AF.Copy),
                              reads=[b_trb], writes=[b_pT])
                    for c in range(16):
                        fw.op("gpsimd", lambda e, c=c: e.tensor_copy(out=ac(c), in_=hc(c)),
                              reads=[b_hT[c]], writes=[b_act[c]])
                    for og in range(4):
                        W, bW = wload(wb_gate[l, og], ("gate", l, og))
                        ip = nxt("wp", 2)
                        fw.dma("sync", Wp[ip][:], wb_ple[l, og], reads=[wbuf[("ple", l, og)]], writes=[b_Wp[ip]])
                        for j in range(4):
                            oc = og * 4 + j
                            a, ba = gemm_f(W, bW, j, rhs_act, b_act, 16)
                            a2, ba2 = gemm_f(Wp[ip], b_Wp[ip], j, lambda k: pT[:, k * 512:(k + 1) * 512], [b_pT], 2)
                            i = nxt("tmp", 4)
                            fw.op("scalar", lambda e, i=i: e.activation(out=tmp[i][:], in_=a[:], func=AF.Sigmoid),
                                  reads=[ba], writes=[b_tmp[i]])
                            fw.op("vector", lambda e, i=i, oc=oc: e.tensor_tensor(out=yc(oc), in0=tmp[i][:],
                                                                                  in1=a2[:], op=ALU.mult),
                                  reads=[b_tmp[i], ba2], writes=[b_yT[oc]])
                    norm_add("g_ple", l)

                for ti in range(NTILE):
                    tsl = slice(ti * TT, (ti + 1) * TT)
                    if l_prev is None:
                        for tb in range(4):
                            r0 = ti * TT + tb * 128
                            fw.dma("sync", yT[:, tb * 2048:(tb + 1) * 2048], x_d[r0:r0 + 128, :],
                                   writes=b_yT[tb * 4:(tb + 1) * 4])
                        for c in range(16):
                            for tb in range(4):
                                fw.op("tensor", lambda e, c=c, tb=tb: e.transpose(
                                    out=trb[:, tb * 128:(tb + 1) * 128],
                                    in_=yT[:, tb * 2048 + c * 128:tb * 2048 + (c + 1) * 128], identity=identf[:]),
                                    reads=b_yT[tb * 4:(tb + 1) * 4] + [cbuf], writes=[b_trb], signal=(tb == 3))
                            fw.op("scalar", lambda e, c=c: e.activation(out=hc(c), in_=trb[:], func=AF.Copy),
                                  reads=[b_trb], writes=[b_hT[c]])
                    else:
                        fw.dma("sync", hT[:].rearrange("p (c t) -> p c t", t=TT),
                               hT_d[:, tsl].rearrange("(c p) t -> p c t", p=128),
                               reads=[hbuf[ti]], writes=b_hT)
                        p3(l_prev, ti)
                    if l_next is not None:
                        fw.dma("sync", hT_d[:, tsl].rearrange("(c p) t -> p c t", p=128),
                               hT[:].rearrange("p (c t) -> p c t", t=TT), reads=b_hT, writes=[hbuf[ti]])
                        p1(l_next, ti)
                    else:
                        for tb in range(4):
                            for c4 in range(4):
                                for cc in range(4):
                                    c = c4 * 4 + cc
                                    fw.op("tensor", lambda e, c=c, cc=cc, tb=tb: e.transpose(
                                        out=trb[:, cc * 128:(cc + 1) * 128],
                                        in_=hT[:, c * TT + tb * 128:c * TT + (tb + 1) * 128], identity=identf[:]),
                                        reads=[b_hT[c], cbuf], writes=[b_trb], signal=(cc == 3))
                                fw.op("scalar", lambda e, c4=c4, tb=tb: e.activation(
                                    out=yT[:, tb * 2048 + c4 * 512:tb * 2048 + (c4 + 1) * 512], in_=trb[:],
                                    func=AF.Copy), reads=[b_trb], writes=[b_yT[tb * 4 + c4]])
                            r0 = ti * TT + tb * 128
                            fw.dma("sync", out_d[r0:r0 + 128, :], yT[:, tb * 2048:(tb + 1) * 2048],
                                   reads=b_yT[tb * 4:(tb + 1) * 4])
                fw.barrier()

        def seq_phase(l):
            with ExitStack() as ph:
                NBUF = 2
                irep = sb(ph, "irep", [128, 512], BF16)
                causb = sb(ph, "causb", [128, 128], BF16)
                causf = sb(ph, "causf", [128, 128], F32)
                dtt = sb(ph, "dtt", [128, 1024], F32)
                xit = sb(ph, "xit", [128, 512], F32)
                ztt = sb(ph, "ztt", [128, 512], F32)
                gtt = sb(ph, "gtt", [128, 4], F32)
                bist = sb(ph, "bist", [128, KBIS], F32)
                pS = pst(ph, "pS", [128, 1024], F32)
                pO = pst(ph, "pO", [128, 1024], F32)
                pD = pst(ph, "pD", [128, 1024], F32)
                pD16 = pD[:].bitcast(BF16)
                b_c = Buf()
                for tdst, nm in [(irep, "c_irep"), (causb, "c_causb"), (causf, "c_causf"), (dtt, "c_dt"),
                                 (xit, "c_xi"), (ztt, "c_zt"), (gtt, "c_gt"), (bist, "c_bis")]:
                    fw.dma("sync", tdst[:], cd[nm], writes=[b_c])
                b_pS, b_pO, b_pD = Buf(), Buf(), Buf()

                class St:
                    pass

                sts = []
                for s in range(nseq):
                    z = St()
                    z.s = s
                    z.kTs = sb(ph, "kTs", [128, S], BF16)
                    z.ikTs = sb(ph, "ikTs", [128, S], BF16)
                    z.Vs = sb(ph, "Vs", [128, S], BF16)
                    z.qTb = [sb(ph, "qTb", [128, 1024], BF16) for i in range(NBUF)]
                    z.iqTb = [sb(ph, "iqTb", [128, 1024], BF16) for i in range(NBUF)]
                    z.iwb = [sb(ph, "iwb", [128, 16], F32) for i in range(NBUF)]
                    z.rqTb = [sb(ph, "rqTb", [128, 512], BF16) for i in range(NBUF)]
                    z.rkTb = [sb(ph, "rkTb", [128, 512], BF16) for i in range(NBUF)]
                    z.rkb = [sb(ph, "rkb", [128, 512], BF16) for i in range(NBUF)]
                    z.rvb = [sb(ph, "rvb", [128, 1024], BF16) for i in range(NBUF)]
                    z.rgTb = [sb(ph, "rgTb", [128, 1024], BF16) for i in range(NBUF)]
                    z.sacc = sb(ph, "sacc", [128, S], F32)
                    z.work = sb(ph, "work", [128, S], F32)
                    z.mb = sb(ph, "mb", [128, S], BF16)
                    z.mx = sb(ph, "mx", [128, 8], F32)
                    z.steps = sb(ph, "steps", [128, KBIS], F32)
                    z.rr = [sb(ph, "rr", [128, 512], F32) for i in range(2)]
                    z.ET = [sb(ph, "ET", [128, 1024], BF16) for i in range(2)]
                    z.rec = sb(ph, "rec", [128, 1024], F32)
                    z.ast = sb(ph, "ast", [128, 1024], BF16)
                    z.Aall = sb(ph, "Aall", [128, 1024], BF16)
                    z.rqxi = sb(ph, "rqxi", [128, 512], BF16)
                    z.rkz = sb(ph, "rkz", [128, 512], BF16)
                    z.Rst = sb(ph, "Rst", [128, 512], F32)
                    z.Rb = sb(ph, "Rb", [128, 512], BF16)
                    z.ocp = sb(ph, "ocp", [128, 1024], F32)
                    z.osq = sb(ph, "osq", [128, 1024], F32)
                    z.st = sb(ph, "st", [128, 64], F32)
                    z.yb = sb(ph, "yb", [128, 1024], BF16)
                    z.rst = sb(ph, "rst", [128, 1024], BF16)
                    z.pA = pst(ph, "pA", [128, 512], F32)
                    (z.b_kTs, z.b_ikTs, z.b_Vs, z.b_sacc, z.b_work, z.b_mb, z.b_mx, z.b_rec, z.b_ast, z.b_Aall,
                     z.b_rqxi, z.b_rkz, z.b_R, z.b_Rb, z.b_ocp, z.b_osq, z.b_st, z.b_yb, z.b_rst, z.b_pA) = bufs(20)
                    z.b_blk = bufs(NBUF)
                    z.b_rr, z.b_ET = bufs(2), bufs(2)
                    z.ctr = dict(rr=0, et=0)
                    sts.append(z)

                def nxt(z, k, n):
                    i = z.ctr[k] % n
                    z.ctr[k] += 1
                    return i

                def seq_setup(z):
                    ssl = slice(z.s * S, (z.s + 1) * S)
                    fw.dma("sync", z.kTs[:], kT_d[:, ssl], writes=[z.b_kTs])
                    fw.dma("sync", z.ikTs[:], ikT_d[:, ssl], writes=[z.b_ikTs])
                    fw.dma("sync", z.Vs[:].rearrange("p (b f) -> p b f", f=128),
                           v_d[ssl, :].rearrange("(b p) f -> p b f", p=128), writes=[z.b_Vs])
                    fw.op("vector", lambda e: e.memset(z.Rst[:], 0.0), writes=[z.b_R])
                    fw.op("vector", lambda e: e.memset(z.Rb[:], 0.0), writes=[z.b_Rb])

                def blk(z, j):
                    bi = j % NBUF
                    bb = z.b_blk[bi]
                    qTb, iqTb, iwb, rqTb, rkTb, rkb, rvb, rgTb = (z.qTb[bi], z.iqTb[bi], z.iwb[bi], z.rqTb[bi],
                                                                 z.rkTb[bi], z.rkb[bi], z.rvb[bi], z.rgTb[bi])
                    sacc, work, mb, mx = z.sacc, z.work, z.mb, z.mx
                    t0 = z.s * S + j * 128
                    bsl = slice(t0, t0 + 128)
                    nk = (j + 1) * 128
                    for (dst, src, w_) in [(qTb, qT_d, 128), (iqTb, iqT_d, 128), (rqTb, rqT_d, 128),
                                           (rkTb, rkT_d, 128), (rgTb, rgT_d, 128)]:
                        fw.dma("sync", dst[:].rearrange("p (h t) -> p h t", t=128),
                               src[:, bsl].rearrange("(h p) t -> p h t", p=128), writes=[bb])
                    fw.dma("sync", iwb[:], iw_d[bsl, :], writes=[bb])
                    fw.dma("sync", rkb[:], rk_d[bsl, :], writes=[bb])
                    fw.dma("sync", rvb[:], rv_d[bsl, :], writes=[bb])
                    yield
                    if j >= 2:
                        npc = (nk + 511) // 512
                        for pc in range(npc):
                            w = min(512, nk - pc * 512)
                            csl = slice(pc * 512, pc * 512 + w)
                            for h in range(16):
                                base = 64 * (h % 2)
                                pr = h // 2
                                fw.op("tensor", lambda e: e.matmul(
                                    z.pA[:, 0:w], lhsT=iqTb[base:base + 64, pr * 128:(pr + 1) * 128],
                                    rhs=z.ikTs[base:base + 64, csl], start=True, stop=True),
                                    reads=[bb, z.b_ikTs], writes=[z.b_pA])
                                ir = nxt(z, "rr", 2)
                                fw.op("scalar", lambda e: e.activation(out=z.rr[ir][:, 0:w], in_=z.pA[:, 0:w],
                                                                       func=AF.Relu),
                                      reads=[z.b_pA], writes=[z.b_rr[ir]])
                                if h == 0:
                                    fw.op("vector", lambda e: e.tensor_scalar(
                                        out=sacc[:, csl], in0=z.rr[ir][:, 0:w], scalar1=iwb[:, 0:1],
                                        scalar2=None, op0=ALU.mult),
                                        reads=[z.b_rr[ir], bb], writes=[z.b_sacc])
                                else:
                                    fw.op("vector", lambda e: e.scalar_tensor_tensor(
                                        out=sacc[:, csl], in0=z.rr[ir][:, 0:w], scalar=iwb[:, h:h + 1],
                                        in1=sacc[:, csl], op0=ALU.mult, op1=ALU.add),
                                        reads=[z.b_rr[ir], bb, z.b_sacc], writes=[z.b_sacc])
                                yield
                        fw.op("vector", lambda e: e.tensor_reduce(out=mx[:, 1:2], in_=sacc[:, 0:nk], axis=AX.X,
                                                                  op=ALU.min),
                              reads=[z.b_sacc], writes=[z.b_mx])
                        yield
                        dsl = slice(j * 128, (j + 1) * 128)
                        fw.op("vector", lambda e: e.tensor_tensor(out=sacc[:, dsl], in0=sacc[:, dsl],
                                                                  in1=causf[:], op=ALU.add),
                              reads=[z.b_sacc, b_c], writes=[z.b_sacc])
                        yield
                        fw.op("vector", lambda e: e.tensor_reduce(out=mx[:, 0:1], in_=sacc[:, 0:nk], axis=AX.X,
                                                                  op=ALU.max),
                              reads=[z.b_sacc], writes=[z.b_mx])
                        yield
                        fw.op("vector", lambda e: e.tensor_tensor(out=mx[:, 2:3], in0=mx[:, 0:1], in1=mx[:, 1:2],
                                                                  op=ALU.subtract),
                              reads=[z.b_mx], writes=[z.b_mx])
                        yield
                        fw.op("vector", lambda e: e.tensor_scalar(out=z.steps[:], in0=bist[:], scalar1=mx[:, 2:3],
                                                                  scalar2=None, op0=ALU.mult),
                              reads=[z.b_mx, b_c], writes=[z.b_mx])
                        fw.op("vector", lambda e: e.scalar_tensor_tensor(
                            out=mx[:, 3:4], in0=mx[:, 2:3], scalar=0.5, in1=mx[:, 1:2], op0=ALU.mult, op1=ALU.add),
                            reads=[z.b_mx], writes=[z.b_mx])
                        yield
                        for k in range(KBIS):
                            fw.op("vector", lambda e: e.tensor_scalar(
                                out=work[:, 0:nk], in0=sacc[:, 0:nk], scalar1=mx[:, 3:4], scalar2=None,
                                op0=ALU.is_ge, op1=ALU.add, accum_out=mx[:, 4:5]),
                                reads=[z.b_sacc, z.b_mx], writes=[z.b_work, z.b_mx])
                            yield
                            fw.op("vector", lambda e: e.tensor_scalar(
                                out=mx[:, 5:6], in0=mx[:, 4:5], scalar1=255.5, scalar2=2.0,
                                op0=ALU.is_ge, op1=ALU.mult), reads=[z.b_mx], writes=[z.b_mx])
                            yield
                            fw.op("vector", lambda e: e.tensor_scalar(
                                out=mx[:, 5:6], in0=mx[:, 5:6], scalar1=-1.0, scalar2=z.steps[:, k:k + 1],
                                op0=ALU.add, op1=ALU.mult), reads=[z.b_mx], writes=[z.b_mx])
                            yield
                            fw.op("vector", lambda e: e.tensor_tensor(out=mx[:, 3:4], in0=mx[:, 3:4], in1=mx[:, 5:6],
                                                                      op=ALU.add),
                                  reads=[z.b_mx], writes=[z.b_mx])
                            yield
                        fw.op("vector", lambda e: e.tensor_tensor(out=mx[:, 7:8], in0=mx[:, 3:4],
                                                                  in1=z.steps[:, KBIS - 1:KBIS], op=ALU.subtract),
                              reads=[z.b_mx], writes=[z.b_mx])
                        yield
                        fw.op("vector", lambda e: e.tensor_scalar(
                            out=mb[:, 0:nk], in0=sacc[:, 0:nk], scalar1=mx[:, 7:8], scalar2=-30000.0,
                            op0=ALU.is_lt, op1=ALU.mult), reads=[z.b_sacc, z.b_mx], writes=[z.b_mb])
                    else:
                        if j == 1:
                            fw.op("vector", lambda e: e.memset(mb[:, 0:128], 0.0), writes=[z.b_mb])
                        dsl = slice(j * 128, (j + 1) * 128)
                        fw.op("vector", lambda e: e.tensor_copy(out=mb[:, dsl], in_=causb[:]),
                              reads=[b_c], writes=[z.b_mb])
                    yield
                    for kc in range(j + 1):
                        ksl = slice(kc * 128, (kc + 1) * 128)
                        for half in range(2):
                            hs = slice(half * 512, (half + 1) * 512)
                            fw.op("tensor", lambda e: e.matmul(
                                pS[:, hs], lhsT=z.kTs[:, ksl], rhs=qTb[:, hs], start=True, stop=False),
                                reads=[z.b_kTs, bb], writes=[b_pS], signal=False)
                            fw.op("tensor", lambda e: e.matmul(
                                pS[:, hs], lhsT=mb[:, ksl], rhs=irep[:], start=False, stop=True),
                                reads=[z.b_mb, b_c], writes=[b_pS], signal=(half == 1))
                        ie = nxt(z, "et", 2)
                        fw.op("scalar", lambda e: e.activation(out=z.ET[ie][:], in_=pS[:], func=AF.Exp),
                              reads=[b_pS], writes=[z.b_ET[ie]])
                        for half in range(2):
                            hs = slice(half * 512, (half + 1) * 512)
                            fw.op("tensor", lambda e: e.matmul(
                                pO[:, hs], lhsT=z.Vs[:, ksl], rhs=z.ET[ie][:, hs], start=(kc == 0), stop=(kc == j)),
                                reads=[z.b_Vs, z.b_ET[ie]], writes=[b_pO], signal=False)
                            fw.op("tensor", lambda e: e.matmul(
                                pD[:, hs], lhsT=onesb[:], rhs=z.ET[ie][:, hs], start=(kc == 0), stop=(kc == j)),
                                reads=[cbuf, z.b_ET[ie]], writes=[b_pD], signal=(half == 1))
                    fw.op("vector", lambda e: e.reciprocal(out=z.rec[:], in_=pD[:]), reads=[b_pD], writes=[z.b_rec])
                    fw.op("vector", lambda e: e.tensor_tensor(out=z.ast[:], in0=pO[:], in1=z.rec[:], op=ALU.mult),
                          reads=[b_pO, z.b_rec], writes=[z.b_ast])
                    fw.dma("sync", mixT_d[0:1024, bsl].rearrange("(h p) t -> p h t", p=128),
                           z.ast[:].rearrange("p (h t) -> p h t", t=128), reads=[z.b_ast])
                    yield
                    for h in range(8):
                        base = 64 * (h % 2)
                        c = h // 2
                        fw.op("tensor", lambda e: e.matmul(
                            pD[:, (h % 2) * 512 + c * 128:(h % 2) * 512 + (c + 1) * 128],
                            lhsT=rkTb[base:base + 64, c * 128:(c + 1) * 128],
                            rhs=rqTb[base:base + 64, c * 128:(c + 1) * 128], start=True, stop=True),
                            reads=[bb], writes=[b_pD], signal=(h == 7))
                    fw.op("vector", lambda e: e.tensor_tensor(out=z.Aall[:], in0=pD[:], in1=dtt[:], op=ALU.mult),
                          reads=[b_pD, b_c], writes=[z.b_Aall])
                    fw.op("gpsimd", lambda e: e.tensor_tensor(out=z.rqxi[:], in0=rqTb[:], in1=xit[:], op=ALU.mult),
                          reads=[bb, b_c], writes=[z.b_rqxi])
                    fw.op("gpsimd", lambda e: e.tensor_tensor(out=z.rkz[:], in0=rkb[:], in1=ztt[:], op=ALU.mult),
                          reads=[bb, b_c], writes=[z.b_rkz])
                    yield
                    for h in range(8):
                        base = 64 * (h % 2)
                        c = h // 2
                        hsl = slice(h * 128, (h + 1) * 128)
                        asl = slice((h % 2) * 512 + c * 128, (h % 2) * 512 + (c + 1) * 128)
                        fw.op("tensor", lambda e: e.matmul(
                            pS[:, hsl], lhsT=z.Aall[:, asl], rhs=rvb[:, hsl], start=True, stop=False),
                            reads=[z.b_Aall, bb], writes=[b_pS], signal=False)
                        fw.op("tensor", lambda e: e.matmul(
                            pS[:, hsl], lhsT=z.rqxi[base:base + 64, c * 128:(c + 1) * 128],
                            rhs=z.Rb[base:base + 64, c * 128:(c + 1) * 128], start=False, stop=True),
                            reads=[z.b_rqxi, z.b_Rb], writes=[b_pS], signal=(h == 7))
                    fw.op("scalar", lambda e: e.activation(out=z.ocp[:], in_=pS[:], func=AF.Copy),
                          reads=[b_pS], writes=[z.b_ocp])
                    fw.op("scalar", lambda e: e.activation(out=z.osq[:], in_=pS[:], func=AF.Square),
                          reads=[b_pS], writes=[z.b_osq])
                    for c in range(4):
                        fw.op("tensor", lambda e: e.matmul(
                            pO[:, c * 256:(c + 1) * 256], lhsT=z.rkz[:, c * 128:(c + 1) * 128],
                            rhs=rvb[:, c * 256:(c + 1) * 256], start=True, stop=True),
                            reads=[z.b_rkz, bb], writes=[b_pO], signal=(c == 3))
                    for c in range(4):
                        for hh in range(2):
                            ps_ = slice(hh * 64, (hh + 1) * 64)
                            fw.op("vector", lambda e: e.scalar_tensor_tensor(
                                out=z.Rst[ps_, c * 128:(c + 1) * 128], in0=z.Rst[ps_, c * 128:(c + 1) * 128],
                                scalar=gtt[ps_, c:c + 1],
                                in1=pO[ps_, c * 256 + hh * 128:c * 256 + (hh + 1) * 128],
                                op0=ALU.mult, op1=ALU.add),
                                reads=[z.b_R, b_pO, b_c], writes=[z.b_R])
                    fw.op("vector", lambda e: e.tensor_copy(out=z.Rb[:], in_=z.Rst[:]), reads=[z.b_R], writes=[z.b_Rb])
                    yield
                    st = z.st
                    fw.op("vector", lambda e: e.tensor_reduce(
                        out=st[:, 0:8], in_=z.ocp[:].rearrange("p (h f) -> p h f", f=128), axis=AX.X, op=ALU.add),
                        reads=[z.b_ocp], writes=[z.b_st])
                    fw.op("vector", lambda e: e.tensor_reduce(
                        out=st[:, 8:16], in_=z.osq[:].rearrange("p (h f) -> p h f", f=128), axis=AX.X, op=ALU.add),
                        reads=[z.b_osq], writes=[z.b_st])
                    yield
                    fw.op("vector", lambda e: e.tensor_scalar(out=st[:, 16:24], in0=st[:, 0:8],
                                                              scalar1=1.0 / 128, scalar2=None, op0=ALU.mult),
                          reads=[z.b_st], writes=[z.b_st])
                    yield
                    fw.op("vector", lambda e: e.tensor_tensor(out=st[:, 24:32], in0=st[:, 16:24],
                                                              in1=st[:, 16:24], op=ALU.mult),
                          reads=[z.b_st], writes=[z.b_st])
                    yield
                    fw.op("vector", lambda e: e.scalar_tensor_tensor(
                        out=st[:, 32:40], in0=st[:, 8:16], scalar=1.0 / 128, in1=st[:, 24:32],
                        op0=ALU.mult, op1=ALU.subtract), reads=[z.b_st], writes=[z.b_st])
                    fw.op("scalar", lambda e: e.activation(out=st[:, 40:48], in_=st[:, 32:40], func=AF.Sqrt,
                                                           bias=epsb[:, 0:1]),
                          reads=[z.b_st, cbuf], writes=[z.b_st])
                    yield
                    fw.op("vector", lambda e: e.reciprocal(out=st[:, 40:48], in_=st[:, 40:48]),
                          reads=[z.b_st], writes=[z.b_st])
                    yield
                    fw.op("vector", lambda e: e.scalar_tensor_tensor(
                        out=st[:, 48:56], in0=st[:, 16:24], scalar=-1.0, in1=st[:, 40:48],
                        op0=ALU.mult, op1=ALU.mult), reads=[z.b_st], writes=[z.b_st])
                    for h in range(8):
                        hsl = slice(h * 128, (h + 1) * 128)
                        fw.op("gpsimd", lambda e: e.tensor_scalar(
                            out=z.yb[:, hsl], in0=z.ocp[:, hsl], scalar1=st[:, 40 + h:41 + h],
                            scalar2=st[:, 48 + h:49 + h], op0=ALU.mult, op1=ALU.add),
                            reads=[z.b_ocp, z.b_st], writes=[z.b_yb])
                    yield
                    for h in range(8):
                        hsl = slice(h * 128, (h + 1) * 128)
                        fw.op("tensor", lambda e: e.transpose(out=pD16[:, hsl], in_=z.yb[:, hsl],
                                                              identity=identb[:]),
                              reads=[z.b_yb, cbuf], writes=[b_pD], signal=(h == 7))
                    for h in range(8):
                        hsl = slice(h * 128, (h + 1) * 128)
                        fw.op("vector", lambda e: e.scalar_tensor_tensor(
                            out=z.rst[:, hsl], in0=pD16[:, hsl], scalar=gn_t[:, l * 8 + h:l * 8 + h + 1],
                            in1=rgTb[:, hsl], op0=ALU.mult, op1=ALU.mult),
                            reads=[b_pD, bb, cbuf], writes=[z.b_rst])
                    fw.dma("sync", mixT_d[1024:2048, bsl].rearrange("(h p) t -> p h t", p=128),
                           z.rst[:].rearrange("p (h t) -> p h t", t=128), reads=[z.b_rst])
                    yield

                for z in sts:
                    seq_setup(z)
                for j in range(nblk):
                    gens = [blk(z, j) for z in sts]
                    while gens:
                        for g in list(gens):
                            try:
                                next(g)
                            except StopIteration:
                                gens.remove(g)
                fw.barrier()


        if only_seq:
            seq_phase(0)
            fw.barrier()
            return nc
        dense_phase(None, 0)
        if stop_after == "p1":
            return nc
        for l in range(nlayers):
            if l + 1 < nlayers:
                conv_layer(l + 1)
            seq_phase(l)
            if stop_after == f"p2_{l}":
                return nc
            dense_phase(l, l + 1 if l + 1 < nlayers else None)
        fw.barrier()
    return nc


_NC_CACHE = {}


def _prep_core_inputs(inputs, c, nseq, consts):
    b0 = c * nseq
    m = {}
    m["x"] = np.ascontiguousarray(inputs["x"][b0:b0 + nseq]).reshape(nseq * S, D)
    m["p"] = np.ascontiguousarray(inputs["p"][:, b0:b0 + nseq]).reshape(DEPTH, nseq * S, 256)
    m["pos"] = np.ascontiguousarray(inputs["positions"][b0:b0 + nseq]).reshape(1, nseq * S).astype(np.int32)
    return m


def _shared_inputs(inputs):
    m = {}
    for k_, n_ in [("w_in", "w_in"), ("w_out", "w_out"), ("w_ff1", "w_ff1"), ("w_ff2", "w_ff2"),
                   ("w_ple", "w_ple"), ("w_ple_gate", "w_gate")]:
        m[n_] = np.ascontiguousarray(np.asarray(inputs[k_], dtype=np.float32))
    for k_, n_ in [("pre_mix_norm", "g_premix"), ("post_mix_norm", "g_postmix"), ("pre_ff_norm", "g_preff"),
                   ("post_ff_norm", "g_postff"), ("ple_norm", "g_ple")]:
        g = np.asarray(inputs[k_], dtype=np.float32)
        m[n_] = np.ascontiguousarray(g.reshape(DEPTH, 16, 128).transpose(0, 2, 1))
    g = np.asarray(inputs["ret_gn"], dtype=np.float32)
    m["g_gn"] = np.ascontiguousarray(g.reshape(DEPTH, 8, 128).transpose(0, 2, 1))
    m.update(_consts())
    return m


def kernel(**inputs):
    inputs = {k: np.asarray(v) for k, v in inputs.items()}
    B = inputs["x"].shape[0]
    ncores = 8
    nseq = B // ncores
    if "nc" not in _NC_CACHE:
        _NC_CACHE["nc"] = build(nseq=nseq)
    nc = _NC_CACHE["nc"]
    shared = _shared_inputs(inputs)
    in_maps = []
    for c in range(ncores):
        m = dict(shared)
        m.update(_prep_core_inputs(inputs, c, nseq, None))
        in_maps.append(m)
    res = run_bass_kernel_spmd(nc, in_maps, core_ids=list(range(ncores)))
    outs = [np.asarray(r["out"]).reshape(nseq, S, D) for r in res.results]
    return np.concatenate(outs, axis=0).astype(np.float32)
```
